# Optimizing a Trainium2 kernel written in Bass

```python
import math
import jax
import jax.numpy as jnp
from jax import lax
import numpy as np

D_MODEL = 1024
BATCH = 8
SEQ = 2048
DEPTH = 4

CHUNK = 64
MAX_STREAM_CHUNKS = 64
POOL_WINDOWS = (2, 4, 8, 16)
POOL_GROUPS = len(POOL_WINDOWS)
D_POOL = D_MODEL // 2
POOL_GW = D_POOL // POOL_GROUPS
D_CONV = D_MODEL // 2
CONV_WIDTH = 31
N_HEADS = 8
QK_NOPE = 64
QK_ROPE = 32
V_HEAD = 64
Q_LORA = 384
KV_LORA = 256
ROPE_THETA = 10000.0
Q_BLOCK = 128
N_BRANCH = 3
OFF_CONV = D_POOL
OFF_Q = OFF_CONV + 2 * D_CONV
OFF_KV = OFF_Q + Q_LORA
OFF_KR = OFF_KV + KV_LORA
OFF_GATE = OFF_KR + QK_ROPE
D_IN = OFF_GATE + N_BRANCH * D_MODEL
IN_SPLITS = (OFF_CONV, OFF_Q, OFF_KV, OFF_KR, OFF_GATE)
N_EXPERTS = 16
N_GROUPS = 4
EXPERTS_PER_GROUP = N_EXPERTS // N_GROUPS
TOPK_GROUPS = 1
TOP_K = 2
D_EXPERT = 512
MOE_BLOCK = 128
D_PLE = 256
DEEPNORM_ALPHA = (2 * DEPTH) ** 0.25
DEEPNORM_BETA = (8 * DEPTH) ** -0.25
LN_EPS = 1e-5
RMS_EPS = 1e-6

kernel_name = 'hybrid_pool_conv_mla_moe_deepnorm'


def layer_norm(x, g, b):
    xf = x.astype(jnp.float32)
    mu = jnp.mean(xf, axis=-1, keepdims=True)
    xc = xf - mu
    var = jnp.mean(xc * xc, axis=-1, keepdims=True)
    return (xc * lax.rsqrt(var + LN_EPS) * g.astype(jnp.float32) + b.astype(jnp.float32)).astype(x.dtype)


def rms_norm(x, g):
    xf = x.astype(jnp.float32)
    ms = jnp.mean(xf * xf, axis=-1, keepdims=True)
    return (xf * lax.rsqrt(ms + RMS_EPS) * g.astype(jnp.float32)).astype(x.dtype)


def rope_tables(positions):
    inv_freq = jnp.power(ROPE_THETA, -jnp.arange(0, QK_ROPE, 2, dtype=jnp.float32) / QK_ROPE)
    ang = positions.astype(jnp.float32)[..., None] * inv_freq
    return jnp.cos(ang), jnp.sin(ang)


def apply_rope(x, cos, sin):
    cos = cos.astype(x.dtype)
    sin = sin.astype(x.dtype)
    x1, x2 = jnp.split(x, 2, axis=-1)
    return jnp.concatenate([x1 * cos - x2 * sin, x2 * cos + x1 * sin], axis=-1)


def pool_mixer(u, w_grp, scale):
    B, S, C = u.shape
    uf = u.astype(jnp.float32)
    cs = jnp.cumsum(uf, axis=1)
    t = jnp.arange(S)
    means = []
    for g, w in enumerate(POOL_WINDOWS):
        csg = cs[..., g * POOL_GW:(g + 1) * POOL_GW]
        lag = jnp.pad(csg[:, :S - w], ((0, 0), (w, 0), (0, 0)))
        cnt = jnp.minimum(t + 1, w).astype(jnp.float32)[None, :, None]
        means.append((csg - lag) / cnt)
    mixed = (jnp.concatenate(means, axis=-1) - uf).astype(u.dtype)
    mixed = jnp.einsum('bsgc,gcd->bsgd', mixed.reshape(B, S, POOL_GROUPS, POOL_GW), w_grp)
    return mixed.reshape(B, S, C) * scale


def conv_module(u2, dw, db, g, b):
    a, gate = jnp.split(u2, 2, axis=-1)
    z = a * jax.nn.sigmoid(gate)
    z = lax.conv_general_dilated(z, dw[:, None, :], window_strides=(1,),
                                 padding=[(CONV_WIDTH - 1, 0)],
                                 dimension_numbers=('NWC', 'WIO', 'NWC'),
                                 feature_group_count=D_CONV) + db
    return jax.nn.silu(layer_norm(z, g, b))


def chunk_causal_attention(q, k, v):
    B, S, H, Dk = q.shape
    nq = S // Q_BLOCK
    scale = Dk ** -0.5
    qb = q.reshape(B, nq, Q_BLOCK, H, Dk).transpose(1, 0, 2, 3, 4)
    key_chunk = jnp.arange(S) // CHUNK

    def block(args):
        qi, i = args
        s = jnp.einsum('bqhd,bkhd->bhqk', qi, k, preferred_element_type=jnp.float32) * scale
        q_chunk = (i * Q_BLOCK + jnp.arange(Q_BLOCK)) // CHUNK
        mask = key_chunk[None, :] <= q_chunk[:, None]
        pr = jax.nn.softmax(jnp.where(mask, s, -jnp.inf), axis=-1).astype(v.dtype)
        return jnp.einsum('bhqk,bkhd->bqhd', pr, v)

    o = lax.map(block, (qb, jnp.arange(nq)))
    return o.transpose(1, 0, 2, 3, 4).reshape(B, S, H, v.shape[-1])


def mla(c_q, c_kv, k_r, cos, sin, q_norm_g, w_uq, kv_norm_g, w_ukv):
    B, S, _ = c_q.shape
    q = jnp.matmul(rms_norm(c_q, q_norm_g), w_uq).reshape(B, S, N_HEADS, QK_NOPE + QK_ROPE)
    q_nope, q_rope = q[..., :QK_NOPE], q[..., QK_NOPE:]
    q_rope = apply_rope(q_rope, cos[:, :, None, :], sin[:, :, None, :])
    kv = jnp.matmul(rms_norm(c_kv, kv_norm_g), w_ukv).reshape(B, S, N_HEADS, QK_NOPE + V_HEAD)
    k_nope, v = kv[..., :QK_NOPE], kv[..., QK_NOPE:]
    k_rope = apply_rope(k_r, cos, sin)
    qf = jnp.concatenate([q_nope, q_rope], axis=-1)
    kf = jnp.concatenate([k_nope, jnp.broadcast_to(k_rope[:, :, None, :], (B, S, N_HEADS, QK_ROPE))], axis=-1)
    o = chunk_causal_attention(qf, kf, v)
    return o.reshape(B, S, N_HEADS * V_HEAD)


def mixer_sublayer(h, cos, sin, w_in, b_gate, pool_w, pool_scale, pool_proj, conv_dw, conv_b,
                   conv_ln_g, conv_ln_b, conv_proj, q_norm_g, w_uq, kv_norm_g, w_ukv, mla_proj, w_out):
    B, S, D = h.shape
    proj = jnp.matmul(h, w_in)
    u_pool, u_conv, c_q, c_kv, k_r, g_logits = jnp.split(proj, IN_SPLITS, axis=-1)
    gates = jax.nn.sigmoid(g_logits.reshape(B, S, N_BRANCH, D) + b_gate)
    y_pool = jnp.matmul(pool_mixer(u_pool, pool_w, pool_scale), pool_proj)
    y_conv = jnp.matmul(conv_module(u_conv, conv_dw, conv_b, conv_ln_g, conv_ln_b), conv_proj)
    y_mla = jnp.matmul(mla(c_q, c_kv, k_r, cos, sin, q_norm_g, w_uq, kv_norm_g, w_ukv), mla_proj)
    merged = gates[:, :, 0] * y_pool + gates[:, :, 1] * y_conv + gates[:, :, 2] * y_mla
    return jnp.matmul(merged, w_out)


def moe_ffn(h, w_router, router_bias, w_up, w_down):
    B, S, D = h.shape
    T = B * S
    A = T * TOP_K
    xt = h.reshape(T, D)
    aff = jax.nn.sigmoid(jnp.matmul(xt, w_router).astype(jnp.float32))
    sel = aff + router_bias.astype(jnp.float32)
    grp_score = jnp.sum(lax.top_k(sel.reshape(T, N_GROUPS, EXPERTS_PER_GROUP), TOP_K)[0], axis=-1)
    _, g_idx = lax.top_k(grp_score, TOPK_GROUPS)
    g_keep = jnp.any(jnp.arange(N_GROUPS)[None, None, :] == g_idx[:, :, None], axis=1)
    e_keep = jnp.repeat(g_keep, EXPERTS_PER_GROUP, axis=-1)
    _, e_idx = lax.top_k(jnp.where(e_keep, sel, -jnp.inf), TOP_K)
    w = jnp.take_along_axis(aff, e_idx, axis=-1)
    w = w / jnp.sum(w, axis=-1, keepdims=True)
    n_slots = ((A + N_EXPERTS * MOE_BLOCK + MOE_BLOCK - 1) // MOE_BLOCK) * MOE_BLOCK
    n_blocks = n_slots // MOE_BLOCK
    flat_e = e_idx.reshape(A)
    order = jnp.argsort(flat_e)
    s_e = flat_e[order]
    s_tok = (order // TOP_K).astype(jnp.int32)
    s_w = w.reshape(A)[order]
    counts = jnp.bincount(flat_e, length=N_EXPERTS)
    start = jnp.cumsum(counts) - counts
    padded = (counts + MOE_BLOCK - 1) // MOE_BLOCK * MOE_BLOCK
    pad_end = jnp.cumsum(padded)
    pad_start = pad_end - padded
    dest = pad_start[s_e] + jnp.arange(A) - start[s_e]
    slot_tok = jnp.full((n_slots,), T, jnp.int32).at[dest].set(s_tok)
    slot_w = jnp.zeros((n_slots,), jnp.float32).at[dest].set(s_w)
    blk_e = jnp.minimum(jnp.searchsorted(pad_end, jnp.arange(n_blocks) * MOE_BLOCK, side='right'),
                        N_EXPERTS - 1)
    xs = jnp.concatenate([xt, jnp.zeros((1, D), xt.dtype)], axis=0)[slot_tok]
    xs = xs.reshape(n_blocks, MOE_BLOCK, D)

    def expert_block(args):
        xb, e = args
        gate, up = jnp.split(jnp.matmul(xb, w_up[e]), 2, axis=-1)
        return jnp.matmul(jax.nn.silu(gate) * up, w_down[e])

    ys = lax.map(expert_block, (xs, blk_e)).reshape(n_slots, D)
    out = jnp.zeros((T + 1, D), h.dtype).at[slot_tok].add(ys * slot_w[:, None].astype(ys.dtype))
    return out[:T].reshape(B, S, D)


def setup_inputs(seed: int = 0) -> dict:
    key = jax.random.key(seed)
    ks = iter(jax.random.split(key, 40))
    f32 = jnp.float32
    L, D = DEPTH, D_MODEL

    def dense(shape, fan_in, gain=1.0):
        return jax.random.normal(next(ks), shape, f32) * (gain * fan_in ** -0.5)

    def norm_gain(shape):
        return 1.0 + 0.05 * jax.random.normal(next(ks), shape, f32)

    def small(shape, s=0.02):
        return s * jax.random.normal(next(ks), shape, f32)

    x = jax.random.normal(next(ks), (BATCH, SEQ, D), f32)
    p = jax.random.normal(next(ks), (L, BATCH, SEQ, D_PLE), f32)
    offset = jax.random.randint(next(ks), (BATCH, 1), 0, MAX_STREAM_CHUNKS) * CHUNK
    positions = (offset + jnp.arange(SEQ, dtype=jnp.int32)[None, :]).astype(jnp.int32)
    return {
        'x': x,
        'p': p,
        'positions': positions,
        'ln_in_g': norm_gain((D,)),
        'ln_in_b': small((D,)),
        'w_in': dense((L, D, D_IN), D),
        'b_gate': small((L, N_BRANCH, D), 0.1),
        'pool_w': dense((L, POOL_GROUPS, POOL_GW, POOL_GW), POOL_GW),
        'pool_scale': norm_gain((L, D_POOL)),
        'pool_proj': dense((L, D_POOL, D), D_POOL),
        'conv_dw': dense((L, CONV_WIDTH, D_CONV), CONV_WIDTH),
        'conv_b': small((L, D_CONV)),
        'conv_ln_g': norm_gain((L, D_CONV)),
        'conv_ln_b': small((L, D_CONV)),
        'conv_proj': dense((L, D_CONV, D), D_CONV),
        'q_norm_g': norm_gain((L, Q_LORA)),
        'w_uq': dense((L, Q_LORA, N_HEADS * (QK_NOPE + QK_ROPE)), Q_LORA),
        'kv_norm_g': norm_gain((L, KV_LORA)),
        'w_ukv': dense((L, KV_LORA, N_HEADS * (QK_NOPE + V_HEAD)), KV_LORA),
        'mla_proj': dense((L, N_HEADS * V_HEAD, D), N_HEADS * V_HEAD),
        'w_out': dense((L, D, D), D, DEEPNORM_BETA),
        'ln1_g': norm_gain((L, D)),
        'ln1_b': small((L, D)),
        'w_router': dense((D, N_EXPERTS), D),
        'router_bias': small((N_EXPERTS,), 0.01),
        'exp_w_up': dense((L, N_EXPERTS, D, 2 * D_EXPERT), D),
        'exp_w_down': dense((L, N_EXPERTS, D_EXPERT, D), D_EXPERT, DEEPNORM_BETA),
        'ple_proj': dense((L, D_PLE, D), D_PLE, DEEPNORM_BETA),
        'ple_gate': dense((L, D, D), D),
        'ln2_g': norm_gain((L, D)),
        'ln2_b': small((L, D)),
    }


def reference(x, p, positions, ln_in_g, ln_in_b, w_in, b_gate, pool_w, pool_scale, pool_proj,
              conv_dw, conv_b, conv_ln_g, conv_ln_b, conv_proj, q_norm_g, w_uq, kv_norm_g, w_ukv,
              mla_proj, w_out, ln1_g, ln1_b, w_router, router_bias, exp_w_up, exp_w_down,
              ple_proj, ple_gate, ln2_g, ln2_b):
    cos, sin = rope_tables(positions)
    h = layer_norm(x, ln_in_g, ln_in_b)
    for i in range(DEPTH):
        y = mixer_sublayer(h, cos, sin, w_in[i], b_gate[i], pool_w[i], pool_scale[i], pool_proj[i],
                           conv_dw[i], conv_b[i], conv_ln_g[i], conv_ln_b[i], conv_proj[i],
                           q_norm_g[i], w_uq[i], kv_norm_g[i], w_ukv[i], mla_proj[i], w_out[i])
        h = layer_norm(DEEPNORM_ALPHA * h + y, ln1_g[i], ln1_b[i])
        e = jnp.matmul(p[i], ple_proj[i]) * jax.nn.sigmoid(jnp.matmul(h, ple_gate[i]))
        m = moe_ffn(h, w_router, router_bias, exp_w_up[i], exp_w_down[i])
        h = layer_norm(DEEPNORM_ALPHA * h + m + e, ln2_g[i], ln2_b[i])
    return h
```

```python
import os
import numpy as np
import concourse.bass as bass
import concourse.mybir as mybir
from concourse.bass_utils import run_bass_kernel_spmd
from contextlib import ExitStack

F32 = mybir.dt.float32
BF16 = mybir.dt.bfloat16
I32 = mybir.dt.int32
AF = mybir.ActivationFunctionType
ALU = mybir.AluOpType
AX = mybir.AxisListType

ENGS = ('pe', 'act', 'dve', 'pool', 'sp')


class Buf:
    def __init__(self, name, ap=None, semkey=None):
        self.name = name
        self.ap = ap
        self.last_write = None
        self.readers = []
        self.semkey = semkey if semkey is not None else ('b', name)


class Prog:
    def __init__(self, nc):
        self.nc = nc
        self.stack = ExitStack()
        self.ops = {e: [] for e in ENGS}
        self.cnt = {}
        self.seen = {e: {} for e in ENGS}
        self.nops = 0

    def sb(self, name, shape, dtype):
        t = self.stack.enter_context(self.nc.sbuf_tensor("sb_" + name, list(shape), dtype))
        return Buf(name, t)

    def ps(self, name, shape, dtype=F32):
        t = self.stack.enter_context(self.nc.psum_tensor("ps_" + name, list(shape), dtype))
        return Buf(name, t)

    def dram(self, name, ap=None, semkey=None):
        return Buf(name, ap, semkey)

    def view(self, name, ap, semkey=None):
        return Buf(name, ap, semkey)

    def _waits(self, eng, reads, writes):
        need = {}

        def add(tok, war):
            if tok is None:
                return
            k, v = tok
            if k[0] == 'b':
                v = self.cnt.get(k, 0)
            elif k == ('e', eng):
                if eng == 'pe' or war:
                    return
            if need.get(k, 0) < v:
                need[k] = v

        for b in reads:
            add(b.last_write, False)
        for b in writes:
            add(b.last_write, False)
            for t in b.readers:
                add(t, True)
        waits = []
        for k, v in need.items():
            if self.seen[eng].get(k, 0) < v:
                self.seen[eng][k] = v
                waits.append((k, v))
        return waits

    def _commit(self, tok, reads, writes):
        for b in writes:
            b.last_write = tok
            b.readers = []
        for b in reads:
            if b not in writes:
                b.readers.append(tok)

    def op(self, eng, fn, reads=(), writes=()):
        waits = self._waits(eng, reads, writes)
        k = ('e', eng)
        self.cnt[k] = self.cnt.get(k, 0) + 1
        tok = (k, self.cnt[k])
        self.ops[eng].append((waits, fn, k, 1))
        self._commit(tok, reads, writes)
        self.nops += 1

    def dma(self, q, dst, out_ap, in_ap, reads=(), extra_writes=()):
        writes = [dst] + list(extra_writes)
        waits = self._waits(q, reads, writes)
        k = dst.semkey
        self.cnt[k] = self.cnt.get(k, 0) + 16
        tok = (k, self.cnt[k])
        self.ops[q].append((waits, lambda e: e.dma_start(out=out_ap, in_=in_ap), k, 16))
        self._commit(tok, reads, writes)
        self.nops += 1

    def dma_fn(self, q, dst, fn, reads=(), extra_writes=()):
        writes = [dst] + list(extra_writes)
        waits = self._waits(q, reads, writes)
        k = dst.semkey
        self.cnt[k] = self.cnt.get(k, 0) + 16
        tok = (k, self.cnt[k])
        self.ops[q].append((waits, fn, k, 16))
        self._commit(tok, reads, writes)
        self.nops += 1

    def finish(self, outs):
        waits = self._waits('sp', outs, [])
        self.ops['sp'].append((waits, None, None, 0))
        nc = self.nc
        sems = {}
        for k in self.cnt:
            sems[k] = self.stack.enter_context(nc.semaphore("s_%s_%s" % k))
        ops = self.ops
        with nc.Block() as block:
            def mk(engname):
                def body(e):
                    for waits, fn, k, inc in ops[engname]:
                        for (wk, wv) in waits:
                            e.wait_ge(sems[wk], wv)
                        if fn is not None:
                            fn(e).then_inc(sems[k], inc)
                return body
            block.tensor(mk('pe'))
            block.scalar(mk('act'))
            block.vector(mk('dve'))
            block.gpsimd(mk('pool'))
            block.sync(mk('sp'))
        self.stack.close()


S = 2048
D = 1024
NT = 16
ALPHA = 8.0 ** 0.25
BIG = 100.0
CAP = 512
NSLOT = 16 * CAP
BIGV = 1.0e6
MERGE_ENG = os.environ.get('MERGE_ENG', 'dve')
LN_ENG = os.environ.get('LN_ENG', 'dve')
PREP_ENG = os.environ.get('PREP_ENG', 'dve')
LNQ = os.environ.get('LNQ', 'act')


def build_nc(n_layers=4, tap=None):
    nc = bass.Bass("TRN2", target_bir_lowering=False)

    def din(name, shape, dtype=F32):
        return nc.dram_tensor(name, list(shape), dtype, kind="ExternalInput").ap()

    x_d = din("x", [S, D]); p_d = din("p", [4, S, 256]); pos_d = din("pos", [16, 128], I32)
    invf_d = din("invf", [128, 16]); rcnt_d = din("rcnt", [128, 16]); ecap_d = din("ecap", [128, 16])
    ln_in_g = din("ln_in_g", [1, D]); ln_in_b = din("ln_in_b", [1, D])
    w_in = din("w_in", [4, D, 5280]); b_gate = din("b_gate", [4, 24, 128])
    pool_w = din("pool_w", [4, 4, 128, 128]); pool_scale = din("pool_scale", [4, 4, 128])
    pool_proj = din("pool_proj", [4, 512, D]); conv_dw = din("conv_dw", [4, 124, 128])
    conv_b = din("conv_b", [4, 4, 128]); conv_ln_g = din("conv_ln_g", [4, 4, 128]); conv_ln_b = din("conv_ln_b", [4, 4, 128])
    conv_proj = din("conv_proj", [4, 512, D]); q_norm_g = din("q_norm_g", [4, 3, 128]); w_uq = din("w_uq", [4, 384, 768])
    kv_norm_g = din("kv_norm_g", [4, 2, 128]); w_ukv = din("w_ukv", [4, 256, 1024]); mla_proj = din("mla_proj", [4, 512, D])
    w_out = din("w_out", [4, D, D]); ln1_g = din("ln1_g", [4, D]); ln1_b = din("ln1_b", [4, D])
    w_router = din("w_router", [D, 16]); router_bias = din("router_bias", [1, 16])
    exp_w_up = din("exp_w_up", [4, 16, D, 1024]); exp_w_down = din("exp_w_down", [4, 16, 512, D])
    ple_proj = din("ple_proj", [4, 256, D]); ple_gate = din("ple_gate", [4, D, D])
    ln2_g = din("ln2_g", [4, D]); ln2_b = din("ln2_b", [4, D])
    out_d = nc.dram_tensor("out", [S, D], F32, kind="ExternalOutput").ap()
    h32_d = nc.dram_tensor("h32_d", [S, D], F32, kind="Internal").ap()
    xs_d = nc.dram_tensor("xs_d", [NSLOT + 1, D], BF16, kind="Internal").ap()
    hbf_d = nc.dram_tensor("hbf_d", [S, D], BF16, kind="Internal").ap()
    ys_d = nc.dram_tensor("ys_d", [NSLOT + 1, D], F32, kind="Internal").ap()
    dbg_d = nc.dram_tensor("dbg", [3, 128, 8 * S], F32, kind="ExternalOutput").ap() if tap else None
    dbg_buf = Buf("dbg", semkey=('b', 'dbg'))

    P = Prog(nc)
    hT_t = P.sb("hT", [128, 8 * S], BF16).ap
    hT3 = hT_t[:].rearrange("p (k n) -> p k n", k=8)
    hTb = [Buf("hT%d" % t) for t in range(NT)]
    M_t = P.sb("M", [128, 8 * S], BF16).ap
    M3 = M_t[:].rearrange("p (k n) -> p k n", k=8)
    Mb = [Buf("M%d" % i, semkey=('b', 'wB')) for i in range(4)]
    R_t = P.sb("R", [128, 16 * 1024], F32).ap
    Rb = [Buf("R%d" % i) for i in range(16)]
    W_t = P.sb("W", [128, 4 * 4096], BF16).ap
    Wb = [Buf("W%d" % i, semkey=('b', 'w%d' % i)) for i in range(4)]
    S_t = P.sb("Sx", [128, 8 * 512], F32).ap
    Sb = [Buf("S%d" % i) for i in range(8)]
    Zs = [P.sb("Z0", [128, 1024], F32).ap, P.sb("Z1", [128, 1024], F32).ap]
    Zb = [Buf("Z%d" % i, semkey=('b', 'z%d' % i)) for i in range(2)]
    HB_t = P.sb("HB", [128, 2 * 1024], BF16).ap
    HBb = [Buf("HB%d" % i) for i in range(2)]
    identF = P.sb("identF", [128, 128], F32); identB = P.sb("identB", [128, 128], BF16)
    onesF = P.sb("onesF", [128, 128], F32)
    epsb = P.sb("epsb", [128, 2], F32)
    cosT = P.sb("cosT", [128, 256], F32); sinT = P.sb("sinT", [128, 256], F32)
    wr_t = P.sb("wr", [128, 8 * 16], BF16); rb_t = P.sb("rb", [128, 16], F32)
    rcnt = P.sb("rcnt", [128, 16], F32)
    colsrcA = P.sb("colsrcA", [48, 128], F32); colsrcB = P.sb("colsrcB", [124, 128], F32)
    colsrcA.semkey = colsrcB.semkey = ('b', 'cols')
    colA = P.sb("colA", [128, 48], F32); colB = P.sb("colB", [128, 124], F32)
    poolw_t = P.sb("poolw", [128, 4 * 128], BF16)
    st_t = P.sb("st", [128, 4 * 12], F32); mv_t = P.sb("mv", [128, 4 * 2], F32); sd_t = P.sb("sd", [128, 4 * 2], F32)
    stb = [Buf("st%d" % i) for i in range(4)]
    small = P.sb("small", [128, 64], F32)
    actT_t = P.sb("actT", [128, 2 * 2048], BF16)
    actTb = [Buf("actT%d" % i) for i in range(2)]
    ZL = [(Zb[0], Zs[0][:, :]), (Zb[1], Zs[1][:, :]),
          (actTb[0], actT_t.ap[:, 0:2048].bitcast(F32)), (actTb[1], actT_t.ap[:, 2048:4096].bitcast(F32))]
    NZ = 4
    onesB = P.sb("onesB", [128, 128], BF16)
    upperB = P.sb("upperB", [128, 128], BF16)
    ecap_t = P.sb("ecap", [128, 16], F32)
    xs_bufs = [Buf("xs%d" % i, semkey=('b', 'xsd')) for i in range(NT)]
    hbf_bufs = [Buf("hbf%d" % i, semkey=('b', 'hbfd')) for i in range(NT)]
    ys_bufs = [Buf("ys%d" % i, semkey=('b', 'ysd')) for i in range(16)]
    YSb = [Buf("YS%d" % i, semkey=('b', 'ysl%d' % i)) for i in range(2)]
    psF = [P.ps("psF%d" % i, [128, 512], F32) for i in range(4)]
    psB = [P.ps("psB%d" % i, [128, 1024], BF16) for i in range(2)]
    ctr = {'f': 0, 'b': 0, 's': 0, 'z': 0}

    def nf():
        ctr['f'] += 1
        return psF[ctr['f'] % 4]

    def nb():
        ctr['b'] += 1
        return psB[ctr['b'] % 2]

    def ns():
        ctr['s'] += 1
        lo = ctr.get('smin', 0)
        i = lo + ctr['s'] % (8 - lo)
        return Sb[i], S_t[:, i * 512:(i + 1) * 512]

    def mm(ps, out_ap, terms, reads):
        def fn(e):
            n = len(terms)
            ins = None
            for i, (a, b) in enumerate(terms):
                ins = e.matmul(out_ap, a, b, start=(i == 0), stop=(i == n - 1))
            return ins
        P.op('pe', fn, reads=reads, writes=[ps])

    def tr(ps, out_ap, in_ap, ident_ap, reads, start=True):
        P.op('pe', lambda e: e.transpose(out_ap, in_ap, ident_ap), reads=reads, writes=[ps])

    def act(out, in_, func, reads, writes, bias=None, scale=1.0):
        if bias is None:
            P.op('act', lambda e: e.activation(out, in_, func, scale=scale), reads, writes)
        else:
            P.op('act', lambda e: e.activation(out, in_, func, bias=bias, scale=scale), reads, writes)

    def tt(eng, out, a, b, op, reads, writes):
        P.op(eng, lambda e: e.tensor_tensor(out, a, b, op), reads, writes)

    def ts(eng, out, a, s1, s2, op0, op1, reads, writes):
        if op1 is None:
            P.op(eng, lambda e: e.tensor_scalar(out, a, s1, None, op0), reads, writes)
        else:
            P.op(eng, lambda e: e.tensor_scalar(out, a, s1, s2, op0, op1), reads, writes)

    def stt(eng, out, a, s, b, op0, op1, reads, writes):
        P.op(eng, lambda e: e.scalar_tensor_tensor(out, a, s, b, op0, op1), reads, writes)

    def cp(eng, out, in_, reads, writes):
        if eng == 'act':
            P.op('act', lambda e: e.copy(out, in_), reads, writes)
        else:
            P.op(eng, lambda e: e.tensor_copy(out, in_), reads, writes)

    def red(out, in_, op, reads, writes):
        P.op('dve', lambda e: e.tensor_reduce(out, in_, AX.X, op), reads, writes)

    def wload(bufs, dst_ap, src_ap, q='pool'):
        P.dma(q, bufs[0], dst_ap, src_ap, extra_writes=bufs[1:])

    def Wv(slot, n=1):
        return W_t[:, slot * 4096:(slot + n) * 4096]

    def Rv(i0, n, dtype=F32):
        v = R_t[:, i0 * 1024:(i0 + n) * 1024]
        return v.bitcast(BF16) if dtype == BF16 else v

    taps = {}

    P.op('pool', lambda e: e.memset(identF.ap[:], 1.0), writes=[identF])
    P.op('pool', lambda e: e.affine_select(identF.ap[:], identF.ap[:], pattern=[[-1, 128]], compare_op=ALU.is_equal,
                                           fill=0.0, base=0, channel_multiplier=1), reads=[identF], writes=[identF])
    cp('dve', identB.ap[:], identF.ap[:], [identF], [identB])
    P.op('pool', lambda e: e.memset(onesF.ap[:], 1.0), writes=[onesF])
    P.op('pool', lambda e: e.memset(epsb.ap[:, 0:1], 1e-5), writes=[epsb])
    P.op('pool', lambda e: e.memset(epsb.ap[:, 1:2], 1e-6), reads=[], writes=[epsb])
    P.dma('sp', rcnt, rcnt.ap[:], rcnt_d[:, :])
    P.dma('sp', ecap_t, ecap_t.ap[:], ecap_d[:, :])
    P.op('pool', lambda e: e.memset(onesB.ap[:], 1.0), writes=[onesB])
    P.op('pool', lambda e: e.memset(upperB.ap[:], 1.0), writes=[upperB])
    P.op('pool', lambda e: e.affine_select(upperB.ap[:], upperB.ap[:], pattern=[[1, 128]], compare_op=ALU.is_gt,
                                           fill=0.0, base=0, channel_multiplier=-1), reads=[upperB], writes=[upperB])
    P.op('pool', lambda e: e.memset(Zs[0][0:1, :], 0.0), writes=[Zb[0]])
    P.dma('sp', ys_bufs[0], ys_d[NSLOT:NSLOT + 1, :], Zs[0][0:1, :], reads=[Zb[0]])
    P.dma('sp', rb_t, rb_t.ap[:], router_bias[0:1, :].broadcast_to([128, 16]))
    P.dma('pool', wr_t, wr_t.ap[:].rearrange("p (k e) -> p k e", k=8), w_router.rearrange("(k p) e -> p k e", p=128))
    pi_b, pi_ap = ns()
    P.dma('sp', pi_b, pi_ap[0:16, 0:128].bitcast(I32), pos_d[:, :])
    pf_b, pf_ap = ns()
    cp('dve', pf_ap[0:16, 0:128], pi_ap[0:16, 0:128].bitcast(I32), [pi_b], [pf_b])
    pp = nf()
    tr(pp, pp.ap[:, 0:16], pf_ap[0:16, 0:128], identF.ap[0:16, 0:16], [pf_b, identF])
    post_b, post_ap = ns()
    cp('dve', post_ap[:, 0:16], pp.ap[:, 0:16], [pp], [post_b])
    invf_b, invf_ap = ns()
    P.dma('sp', invf_b, invf_ap[:, 0:16], invf_d[:, :])
    ang_b, ang_ap = ns()
    ang3 = ang_ap[:, 0:256].rearrange("p (t i) -> p t i", t=16)
    tt('dve', ang3, post_ap[:, 0:16, None].broadcast_to([128, 16, 16]), invf_ap[:, None, 0:16].broadcast_to([128, 16, 16]),
       ALU.mult, [post_b, invf_b], [ang_b])

    def sin_of(dst, shift):
        kf_b, kf_ap = ns()
        ki_b, ki_ap = ns()
        a_b, a_ap = ns()
        kf, ki, a = kf_ap[:, 0:256], ki_ap[:, 0:256].bitcast(I32), a_ap[:, 0:256]
        ts('dve', a, ang_ap[:, 0:256], shift, None, ALU.add, None, [ang_b], [a_b])
        ts('dve', kf, a, 1.0 / (2 * np.pi), None, ALU.mult, None, [a_b], [kf_b])
        cp('dve', ki, kf, [kf_b], [ki_b])
        cp('dve', kf, ki, [ki_b], [kf_b])
        stt('dve', a, kf, -2 * np.pi, a, ALU.mult, ALU.add, [a_b, kf_b], [a_b])
        ts('dve', kf, a, np.pi, -2 * np.pi, ALU.is_gt, ALU.mult, [a_b], [kf_b])
        tt('dve', a, a, kf, ALU.add, [a_b, kf_b], [a_b])
        ts('dve', kf, a, -np.pi, 2 * np.pi, ALU.is_lt, ALU.mult, [a_b], [kf_b])
        tt('dve', a, a, kf, ALU.add, [a_b, kf_b], [a_b])
        act(dst.ap[:], a, AF.Sin, [a_b], [dst])
    sin_of(sinT, 0.0)
    sin_of(cosT, np.pi / 2)
    cos3 = cosT.ap[:].rearrange("p (t i) -> p t i", t=16)
    sin3 = sinT.ap[:].rearrange("p (t i) -> p t i", t=16)

    def ln_tile(t, zi, g_ap, b_ap, gb_bufs, dest, part='ab'):
        zb, z = ZL[zi]
        st = st_t.ap[:, zi * 12:(zi + 1) * 12]
        mv = mv_t.ap[:, zi * 2:(zi + 1) * 2]
        sd = sd_t.ap[:, zi * 2:(zi + 1) * 2]
        sb_ = stb[zi]
        if 'a' in part:
            P.op('dve', lambda e: e.bn_stats(st[:, 0:6], z[:, 0:512]), [zb], [sb_])
            P.op('dve', lambda e: e.bn_stats(st[:, 6:12], z[:, 512:1024]), [zb, sb_], [sb_])
            P.op('dve', lambda e: e.bn_aggr(mv, st), [sb_], [sb_])
            act(sd[:, 0:1], mv[:, 1:2], AF.Sqrt, [sb_, epsb], [sb_], bias=epsb.ap[:, 0:1])
        if 'b' not in part:
            return
        P.op('dve', lambda e: e.reciprocal(sd[:, 1:2], sd[:, 0:1]), [sb_], [sb_])
        ts('dve', z, z, mv[:, 0:1], sd[:, 1:2], ALU.subtract, ALU.mult, [zb, sb_], [zb])
        tt(LN_ENG, z, z, g_ap, ALU.mult, [zb] + gb_bufs, [zb])
        tt(LN_ENG, z, z, b_ap, ALU.add, [zb] + gb_bufs, [zb])
        hbb, hb = HBb[zi % 2], HB_t[:, (zi % 2) * 1024:((zi % 2) + 1) * 1024]
        cp('act', hb, z, [zb], [hbb])
        pb = nb()
        for j in range(8):
            tr(pb, pb.ap[:, j * 128:(j + 1) * 128], hb[:, j * 128:(j + 1) * 128], identB.ap[:], [hbb, identB])
        cp('dve', hT3[:, :, t * 128:(t + 1) * 128], pb.ap[:].rearrange("p (k n) -> p k n", k=8), [pb], [hTb[t]])
        if dest == 'h32':
            P.dma('sp', h32_buf, h32_d[t * 128:(t + 1) * 128, :], z, reads=[zb])
        elif dest == 'out':
            P.dma('sp', out_buf, out_d[t * 128:(t + 1) * 128, :], z, reads=[zb])
        else:
            P.op('act', lambda e: e.mul(R_t[:, t * 1024:(t + 1) * 1024], z, ALPHA), [zb], [Rb[t]])
            P.dma(LNQ, hbf_bufs[t], hbf_d[t * 128:(t + 1) * 128, :], hb, reads=[hbb])
            if dest == 'acc+out':
                P.dma('sp', out_buf, out_d[t * 128:(t + 1) * 128, :], z, reads=[zb])

    out_buf = Buf("outd", semkey=('b', 'outd'))
    h32_buf = Buf("h32d_all", semkey=('b', 'h32d'))

    def load_gb(g_src, b_src, slot):
        v = Wv(slot).bitcast(F32)
        P.dma('sp', Wb[slot], v[:, 0:1024], g_src.broadcast_to([128, 1024]))
        P.dma('sp', Wb[slot], v[:, 1024:2048], b_src.broadcast_to([128, 1024]))
        return v[:, 0:1024], v[:, 1024:2048], [Wb[slot]]

    g_ap, b_ap, gbb = load_gb(ln_in_g[0:1, :], ln_in_b[0:1, :], 3)
    d0 = 'h32' if n_layers > 0 else 'out'
    P.dma('sp', ZL[0][0], ZL[0][1], x_d[0:128, :])
    ln_tile(0, 0, g_ap, b_ap, gbb, d0, part='a')
    for t in range(NT):
        if t + 1 < NT:
            zi = (t + 1) % NZ
            P.dma('sp', ZL[zi][0], ZL[zi][1], x_d[(t + 1) * 128:(t + 2) * 128, :])
            ln_tile(t + 1, zi, g_ap, b_ap, gbb, d0, part='a')
        ln_tile(t, t % NZ, g_ap, b_ap, gbb, d0, part='b')


    wt_t = P.sb("wt", [128, 256], F32)
    psS = P.ps("psS", [128, 512], F32)
    psO = [psS, P.ps("psO1", [128, 512], F32)]
    kro_t = P.sb("kro", [128, 512], BF16)
    CB = [[Buf("CB%d_%d" % (c, r)) for r in range(4)] for c in range(4)]
    wsrc = lambda l: w_in[l].rearrange("(k p) n -> p k n", p=128)
    QS = 96.0 ** -0.5

    def merge_branch(l, b, srcT3, src_bufs, proj_d, first):
        Wg3 = Wv(0, 2).rearrange("p (k n) -> p k n", k=8)
        Wp3 = Wv(2).rearrange("p (c n) -> p c n", c=4)
        c0 = 2208 + b * 1024
        wload([Wb[0]], Wg3[:, 0:4, :], wsrc(l)[:, 0:4, c0:c0 + 1024])
        wload([Wb[1]], Wg3[:, 4:8, :], wsrc(l)[:, 4:8, c0:c0 + 1024])
        wload([Wb[2]], Wp3, proj_d.rearrange("(c p) n -> p c n", p=128))
        for T in range(4):
            tsl = slice(T * 512, (T + 1) * 512)
            for j in range(8):
                jsl = slice(j * 128, (j + 1) * 128)
                psy = nf()
                mm(psy, psy.ap[:, :], [(Wp3[:, c, jsl], srcT3[:, c, tsl]) for c in range(4)], [Wb[2]] + src_bufs)
                psg = nf()
                mm(psg, psg.ap[:, :], [(Wg3[:, k, jsl], hT3[:, k, tsl]) for k in range(8)], [Wb[0], Wb[1]] + hTb[4 * T:4 * T + 4])
                gb_, gs_ = ns()
                gs = gs_.bitcast(BF16)[:, 0:512]
                act(gs, psg.ap[:, :], AF.Sigmoid, [psg, colA], [gb_], bias=colA.ap[:, b * 8 + j:b * 8 + j + 1])
                if first:
                    tt('dve', M3[:, j, tsl], psy.ap[:, :], gs, ALU.mult, [psy, gb_], [Mb[T]])
                else:
                    tb_, tm_ = ns()
                    tmp = tm_.bitcast(BF16)[:, 0:512]
                    tt('dve', tmp, psy.ap[:, :], gs, ALU.mult, [psy, gb_], [tb_])
                    tt(MERGE_ENG, M3[:, j, tsl], M3[:, j, tsl], tmp, ALU.add, [Mb[T], tb_], [Mb[T]])

    def proj_T(l, T):
        W0 = Wv(0)[:, 0:8 * 384].rearrange("p (k n) -> p k n", k=8)
        W1 = Wv(1)[:, 0:8 * 288].rearrange("p (k n) -> p k n", k=8)
        cqT = Rv(0, 3, BF16).rearrange("p (k n) -> p k n", k=3)
        ckvT = Rv(3, 2, BF16).rearrange("p (k n) -> p k n", k=2)
        tsl = slice(T * 512, (T + 1) * 512)
        for (nj, Wx, dstT, rb0, col0) in ((3, W0, cqT, 0, 0), (2, W1, ckvT, 3, 16)):
            wbx = Wb[0] if nj == 3 else Wb[1]
            for j in range(nj):
                ps = nf()
                mm(ps, ps.ap[:, :], [(Wx[:, k, j * 128:(j + 1) * 128], hT3[:, k, tsl]) for k in range(8)], [wbx] + hTb[4 * T:4 * T + 4])
                cp('act', dstT[:, j, tsl], ps.ap[:, :], [ps], [Rb[rb0 + j]])
                sqb, sq = ns()
                act(sq, ps.ap[:, :], AF.Square, [ps], [sqb])

                def fn(e, sq=sq, T=T, j=j, nj=nj, col0=col0):
                    ins = None
                    for s_ in range(4):
                        c = col0 + 4 * T + s_
                        ins = e.matmul(psS.ap[:, c:c + 1], sq[:, s_ * 128:(s_ + 1) * 128], onesF.ap[:, 0:1],
                                       start=(c == 0 and j == 0), stop=(j == nj - 1), skip_group_check=True)
                    return ins
                P.op('pe', fn, [sqb, onesF], [psS])

    def layer_prologue(l):
        for (r0, n, src) in ((0, 24, b_gate[l]), (24, 4, pool_scale[l]), (28, 4, conv_b[l]), (32, 4, conv_ln_g[l]),
                             (36, 4, conv_ln_b[l]), (40, 3, q_norm_g[l]), (43, 2, kv_norm_g[l])):
            P.dma('sp', colsrcA, colsrcA.ap[r0:r0 + n, :], src)
        P.dma('sp', colsrcB, colsrcB.ap[:, :], conv_dw[l])
        pa = nf()
        tr(pa, pa.ap[:, 0:45], colsrcA.ap[0:45, :], identF.ap[0:45, 0:45], [colsrcA, identF])
        cp('dve', colA.ap[:, 0:45], pa.ap[:, 0:45], [pa], [colA])
        pa = nf()
        tr(pa, pa.ap[:, 0:124], colsrcB.ap[0:124, :], identF.ap[0:124, 0:124], [colsrcB, identF])
        cp('dve', colB.ap[:, 0:124], pa.ap[:, 0:124], [pa], [colB])

        W0 = Wv(0)[:, 0:8 * 384].rearrange("p (k n) -> p k n", k=8)
        W1 = Wv(1)[:, 0:8 * 288].rearrange("p (k n) -> p k n", k=8)
        Wq3 = Wv(2)[:, 0:3 * 768].rearrange("p (k n) -> p k n", k=3)
        wload([Wb[0]], W0, wsrc(l)[:, :, 1536:1920])
        wload([Wb[1]], W1, wsrc(l)[:, :, 1920:2208])
        wload([Wb[2]], Wq3, w_uq[l].rearrange("(k p) n -> p k n", p=128))
        for j in range(3):
            ts('dve', Wq3[:, j, :], Wq3[:, j, :], colA.ap[:, 40 + j:41 + j], None, ALU.mult, None, [Wb[2], colA], [Wb[2]])

    if n_layers > 0:
        layer_prologue(0)
    for l in range(n_layers):
        last = (l == n_layers - 1)
        W0 = Wv(0)[:, 0:8 * 384].rearrange("p (k n) -> p k n", k=8)
        W1 = Wv(1)[:, 0:8 * 288].rearrange("p (k n) -> p k n", k=8)
        Wq3 = Wv(2)[:, 0:3 * 768].rearrange("p (k n) -> p k n", k=3)
        Wkv3 = Wv(3)[:, 0:2 * 1024].rearrange("p (k n) -> p k n", k=2)
        wload([Wb[3]], Wkv3, w_ukv[l].rearrange("(k p) n -> p k n", p=128))
        for j in range(2):
            ts('dve', Wkv3[:, j, :], Wkv3[:, j, :], colA.ap[:, 43 + j:44 + j], None, ALU.mult, None, [Wb[3], colA], [Wb[3]])
        cqT = Rv(0, 3, BF16).rearrange("p (k n) -> p k n", k=3)
        ckvT = Rv(3, 2, BF16).rearrange("p (k n) -> p k n", k=2)
        if l == 0:
            for T in range(4):
                proj_T(l, T)
        rq = small.ap[:, 0:16]
        rkv = small.ap[:, 16:32]
        act(small.ap[:, 32:48], psS.ap[:, 0:16], AF.Sqrt, [psS, epsb], [small], bias=epsb.ap[:, 1:2], scale=1.0 / 384)
        act(small.ap[:, 48:64], psS.ap[:, 16:32], AF.Sqrt, [psS, epsb], [small], bias=epsb.ap[:, 1:2], scale=1.0 / 256)
        P.op('dve', lambda e: e.reciprocal(small.ap[:, 0:32], small.ap[:, 32:64]), [small], [small])
        ts('dve', rq, rq, QS, None, ALU.mult, None, [small], [small])
        psK = nf()
        for t in range(NT):
            mm(psK, psK.ap[:, t * 32:(t + 1) * 32], [(hT3[:, k, t * 128:(t + 1) * 128], W1[:, k, 256:288]) for k in range(8)], [Wb[1], hTb[t]])
        krb, kr_ = ns()
        cp('act', kr_, psK.ap[:, :], [psK], [krb])
        kr3 = kr_.rearrange("p (t d) -> p t d", t=16)
        kob = kro_t
        ko3 = kro_t.ap[:, :].rearrange("p (t d) -> p t d", t=16)
        tb1, t1_ = ns()
        tb2, t2_ = ns()
        t1 = t1_[:, 0:256].rearrange("p (t d) -> p t d", t=16)
        t2 = t2_[:, 0:256].rearrange("p (t d) -> p t d", t=16)
        tt('dve', t1, kr3[:, :, 0:16], cos3, ALU.mult, [krb, cosT], [tb1])
        tt('dve', t2, kr3[:, :, 16:32], sin3, ALU.mult, [krb, sinT], [tb2])
        tt('dve', ko3[:, :, 0:16], t1, t2, ALU.subtract, [tb1, tb2], [kob])
        tt('dve', t1, kr3[:, :, 16:32], cos3, ALU.mult, [krb, cosT, kob], [tb1])
        tt('dve', t2, kr3[:, :, 0:16], sin3, ALU.mult, [krb, sinT, kob], [tb2])
        tt('dve', ko3[:, :, 16:32], t1, t2, ALU.add, [tb1, tb2], [kob])

        qT3 = Rv(5, 4, BF16).rearrange("p (h n) -> p h n", h=4)
        kT3 = Rv(9, 4, BF16).rearrange("p (h n) -> p h n", h=4)
        v4 = Rv(13, 3, BF16)[:, 0:16 * 260].rearrange("p (t h d) -> p t h d", t=16, h=4)
        o_tm = M_t[:, 0:16 * 512].rearrange("p (t c) -> p t c", t=16)
        for g in range(2):
            P.op('pool', lambda e: e.memset(v4[:, :, :, 64:65], 1.0), [], Rb[13:16])
            def prep_front(t):
                tsl = slice(t * 128, (t + 1) * 128)
                psq = nf()
                mm(psq, psq.ap[:, 0:384], [(cqT[:, j, tsl], Wq3[:, j, g * 384:(g + 1) * 384]) for j in range(3)], [Wb[2]] + Rb[0:3])
                q32b, q32_ = ns()
                ts('dve', q32_[:, 0:384], psq.ap[:, 0:384], rq[:, t:t + 1], None, ALU.mult, None, [psq, small], [q32b])
                q3 = q32_[:, 0:384].rearrange("p (h d) -> p h d", h=4)
                qbb, qb_ = ns()
                qb3 = qb_.bitcast(BF16)[:, 0:384].rearrange("p (h d) -> p h d", h=4)
                cp(PREP_ENG, qb3[:, :, 0:64], q3[:, :, 0:64], [q32b], [qbb])
                cs = cos3[:, t, None, :].broadcast_to([128, 4, 16])
                sn = sin3[:, t, None, :].broadcast_to([128, 4, 16])
                ab, a_ = ns()
                a1 = a_[:, 0:64].rearrange("p (h d) -> p h d", h=4)
                a2 = a_[:, 64:128].rearrange("p (h d) -> p h d", h=4)
                a3 = a_[:, 128:192].rearrange("p (h d) -> p h d", h=4)
                a4 = a_[:, 192:256].rearrange("p (h d) -> p h d", h=4)
                tt('dve', a1, q3[:, :, 64:80], cs, ALU.mult, [q32b, cosT], [ab])
                tt(PREP_ENG, a2, q3[:, :, 80:96], sn, ALU.mult, [q32b, sinT], [ab])
                tt('dve', a3, q3[:, :, 80:96], cs, ALU.mult, [q32b, cosT], [ab])
                tt(PREP_ENG, a4, q3[:, :, 64:80], sn, ALU.mult, [q32b, sinT], [ab])
                tt('dve', qb3[:, :, 64:80], a1, a2, ALU.subtract, [ab], [qbb])
                tt('dve', qb3[:, :, 80:96], a3, a4, ALU.add, [ab], [qbb])
                pskv = nf()
                mm(pskv, pskv.ap[:, :], [(ckvT[:, j, tsl], Wkv3[:, j, g * 512:(g + 1) * 512]) for j in range(2)], [Wb[3]] + Rb[3:5])
                kv3 = pskv.ap[:, :].rearrange("p (h d) -> p h d", h=4)
                kbb, kb_ = ns()
                kb3 = kb_.bitcast(BF16)[:, 0:384].rearrange("p (h d) -> p h d", h=4)
                ts('dve', kb3[:, :, 0:64], kv3[:, :, 0:64], rkv[:, t:t + 1], None, ALU.mult, None, [pskv, small], [kbb])
                ts('dve', v4[:, t, :, 0:64], kv3[:, :, 64:128], rkv[:, t:t + 1], None, ALU.mult, None, [pskv, small], Rb[13:16])
                cp(PREP_ENG, kb3[:, :, 64:96], ko3[:, t, None, :].broadcast_to([128, 4, 32]), [kob], [kbb])
                return qbb, qb3, kbb, kb3

            def prep_back(t, st_):
                qbb, qb3, kbb, kb3 = st_
                tsl = slice(t * 128, (t + 1) * 128)
                pb = nb()
                for hh in range(4):
                    tr(pb, pb.ap[0:96, hh * 128:(hh + 1) * 128], qb3[:, hh, :], identB.ap[:], [qbb, identB])
                cp('act', qT3[0:96, :, tsl], pb.ap[0:96, 0:512].rearrange("p (h n) -> p h n", h=4), [pb], Rb[5:9])
                pb = nb()
                for hh in range(4):
                    tr(pb, pb.ap[0:96, hh * 128:(hh + 1) * 128], kb3[:, hh, :], identB.ap[:], [kbb, identB])
                cp('act', kT3[0:96, :, tsl], pb.ap[0:96, 0:512].rearrange("p (h n) -> p h n", h=4), [pb], Rb[9:13])

            st_ = prep_front(0)
            for t in range(NT):
                nx_ = prep_front(t + 1) if t + 1 < NT else None
                prep_back(t, st_)
                st_ = nx_
            steps = [(hh, Q, kt) for hh in range(4) for Q in range(4) for kt in range(4 * Q + 4)]

            def qk_exp(hh, Q, kt):
                qlo = max(512 * Q, 128 * kt)
                n = 512 * (Q + 1) - qlo
                pss = nf()
                mm(pss, pss.ap[:, 0:n], [(kT3[0:96, hh, kt * 128:(kt + 1) * 128], qT3[0:96, hh, qlo:qlo + n])], [Rb[5 + hh], Rb[9 + hh]])
                ptb, pt_ = ns()
                pt = pt_.bitcast(BF16)[:, 0:512]
                act(pt[:, 0:n], pss.ap[:, 0:n], AF.Exp, [pss], [ptb])
                if kt >= 4 * Q:
                    P.op('pool', lambda e, pt=pt: e.memset(pt[64:128, 0:64], 0.0), [ptb], [ptb])
                return ptb, pt, qlo

            cur = qk_exp(*steps[0])
            pso = None
            for i_, (hh, Q, kt) in enumerate(steps):
                nxt = qk_exp(*steps[i_ + 1]) if i_ + 1 < len(steps) else None
                ptb, pt, qlo = cur
                if kt == 0:
                    ctr['z'] += 1
                    pso = psO[ctr['z'] % 2]

                def fn(e, pt=pt, kt=kt, Q=Q, qlo=qlo, hh=hh, pso=pso):
                    ins = None
                    for s_ in range((qlo - 512 * Q) // 128, 4):
                        off = 512 * Q + 128 * s_ - qlo
                        ins = e.matmul(pso.ap[:, s_ * 65:(s_ + 1) * 65], pt[:, off:off + 128], v4[:, kt, hh, 0:65],
                                       start=(kt == 0 and s_ == 0), stop=(kt == 4 * Q + s_), skip_group_check=True)
                    return ins
                P.op('pe', fn, [ptb] + Rb[13:16], [pso])
                if kt == 4 * Q + 3:
                    pso3 = pso.ap[:, 0:260].rearrange("p (s d) -> p s d", s=4)
                    rdb, rd_ = ns()
                    rd3 = rd_[:, 0:4].rearrange("p (s o) -> p s o", o=1)
                    P.op('dve', lambda e, rd3=rd3, pso3=pso3: e.reciprocal(rd3, pso3[:, :, 64:65]), [pso], [rdb])
                    hc = (4 * g + hh) * 64
                    tt('dve', o_tm[:, 4 * Q:4 * Q + 4, hc:hc + 64], pso3[:, :, 0:64], rd3.broadcast_to([128, 4, 64]), ALU.mult,
                       [pso, rdb], Mb[0:4])
                cur = nxt
        if tap == 'qk':
            P.dma('sp', dbg_buf, dbg_d[0], R_t[:, :], reads=Rb[0:16])
            P.dma('sp', dbg_buf, dbg_d[1, :, 0:8192], M_t[:, :].bitcast(F32), reads=Mb[0:4])
            break
        oT3 = Rv(0, 4, BF16).rearrange("p (c n) -> p c n", c=4)
        for t in range(NT):
            pb = nb()
            for c in range(4):
                tr(pb, pb.ap[:, c * 128:(c + 1) * 128], o_tm[:, t, c * 128:(c + 1) * 128], identB.ap[:], Mb[0:4] + [identB])
            cp('act', oT3[:, :, t * 128:(t + 1) * 128], pb.ap[:, 0:512].rearrange("p (c n) -> p c n", c=4), [pb], Rb[0:4])
        merge_branch(l, 2, oT3, Rb[0:4], mla_proj[l], True)
        if tap:
            P.dma('pool', dbg_buf, dbg_d[0], M_t[:, :], reads=Mb[0:4])

        W3 = Wv(3).rearrange("p (k n) -> p k n", k=8)
        wload([Wb[3]], W3, wsrc(l)[:, :, 0:512])
        pw3 = poolw_t.ap[:].rearrange("p (g d) -> p g d", g=4)
        P.dma('pool', poolw_t, pw3, pool_w[l].rearrange("g c d -> c g d"))
        sU = Rv(0, 3)[:, 0:2064]
        sB_ = Rv(3, 3)[:, 0:2064]
        sC = Rv(11, 3)[:, 0:2064]
        mixT = Rv(6, 1, BF16)
        pmT3 = Rv(7, 4, BF16).rearrange("p (c n) -> p c n", c=4)
        P.op('pool', lambda e: e.memset(sU[:, 0:16], 0.0), [], Rb[0:3])
        P.op('pool', lambda e: e.memset(sB_[:, 0:16], 0.0), [], Rb[3:6])
        P.op('pool', lambda e: e.memset(sC[:, 0:16], 0.0), [], Rb[11:14])
        for g in range(4):
            for T in range(4):
                ps = nf()
                mm(ps, ps.ap[:, :], [(W3[:, k, g * 128:(g + 1) * 128], hT3[:, k, T * 512:(T + 1) * 512]) for k in range(8)], [Wb[3]] + hTb[4 * T:4 * T + 4])
                cp('act', sU[:, 16 + T * 512:16 + (T + 1) * 512], ps.ap[:, :], [ps], Rb[0:3])
            src, srcb = sU, Rb[0:3]
            pp_ = [(sB_, Rb[3:6]), (sC, Rb[11:14])]
            for jj in range(g + 1):
                sh = 1 << jj
                dst, dstb = pp_[jj % 2]
                tt('dve', dst[:, 16:2064], src[:, 16:2064], src[:, 16 - sh:2064 - sh], ALU.add, srcb, dstb)
                src, srcb = dst, dstb
            w = 2 << g
            stt('dve', mixT[:, 0:2048], src[:, 16:2064], 1.0 / w, sU[:, 16:2064], ALU.mult, ALU.subtract, srcb + Rb[0:3], [Rb[6]])
            fb, f_ = ns()
            tt('dve', f_[:, 0:w - 1], src[:, 16:16 + w - 1], rcnt.ap[:, 0:w - 1], ALU.mult, srcb + [rcnt], [fb])
            tt('dve', mixT[:, 0:w - 1], f_[:, 0:w - 1], sU[:, 16:16 + w - 1], ALU.subtract, [fb] + Rb[0:3], [Rb[6]])
            for T in range(4):
                ps = nf()
                mm(ps, ps.ap[:, :], [(pw3[:, g, :], mixT[:, T * 512:(T + 1) * 512])], [poolw_t, Rb[6]])
                ts('dve', pmT3[:, g, T * 512:(T + 1) * 512], ps.ap[:, :], colA.ap[:, 24 + g:25 + g], None, ALU.mult, None, [ps, colA], [Rb[7 + g]])
        merge_branch(l, 0, pmT3, Rb[7:11], pool_proj[l], False)
        if tap:
            P.dma('pool', dbg_buf, dbg_d[1], M_t[:, :], reads=Mb[0:4])

        wload([Wb[3]], W3, wsrc(l)[:, :, 512:1024])
        W0c = Wv(0).rearrange("p (k n) -> p k n", k=8)
        wload([Wb[0]], W0c, wsrc(l)[:, :, 1024:1536])
        zcb = Rv(0, 2, BF16)[:, 0:2080]
        conv4 = Rv(3, 8).rearrange("p (c n) -> p c n", c=4)
        cact3 = Rv(11, 4, BF16).rearrange("p (c n) -> p c n", c=4)
        P.op('pool', lambda e: e.memset(zcb[:, 0:32], 0.0), [], Rb[0:2])
        for c in range(4):
            dslot = 1 + (c % 2)
            D3 = Wv(dslot).rearrange("p (k n) -> p k n", k=32)
            for k in range(31):
                ts('dve', D3[:, k, :], identB.ap[:], colB.ap[:, k * 4 + c:k * 4 + c + 1], None, ALU.mult, None, [identB, colB], [Wb[dslot]])
            for T in range(4):
                tsl = slice(T * 512, (T + 1) * 512)
                psa = nf()
                mm(psa, psa.ap[:, :], [(W3[:, k, c * 128:(c + 1) * 128], hT3[:, k, tsl]) for k in range(8)], [Wb[3]] + hTb[4 * T:4 * T + 4])
                psg = nf()
                mm(psg, psg.ap[:, :], [(W0c[:, k, c * 128:(c + 1) * 128], hT3[:, k, tsl]) for k in range(8)], [Wb[0]] + hTb[4 * T:4 * T + 4])
                sgb, sg = ns()
                act(sg, psg.ap[:, :], AF.Sigmoid, [psg], [sgb])
                tt('dve', zcb[:, 32 + T * 512:32 + (T + 1) * 512], psa.ap[:, :], sg, ALU.mult, [psa, sgb], Rb[0:2])
            for T in range(4):
                psc = nf()
                mm(psc, psc.ap[:, :], [(D3[:, k, :], zcb[:, 2 + k + T * 512:2 + k + (T + 1) * 512]) for k in range(31)], [Wb[dslot]] + Rb[0:2])
                ts('dve', conv4[:, c, T * 512:(T + 1) * 512], psc.ap[:, :], colA.ap[:, 28 + c:29 + c], None, ALU.add, None,
                   [psc, colA], [Rb[3 + 2 * c + T // 2]])
        for T in range(4):
            tsl = slice(T * 512, (T + 1) * 512)
            cbs = [[Rb[3 + 2 * c + T // 2]] for c in range(4)]
            ps1 = nf()
            mm(ps1, ps1.ap[:, :], [(onesF.ap[:], conv4[:, c, tsl]) for c in range(4)], [onesF] + sum(cbs, []))
            sqs = []
            for c in range(4):
                sqb, sq = ns()
                act(sq, conv4[:, c, tsl], AF.Square, cbs[c], [sqb])
                sqs.append((sqb, sq))
            ps2 = nf()
            mm(ps2, ps2.ap[:, :], [(onesF.ap[:], sq) for (_, sq) in sqs], [onesF] + [b_ for (b_, _) in sqs])
            mb_, m_ = ns()
            P.op('act', lambda e, m_=m_, ps1=ps1: e.mul(m_, ps1.ap[:, :], 1.0 / 512), [ps1], [mb_])
            qb_, msq = ns()
            tt('dve', msq, m_, m_, ALU.mult, [mb_], [qb_])
            vb_, var = ns()
            stt('dve', var, ps2.ap[:, :], 1.0 / 512, msq, ALU.mult, ALU.subtract, [ps2, qb_], [vb_])
            act(var, var, AF.Sqrt, [vb_, epsb], [vb_], bias=epsb.ap[:, 0:1])
            P.op('dve', lambda e, var=var: e.reciprocal(var, var), [vb_], [vb_])
            for c in range(4):
                t1b, t1 = ns()
                tt('dve', t1, conv4[:, c, tsl], m_, ALU.subtract, cbs[c] + [mb_], [t1b])
                tt('dve', t1, t1, var, ALU.mult, [t1b, vb_], [t1b])
                ts('dve', t1, t1, colA.ap[:, 32 + c:33 + c], colA.ap[:, 36 + c:37 + c], ALU.mult, ALU.add, [t1b, colA], [t1b])
                act(cact3[:, c, tsl], t1, AF.Silu, [t1b], [Rb[11 + c]])
        merge_branch(l, 1, cact3, Rb[11:15], conv_proj[l], False)
        if tap:
            P.dma('pool', dbg_buf, dbg_d[2], M_t[:, :], reads=Mb[0:4])

        Wo3 = Wv(0, 2).rearrange("p (k n) -> p k n", k=8)
        wload([Wb[0]], Wo3[:, 0:4, :], w_out[l].rearrange("(k p) n -> p k n", p=128)[:, 0:4, :])
        wload([Wb[1]], Wo3[:, 4:8, :], w_out[l].rearrange("(k p) n -> p k n", p=128)[:, 4:8, :])
        g_ap, b_ap, gbb = load_gb(ln1_g[l:l + 1, :], ln1_b[l:l + 1, :], 3)
        if tap == 'h32' and last:
            P.dma('sp', dbg_buf, dbg_d[0].rearrange("p (t d) -> p t d", t=16), h32_d.rearrange("(t p) d -> p t d", p=128), reads=[h32_buf])
            P.dma('sp', dbg_buf, dbg_d[1, :, 0:8192], hT_t[:, :].bitcast(F32), reads=hTb[0:16])
        def ln1_front(t):
            zi = t % NZ
            zbuf, z = ZL[zi]
            P.dma('sp', zbuf, z, h32_d[t * 128:(t + 1) * 128, :], reads=[h32_buf])
            for h in range(2):
                psy = nf()
                mm(psy, psy.ap[:, :], [(M3[:, k, t * 128:(t + 1) * 128], Wo3[:, k, h * 512:(h + 1) * 512]) for k in range(8)], [Wb[0], Wb[1], Mb[t // 4]])
                stt('dve', z[:, h * 512:(h + 1) * 512], z[:, h * 512:(h + 1) * 512], ALPHA, psy.ap[:, :], ALU.mult, ALU.add, [zbuf, psy], [zbuf])

        d1 = 'out' if (tap == 'ln1' and last) else ('acc+out' if (tap == 'ffn2' and last) else 'acc')
        ln1_front(0)
        ln1_front(1)
        ln_tile(0, 0, g_ap, b_ap, gbb, d1, part='a')
        for t in range(NT):
            if t + 2 < NT:
                ln1_front(t + 2)
            if t + 1 < NT:
                ln_tile(t + 1, (t + 1) % NZ, g_ap, b_ap, gbb, d1, part='a')
            ln_tile(t, t % NZ, g_ap, b_ap, gbb, d1, part='b')
        if tap == 'ln1' and last:
            break
        if tap == 'h32' and last and os.environ.get("BRK") == "ln1":
            break

        Wpg3 = Wv(0, 2).rearrange("p (k n) -> p k n", k=8)
        wload([Wb[0]], Wpg3[:, 0:4, :], ple_gate[l].rearrange("(k p) n -> p k n", p=128)[:, 0:4, :])
        wload([Wb[1]], Wpg3[:, 4:8, :], ple_gate[l].rearrange("(k p) n -> p k n", p=128)[:, 4:8, :])
        Wpp3 = Wv(3)[:, 0:2048].rearrange("p (c n) -> p c n", c=2)
        wload([Wb[3]], Wpp3, ple_proj[l].rearrange("(c p) n -> p c n", p=128))
        pT3 = Wv(2).rearrange("p (c n) -> p c n", c=2)
        for t in range(NT):
            sbf, sap = ns()
            pin = sap.bitcast(BF16)[:, 0:256]
            P.dma('pool', sbf, pin, p_d[l, t * 128:(t + 1) * 128, :])
            pb = nb()
            for c in range(2):
                tr(pb, pb.ap[:, c * 128:(c + 1) * 128], pin[:, c * 128:(c + 1) * 128], identB.ap[:], [sbf, identB])
            cp('act', pT3[:, :, t * 128:(t + 1) * 128], pb.ap[:, 0:256].rearrange("p (c n) -> p c n", c=2), [pb], [Wb[2]])
        psr = nf()
        wr3 = wr_t.ap[:].rearrange("p (k e) -> p k e", k=8)
        for t in range(NT):
            mm(psr, psr.ap[:, t * 16:(t + 1) * 16], [(hT3[:, k, t * 128:(t + 1) * 128], wr3[:, k, :]) for k in range(8)], [wr_t, hTb[t]])
        ab_, aff = ns(); aff = aff[:, 0:256]
        act(aff, psr.ap[:, 0:256], AF.Sigmoid, [psr], [ab_])
        sb2, sel = ns(); sel = sel[:, 0:256]
        tt('dve', sel.rearrange("p (t e) -> p t e", t=16), aff.rearrange("p (t e) -> p t e", t=16),
           rb_t.ap[:, None, :].broadcast_to([128, 16, 16]), ALU.add, [ab_, rb_t], [sb2])
        sel4 = sel.rearrange("p (g e) -> p g e", e=4)
        xb1, x1 = ns(); xb2, x2 = ns(); xb3, x3 = ns(); xb4, x4 = ns()
        m1 = x1[:, 0:64]; m2 = x1[:, 64:128]; gs_ = x1[:, 128:192]; gmx = x1[:, 192:208]; pen = x1[:, 256:320]
        e1 = x1[:, 320:336]; e2 = x1[:, 336:352]; wsum = x1[:, 352:368]
        eq = x2[:, 0:256]; sel2 = x3[:, 0:256]; mk = x4[:, 0:256]
        red(m1, sel4, ALU.max, [sb2], [xb1])
        tt('dve', eq.rearrange("p (g e) -> p g e", e=4), sel4, m1[:, :, None].broadcast_to([128, 64, 4]), ALU.is_equal, [sb2, xb1], [xb2])
        stt('dve', sel2, eq, -BIG, sel, ALU.mult, ALU.add, [xb2, sb2], [xb3])
        red(m2, sel2.rearrange("p (g e) -> p g e", e=4), ALU.max, [xb3], [xb1])
        tt('dve', gs_, m1, m2, ALU.add, [xb1], [xb1])
        red(gmx, gs_.rearrange("p (t g) -> p t g", g=4), ALU.max, [xb1], [xb1])
        tt('dve', pen.rearrange("p (t g) -> p t g", g=4), gs_.rearrange("p (t g) -> p t g", g=4),
           gmx[:, :, None].broadcast_to([128, 16, 4]), ALU.is_equal, [xb1], [xb1])
        ts('dve', pen, pen, BIG, -BIG, ALU.mult, ALU.add, [xb1], [xb1])
        tt('dve', sel2.rearrange("p (g e) -> p g e", e=4), sel4, pen[:, :, None].broadcast_to([128, 64, 4]), ALU.add, [sb2, xb1], [xb3])
        sel2_3 = sel2.rearrange("p (t e) -> p t e", t=16)
        red(e1, sel2_3, ALU.max, [xb3], [xb1])
        tt('dve', mk.rearrange("p (t e) -> p t e", t=16), sel2_3, e1[:, :, None].broadcast_to([128, 16, 16]), ALU.is_equal, [xb3, xb1], [xb4])
        stt('dve', eq, mk, -2 * BIG, sel2, ALU.mult, ALU.add, [xb4, xb3], [xb2])
        eq3 = eq.rearrange("p (t e) -> p t e", t=16)
        red(e2, eq3, ALU.max, [xb2], [xb1])
        tt('dve', sel2_3, eq3, e2[:, :, None].broadcast_to([128, 16, 16]), ALU.is_equal, [xb2, xb1], [xb3])
        tt('dve', mk, mk, sel2, ALU.add, [xb4, xb3], [xb4])
        mkb = x3[:, 256:384].bitcast(BF16)
        cp('dve', mkb, mk, [xb4], [xb3])
        w_ = eq
        tt('dve', w_, mk, aff, ALU.mult, [xb4, ab_], [xb2])
        P.op('dve', lambda e, wsum=wsum, w_=w_: e.tensor_reduce(wsum, w_.rearrange("p (t e) -> p t e", t=16), AX.X, ALU.add), [xb2], [xb1])
        P.op('dve', lambda e, wsum=wsum: e.reciprocal(wsum, wsum), [xb1], [xb1])
        tt('dve', w_.rearrange("p (t e) -> p t e", t=16), w_.rearrange("p (t e) -> p t e", t=16),
           wsum[:, :, None].broadcast_to([128, 16, 16]), ALU.mult, [xb2, xb1], [xb2])
        psR = nf()

        def fn_rank(e, mkb=mkb, psR=psR):
            ins = None
            first = True
            for i in range(NT):
                for i2 in range(i + 1):
                    ins = e.matmul(psR.ap[:, i * 16:(i + 1) * 16], (upperB.ap[:] if i2 == i else onesB.ap[:]), mkb[:, i2 * 16:(i2 + 1) * 16],
                                   start=first, stop=(i2 == i), skip_group_check=True)
                    first = False
            return ins
        P.op('pe', fn_rank, [xb3, onesB, upperB], [psR])
        xb5, x5 = ns(); xb6, x6 = ns()
        dst = x5[:, 0:256]; lt = x5[:, 256:512]; dm = x6[:, 0:256]; eqa = x6[:, 256:512]
        v3 = lambda a: a.rearrange("p (t e) -> p t e", t=16)
        ts('dve', lt, psR.ap[:, 0:256], float(CAP), None, ALU.is_lt, None, [psR], [xb5])
        tt('dve', v3(dst), v3(psR.ap[:, 0:256]), ecap_t.ap[:, None, :].broadcast_to([128, 16, 16]), ALU.add, [psR, ecap_t], [xb5])
        ts('dve', dst, dst, -float(NSLOT), None, ALU.add, None, [xb5], [xb5])
        tt('dve', dst, dst, lt, ALU.mult, [xb5], [xb5])
        ts('dve', dst, dst, float(NSLOT), None, ALU.add, None, [xb5], [xb5])
        ts('dve', dm, dst, -BIGV, None, ALU.add, None, [xb5], [xb6])
        tt('dve', dm, dm, mk, ALU.mult, [xb6, xb4], [xb6])
        ts('dve', dm, dm, BIGV, None, ALU.add, None, [xb6], [xb6])
        destA = wt_t.ap[:, 32:48]; destB = wt_t.ap[:, 48:64]; wA = wt_t.ap[:, 0:16]; wB = wt_t.ap[:, 16:32]
        idxA = wt_t.ap[:, 64:80].bitcast(I32); idxB = wt_t.ap[:, 80:96].bitcast(I32)
        P.op('dve', lambda e, dm=dm: e.tensor_reduce(destA, v3(dm), AX.X, ALU.min), [xb6], [wt_t])
        tt('dve', v3(eqa), v3(dm), destA[:, :, None].broadcast_to([128, 16, 16]), ALU.is_equal, [xb6, wt_t], [xb6])
        tt('dve', eqa, eqa, w_, ALU.mult, [xb6, xb2], [xb6])
        P.op('dve', lambda e, eqa=eqa: e.tensor_reduce(wA, v3(eqa), AX.X, ALU.add), [xb6], [wt_t])
        ts('dve', wB, wA, -1.0, 1.0, ALU.mult, ALU.add, [wt_t], [wt_t])
        ts('dve', dm, dst, 1.0, None, ALU.add, None, [xb5], [xb6])
        tt('dve', dm, dm, mk, ALU.mult, [xb6, xb4], [xb6])
        ts('dve', dm, dm, -1.0, None, ALU.add, None, [xb6], [xb6])
        P.op('dve', lambda e, dm=dm: e.tensor_reduce(destB, v3(dm), AX.X, ALU.max), [xb6], [wt_t])
        cp('dve', idxA, destA, [wt_t], [wt_t])
        cp('dve', idxB, destB, [wt_t], [wt_t])
        stage4 = [(HBb[0], HB_t[:, 0:1024]), (HBb[1], HB_t[:, 1024:2048]),
                  (Zb[0], Zs[0][:, 0:512].bitcast(BF16)), (Zb[1], Zs[1][:, 0:512].bitcast(BF16))]
        for t in range(NT):
            sbuf_, hbt = stage4[t % 4]
            P.dma('sp', sbuf_, hbt, hbf_d[t * 128:(t + 1) * 128, :], reads=[hbf_bufs[t]])
            for ix in (idxA, idxB):
                P.dma_fn('pool', xs_bufs[t], lambda e, ix=ix, t=t, hbt=hbt: e.indirect_dma_start(
                    out=xs_d[:, :], out_offset=bass.IndirectOffsetOnAxis(ap=ix[:, t:t + 1], axis=0),
                    in_=hbt, in_offset=None), reads=[sbuf_, wt_t])

        for t in range(NT):
            tsl = slice(t * 128, (t + 1) * 128)
            for h in range(2):
                hsl = slice(h * 512, (h + 1) * 512)
                pse = nf()
                mm(pse, pse.ap[:, :], [(pT3[:, c, tsl], Wpp3[:, c, hsl]) for c in range(2)], [Wb[2], Wb[3]])
                psg = nf()
                mm(psg, psg.ap[:, :], [(hT3[:, k, tsl], Wpg3[:, k, hsl]) for k in range(8)], [Wb[0], Wb[1], hTb[t]])
                sgb, sg = ns()
                act(sg, psg.ap[:, :], AF.Sigmoid, [psg], [sgb])
                tb_, tmp = ns()
                tt('dve', tmp, pse.ap[:, :], sg, ALU.mult, [pse, sgb], [tb_])
                accv = R_t[:, t * 1024 + h * 512:t * 1024 + (h + 1) * 512]
                tt('dve', accv, accv, tmp, ALU.add, [Rb[t], tb_], [Rb[t]])

        def wset(e_):
            if e_ % 2 == 0:
                return (Wv(0, 2).rearrange("p (k n) -> p k n", k=8), Wv(2).rearrange("p (k n) -> p k n", k=4),
                        [Wb[0], Wb[1]], [Wb[2]])
            return (M_t[:, 0:8192].rearrange("p (k n) -> p k n", k=8), M_t[:, 8192:12288].rearrange("p (k n) -> p k n", k=4),
                    Mb[0:4], Mb[0:4])

        def wfetch(e_):
            Wu3, Wd3, ub, db = wset(e_)
            src = exp_w_up[l, e_].rearrange("(k p) n -> p k n", p=128)
            if e_ % 2 == 0:
                wload([Wb[0]], Wu3[:, 0:4, :], src[:, 0:4, :])
                wload([Wb[1]], Wu3[:, 4:8, :], src[:, 4:8, :])
                wload([Wb[2]], Wd3, exp_w_down[l, e_].rearrange("(k p) n -> p k n", p=128))
            else:
                wload(Mb[0:4], Wu3, src)
                wload(Mb[0:4], Wd3, exp_w_down[l, e_].rearrange("(k p) n -> p k n", p=128))
        ctr['smin'] = 4
        xsT_views = [S_t[:, 0:2048].bitcast(BF16).rearrange("p (k n) -> p k n", k=8),
                     M_t[:, 12288:16384].rearrange("p (k n) -> p k n", k=8)]
        XB = Buf("XB")
        ysl = Wv(3).bitcast(F32)
        land4 = [(HBb[0], HB_t[:, 0:1024]), (HBb[1], HB_t[:, 1024:2048]),
                 (Zb[0], Zs[0][:, 0:512].bitcast(BF16)), (Zb[1], Zs[1][:, 0:512].bitcast(BF16))]

        def prep_load(e_):
            for blk in range(4):
                lb, lap = land4[blk]
                r0 = e_ * CAP + blk * 128
                P.dma('sp', lb, lap, xs_d[r0:r0 + 128, :], reads=xs_bufs)

        def prep_T(e_):
            xsT3 = xsT_views[e_ % 2]
            xbufs = Sb[0:4] if e_ % 2 == 0 else [XB]
            for blk in range(4):
                lb, lap = land4[blk]
                pb = nb()
                for j in range(8):
                    tr(pb, pb.ap[:, j * 128:(j + 1) * 128], lap[:, j * 128:(j + 1) * 128], identB.ap[:], [lb, identB])
                cp('dve', xsT3[:, :, blk * 128:(blk + 1) * 128], pb.ap[:].rearrange("p (k n) -> p k n", k=8), [pb],
                   xbufs + (Mb[0:4] if (e_ == 1 and blk == 0) else []))

        def compute_up(e_):
            Wu3, Wd3, ub, db = wset(e_)
            xsT3 = xsT_views[e_ % 2]
            xbufs = Sb[0:4] if e_ % 2 == 0 else [XB]
            ai = e_ % 2
            aT3 = actT_t.ap[:, ai * 2048:(ai + 1) * 2048].rearrange("p (j n) -> p j n", j=4)
            for j in range(4):
                psg = nf()
                mm(psg, psg.ap[:, :], [(Wu3[:, k, j * 128:(j + 1) * 128], xsT3[:, k, :]) for k in range(8)], ub + xbufs)
                psu = nf()
                mm(psu, psu.ap[:, :], [(Wu3[:, k, 512 + j * 128:512 + (j + 1) * 128], xsT3[:, k, :]) for k in range(8)], ub + xbufs)
                sgb, sg_ = ns()
                sg = sg_.bitcast(BF16)[:, 0:512]
                act(sg, psg.ap[:, :], AF.Silu, [psg], [sgb])
                tt('dve', aT3[:, j, :], psu.ap[:, :], sg, ALU.mult, [psu, sgb], [actTb[ai]])

        def compute_down(e_):
            Wu3, Wd3, ub, db = wset(e_)
            ai = e_ % 2
            aT3 = actT_t.ap[:, ai * 2048:(ai + 1) * 2048].rearrange("p (j n) -> p j n", j=4)
            for s_ in range(4):
                yi = s_ % 2
                first_ = (e_ == 0 and s_ < 2)
                final_ = (e_ == 15 and s_ >= 2)
                for h in range(2):
                    psd = nf()
                    mm(psd, psd.ap[:, :], [(aT3[:, j, s_ * 128:(s_ + 1) * 128], Wd3[:, j, h * 512:(h + 1) * 512]) for j in range(4)], db + [actTb[ai]])
                    cp('act', ysl[:, yi * 1024 + h * 512:yi * 1024 + (h + 1) * 512], psd.ap[:, :], [psd], [YSb[yi]] + ([Wb[3]] if first_ else []))
                r0 = e_ * CAP + s_ * 128
                P.dma('act', ys_bufs[e_], ys_d[r0:r0 + 128, :], ysl[:, yi * 1024:(yi + 1) * 1024], reads=[YSb[yi]] + ([Wb[3]] if final_ else []))

        wfetch(0)
        wfetch(1)
        prep_load(0)
        prep_T(0)
        for e_ in range(16):
            if e_ + 1 < 16:
                prep_load(e_ + 1)
            compute_up(e_)
            if e_ + 1 < 16:
                prep_T(e_ + 1)
            compute_down(e_)
            if e_ + 2 < 16:
                wfetch(e_ + 2)
        if not last:
            layer_prologue(l + 1)
        g_ap, b_ap, gbb = load_gb(ln2_g[l:l + 1, :], ln2_b[l:l + 1, :], 3)
        land = [(Zb[0], Zs[0][:, :]), (Zb[1], Zs[1][:, :])]
        for t in range(NT):
            accv = R_t[:, t * 1024:(t + 1) * 1024]
            for q_, (ix, wv) in enumerate(((idxA, wA), (idxB, wB))):
                lb, lap = land[q_]
                P.dma_fn('pool', lb, lambda e, lap=lap, ix=ix, t=t: e.indirect_dma_start(
                    out=lap, out_offset=None, in_=ys_d[:, :],
                    in_offset=bass.IndirectOffsetOnAxis(ap=ix[:, t:t + 1], axis=0)),
                    reads=ys_bufs + [wt_t])
                stt('dve', accv, lap, wv[:, t:t + 1], accv, ALU.mult, ALU.add, [lb, wt_t, Rb[t]], [Rb[t]])
            zi = 2 + t % 2
            d2 = 'out' if (last and tap != 'ffn2') else 'h32'
            cp('act', ZL[zi][1], accv, [Rb[t]], [ZL[zi][0]])
            ln_tile(t, zi, g_ap, b_ap, gbb, d2, part='a')
            for tb in ([t - 1] if t >= 1 else []) + ([t] if t == NT - 1 else []):
                ln_tile(tb, 2 + tb % 2, g_ap, b_ap, gbb, d2, part='b')
                if (not last) and tb in (4, 7, 11, 15):
                    proj_T(l + 1, (4, 7, 11, 15).index(tb))
        ctr['smin'] = 0

    P.finish([out_buf] + ([dbg_buf] if tap else []))
    return nc


def make_in_maps(inp, cores=range(8)):
    f = lambda a: np.ascontiguousarray(np.asarray(a, dtype=np.float32))
    invf = (10000.0 ** (-np.arange(0, 32, 2, dtype=np.float32) / 32)).astype(np.float32)
    shared = {
        "invf": np.ascontiguousarray(np.broadcast_to(invf[None, :], (128, 16))),
        "ecap": np.ascontiguousarray(np.broadcast_to((np.arange(16, dtype=np.float32) * CAP)[None, :], (128, 16))),
        "rcnt": np.ascontiguousarray(np.broadcast_to((1.0 / np.arange(1, 17, dtype=np.float32))[None, :], (128, 16))),
        "ln_in_g": f(inp["ln_in_g"]).reshape(1, D), "ln_in_b": f(inp["ln_in_b"]).reshape(1, D),
        "w_in": f(inp["w_in"]), "b_gate": f(inp["b_gate"]).reshape(4, 24, 128),
        "pool_w": f(inp["pool_w"]), "pool_scale": f(inp["pool_scale"]).reshape(4, 4, 128),
        "pool_proj": f(inp["pool_proj"]), "conv_dw": f(inp["conv_dw"]).reshape(4, 124, 128),
        "conv_b": f(inp["conv_b"]).reshape(4, 4, 128), "conv_ln_g": f(inp["conv_ln_g"]).reshape(4, 4, 128),
        "conv_ln_b": f(inp["conv_ln_b"]).reshape(4, 4, 128), "conv_proj": f(inp["conv_proj"]),
        "q_norm_g": f(inp["q_norm_g"]).reshape(4, 3, 128), "w_uq": f(inp["w_uq"]),
        "kv_norm_g": f(inp["kv_norm_g"]).reshape(4, 2, 128), "w_ukv": f(inp["w_ukv"]),
        "mla_proj": f(inp["mla_proj"]), "w_out": f(inp["w_out"]), "ln1_g": f(inp["ln1_g"]), "ln1_b": f(inp["ln1_b"]),
        "w_router": f(inp["w_router"]), "router_bias": f(inp["router_bias"]).reshape(1, 16),
        "exp_w_up": f(inp["exp_w_up"]), "exp_w_down": f(inp["exp_w_down"]),
        "ple_proj": f(inp["ple_proj"]), "ple_gate": f(inp["ple_gate"]), "ln2_g": f(inp["ln2_g"]), "ln2_b": f(inp["ln2_b"]),
    }
    maps = []
    for c in cores:
        m = dict(shared)
        m["x"] = f(inp["x"][c])
        m["p"] = f(inp["p"][:, c])
        m["pos"] = np.ascontiguousarray(np.asarray(inp["positions"][c], dtype=np.int32).reshape(16, 128))
        maps.append(m)
    return maps


_NC_CACHE = {}


def kernel(**inputs):
    if 'nc' not in _NC_CACHE:
        _NC_CACHE['nc'] = build_nc()
    nc = _NC_CACHE['nc']
    maps = make_in_maps(inputs)
    res = run_bass_kernel_spmd(nc, maps, core_ids=list(range(8)))
    return np.stack([np.asarray(r["out"], dtype=np.float32) for r in res.results], axis=0)
```

```python
import os
import numpy as np
import concourse.bass as bass
import concourse.mybir as mybir
from concourse.bass_utils import run_bass_kernel_spmd
from contextlib import ExitStack

F32 = mybir.dt.float32
BF16 = mybir.dt.bfloat16
I32 = mybir.dt.int32
AF = mybir.ActivationFunctionType
ALU = mybir.AluOpType
AX = mybir.AxisListType

ENGS = ('pe', 'act', 'dve', 'pool', 'sp')


class Buf:
    def __init__(self, name, ap=None, semkey=None):
        self.name = name
        self.ap = ap
        self.last_write = None
        self.readers = []
        self.semkey = semkey if semkey is not None else ('b', name)


class Prog:
    def __init__(self, nc):
        self.nc = nc
        self.stack = ExitStack()
        self.ops = {e: [] for e in ENGS}
        self.cnt = {}
        self.seen = {e: {} for e in ENGS}
        self.nops = 0

    def sb(self, name, shape, dtype):
        t = self.stack.enter_context(self.nc.sbuf_tensor("sb_" + name, list(shape), dtype))
        return Buf(name, t)

    def ps(self, name, shape, dtype=F32):
        t = self.stack.enter_context(self.nc.psum_tensor("ps_" + name, list(shape), dtype))
        return Buf(name, t)

    def dram(self, name, ap=None, semkey=None):
        return Buf(name, ap, semkey)

    def view(self, name, ap, semkey=None):
        return Buf(name, ap, semkey)

    def _waits(self, eng, reads, writes):
        need = {}

        def add(tok, war):
            if tok is None:
                return
            k, v = tok
            if k[0] == 'b':
                v = self.cnt.get(k, 0)
            elif k == ('e', eng):
                if eng == 'pe' or war:
                    return
            if need.get(k, 0) < v:
                need[k] = v

        for b in reads:
            add(b.last_write, False)
        for b in writes:
            add(b.last_write, False)
            for t in b.readers:
                add(t, True)
        waits = []
        for k, v in need.items():
            if self.seen[eng].get(k, 0) < v:
                self.seen[eng][k] = v
                waits.append((k, v))
        return waits

    def _commit(self, tok, reads, writes):
        for b in writes:
            b.last_write = tok
            b.readers = []
        for b in reads:
            if b not in writes:
                b.readers.append(tok)

    def op(self, eng, fn, reads=(), writes=()):
        waits = self._waits(eng, reads, writes)
        k = ('e', eng)
        self.cnt[k] = self.cnt.get(k, 0) + 1
        tok = (k, self.cnt[k])
        self.ops[eng].append((waits, fn, k, 1))
        self._commit(tok, reads, writes)
        self.nops += 1

    def dma(self, q, dst, out_ap, in_ap, reads=(), extra_writes=()):
        writes = [dst] + list(extra_writes)
        waits = self._waits(q, reads, writes)
        k = dst.semkey
        self.cnt[k] = self.cnt.get(k, 0) + 16
        tok = (k, self.cnt[k])
        self.ops[q].append((waits, lambda e: e.dma_start(out=out_ap, in_=in_ap), k, 16))
        self._commit(tok, reads, writes)
        self.nops += 1

    def dma_fn(self, q, dst, fn, reads=(), extra_writes=()):
        writes = [dst] + list(extra_writes)
        waits = self._waits(q, reads, writes)
        k = dst.semkey
        self.cnt[k] = self.cnt.get(k, 0) + 16
        tok = (k, self.cnt[k])
        self.ops[q].append((waits, fn, k, 16))
        self._commit(tok, reads, writes)
        self.nops += 1

    def finish(self, outs):
        waits = self._waits('sp', outs, [])
        self.ops['sp'].append((waits, None, None, 0))
        nc = self.nc
        sems = {}
        for k in self.cnt:
            sems[k] = self.stack.enter_context(nc.semaphore("s_%s_%s" % k))
        ops = self.ops
        with nc.Block() as block:
            def mk(engname):
                def body(e):
                    for waits, fn, k, inc in ops[engname]:
                        for (wk, wv) in waits:
                            e.wait_ge(sems[wk], wv)
                        if fn is not None:
                            fn(e).then_inc(sems[k], inc)
                return body
            block.tensor(mk('pe'))
            block.scalar(mk('act'))
            block.vector(mk('dve'))
            block.gpsimd(mk('pool'))
            block.sync(mk('sp'))
        self.stack.close()


S = 2048
D = 1024
NT = 16
ALPHA = 8.0 ** 0.25
BIG = 100.0
CAP = 512
NSLOT = 16 * CAP
BIGV = 1.0e6
MERGE_ENG = os.environ.get('MERGE_ENG', 'dve')
LN_ENG = os.environ.get('LN_ENG', 'dve')
PREP_ENG = os.environ.get('PREP_ENG', 'dve')
LNQ = os.environ.get('LNQ', 'act')


def build_nc(n_layers=4, tap=None):
    nc = bass.Bass("TRN2", target_bir_lowering=False)

    def din(name, shape, dtype=F32):
        return nc.dram_tensor(name, list(shape), dtype, kind="ExternalInput").ap()

    x_d = din("x", [S, D]); p_d = din("p", [4, S, 256]); pos_d = din("pos", [16, 128], I32)
    invf_d = din("invf", [128, 16]); rcnt_d = din("rcnt", [128, 16]); ecap_d = din("ecap", [128, 16])
    ln_in_g = din("ln_in_g", [1, D]); ln_in_b = din("ln_in_b", [1, D])
    w_in = din("w_in", [4, D, 5280]); b_gate = din("b_gate", [4, 24, 128])
    pool_w = din("pool_w", [4, 4, 128, 128]); pool_scale = din("pool_scale", [4, 4, 128])
    pool_proj = din("pool_proj", [4, 512, D]); conv_dw = din("conv_dw", [4, 124, 128])
    conv_b = din("conv_b", [4, 4, 128]); conv_ln_g = din("conv_ln_g", [4, 4, 128]); conv_ln_b = din("conv_ln_b", [4, 4, 128])
    conv_proj = din("conv_proj", [4, 512, D]); q_norm_g = din("q_norm_g", [4, 3, 128]); w_uq = din("w_uq", [4, 384, 768])
    kv_norm_g = din("kv_norm_g", [4, 2, 128]); w_ukv = din("w_ukv", [4, 256, 1024]); mla_proj = din("mla_proj", [4, 512, D])
    w_out = din("w_out", [4, D, D]); ln1_g = din("ln1_g", [4, D]); ln1_b = din("ln1_b", [4, D])
    w_router = din("w_router", [D, 16]); router_bias = din("router_bias", [1, 16])
    exp_w_up = din("exp_w_up", [4, 16, D, 1024]); exp_w_down = din("exp_w_down", [4, 16, 512, D])
    ple_proj = din("ple_proj", [4, 256, D]); ple_gate = din("ple_gate", [4, D, D])
    ln2_g = din("ln2_g", [4, D]); ln2_b = din("ln2_b", [4, D])
    out_d = nc.dram_tensor("out", [S, D], F32, kind="ExternalOutput").ap()
    h32_d = nc.dram_tensor("h32_d", [S, D], F32, kind="Internal").ap()
    xs_d = nc.dram_tensor("xs_d", [NSLOT + 1, D], BF16, kind="Internal").ap()
    hbf_d = nc.dram_tensor("hbf_d", [S, D], BF16, kind="Internal").ap()
    ys_d = nc.dram_tensor("ys_d", [NSLOT + 1, D], F32, kind="Internal").ap()
    dbg_d = nc.dram_tensor("dbg", [3, 128, 8 * S], F32, kind="ExternalOutput").ap() if tap else None
    dbg_buf = Buf("dbg", semkey=('b', 'dbg'))

    P = Prog(nc)
    hT_t = P.sb("hT", [128, 8 * S], BF16).ap
    hT3 = hT_t[:].rearrange("p (k n) -> p k n", k=8)
    hTb = [Buf("hT%d" % t) for t in range(NT)]
    M_t = P.sb("M", [128, 8 * S], BF16).ap
    M3 = M_t[:].rearrange("p (k n) -> p k n", k=8)
    Mb = [Buf("M%d" % i, semkey=('b', 'wB')) for i in range(4)]
    R_t = P.sb("R", [128, 16 * 1024], F32).ap
    Rb = [Buf("R%d" % i) for i in range(16)]
    W_t = P.sb("W", [128, 4 * 4096], BF16).ap
    Wb = [Buf("W%d" % i, semkey=('b', 'w%d' % i)) for i in range(4)]
    S_t = P.sb("Sx", [128, 8 * 512], F32).ap
    Sb = [Buf("S%d" % i) for i in range(8)]
    Zs = [P.sb("Z0", [128, 1024], F32).ap, P.sb("Z1", [128, 1024], F32).ap]
    Zb = [Buf("Z%d" % i, semkey=('b', 'z%d' % i)) for i in range(2)]
    HB_t = P.sb("HB", [128, 2 * 1024], BF16).ap
    HBb = [Buf("HB%d" % i) for i in range(2)]
    identF = P.sb("identF", [128, 128], F32); identB = P.sb("identB", [128, 128], BF16)
    onesF = P.sb("onesF", [128, 128], F32)
    epsb = P.sb("epsb", [128, 2], F32)
    cosT = P.sb("cosT", [128, 256], F32); sinT = P.sb("sinT", [128, 256], F32)
    wr_t = P.sb("wr", [128, 8 * 16], BF16); rb_t = P.sb("rb", [128, 16], F32)
    rcnt = P.sb("rcnt", [128, 16], F32)
    colsrcA = P.sb("colsrcA", [48, 128], F32); colsrcB = P.sb("colsrcB", [124, 128], F32)
    colsrcA.semkey = colsrcB.semkey = ('b', 'cols')
    colA = P.sb("colA", [128, 48], F32); colB = P.sb("colB", [128, 124], F32)
    poolw_t = P.sb("poolw", [128, 4 * 128], BF16)
    st_t = P.sb("st", [128, 4 * 12], F32); mv_t = P.sb("mv", [128, 4 * 2], F32); sd_t = P.sb("sd", [128, 4 * 2], F32)
    stb = [Buf("st%d" % i) for i in range(4)]
    small = P.sb("small", [128, 64], F32)
    actT_t = P.sb("actT", [128, 2 * 2048], BF16)
    actTb = [Buf("actT%d" % i) for i in range(2)]
    ZL = [(Zb[0], Zs[0][:, :]), (Zb[1], Zs[1][:, :]),
          (actTb[0], actT_t.ap[:, 0:2048].bitcast(F32)), (actTb[1], actT_t.ap[:, 2048:4096].bitcast(F32))]
    NZ = 4
    onesB = P.sb("onesB", [128, 128], BF16)
    upperB = P.sb("upperB", [128, 128], BF16)
    ecap_t = P.sb("ecap", [128, 16], F32)
    xs_bufs = [Buf("xs%d" % i, semkey=('b', 'xsd')) for i in range(NT)]
    hbf_bufs = [Buf("hbf%d" % i, semkey=('b', 'hbfd')) for i in range(NT)]
    ys_bufs = [Buf("ys%d" % i, semkey=('b', 'ysd')) for i in range(16)]
    YSb = [Buf("YS%d" % i, semkey=('b', 'ysl%d' % i)) for i in range(2)]
    psF = [P.ps("psF%d" % i, [128, 512], F32) for i in range(4)]
    psB = [P.ps("psB%d" % i, [128, 1024], BF16) for i in range(2)]
    ctr = {'f': 0, 'b': 0, 's': 0, 'z': 0}

    def nf():
        ctr['f'] += 1
        if ctr.get('wide', False):
            return (psF + [psO[1]])[ctr['f'] % 5]
        return psF[ctr['f'] % 4]

    def nb():
        ctr['b'] += 1
        return psB[ctr['b'] % 2]

    def ns():
        ctr['s'] += 1
        lo = ctr.get('smin', 0)
        i = lo + ctr['s'] % (8 - lo)
        return Sb[i], S_t[:, i * 512:(i + 1) * 512]

    def mm(ps, out_ap, terms, reads):
        def fn(e):
            n = len(terms)
            ins = None
            for i, (a, b) in enumerate(terms):
                ins = e.matmul(out_ap, a, b, start=(i == 0), stop=(i == n - 1))
            return ins
        P.op('pe', fn, reads=reads, writes=[ps])

    def tr(ps, out_ap, in_ap, ident_ap, reads, start=True):
        P.op('pe', lambda e: e.transpose(out_ap, in_ap, ident_ap), reads=reads, writes=[ps])

    def act(out, in_, func, reads, writes, bias=None, scale=1.0):
        if bias is None:
            P.op('act', lambda e: e.activation(out, in_, func, scale=scale), reads, writes)
        else:
            P.op('act', lambda e: e.activation(out, in_, func, bias=bias, scale=scale), reads, writes)

    def tt(eng, out, a, b, op, reads, writes):
        P.op(eng, lambda e: e.tensor_tensor(out, a, b, op), reads, writes)

    def ts(eng, out, a, s1, s2, op0, op1, reads, writes):
        if op1 is None:
            P.op(eng, lambda e: e.tensor_scalar(out, a, s1, None, op0), reads, writes)
        else:
            P.op(eng, lambda e: e.tensor_scalar(out, a, s1, s2, op0, op1), reads, writes)

    def stt(eng, out, a, s, b, op0, op1, reads, writes):
        P.op(eng, lambda e: e.scalar_tensor_tensor(out, a, s, b, op0, op1), reads, writes)

    def cp(eng, out, in_, reads, writes):
        if eng == 'act':
            P.op('act', lambda e: e.copy(out, in_), reads, writes)
        else:
            P.op(eng, lambda e: e.tensor_copy(out, in_), reads, writes)

    def red(out, in_, op, reads, writes):
        P.op('dve', lambda e: e.tensor_reduce(out, in_, AX.X, op), reads, writes)

    def wload(bufs, dst_ap, src_ap, q='pool'):
        P.dma(q, bufs[0], dst_ap, src_ap, extra_writes=bufs[1:])

    def Wv(slot, n=1):
        return W_t[:, slot * 4096:(slot + n) * 4096]

    def Rv(i0, n, dtype=F32):
        v = R_t[:, i0 * 1024:(i0 + n) * 1024]
        return v.bitcast(BF16) if dtype == BF16 else v

    taps = {}

    P.op('pool', lambda e: e.memset(identF.ap[:], 1.0), writes=[identF])
    P.op('pool', lambda e: e.affine_select(identF.ap[:], identF.ap[:], pattern=[[-1, 128]], compare_op=ALU.is_equal,
                                           fill=0.0, base=0, channel_multiplier=1), reads=[identF], writes=[identF])
    cp('dve', identB.ap[:], identF.ap[:], [identF], [identB])
    P.op('pool', lambda e: e.memset(onesF.ap[:], 1.0), writes=[onesF])
    P.op('pool', lambda e: e.memset(epsb.ap[:, 0:1], 1e-5), writes=[epsb])
    P.op('pool', lambda e: e.memset(epsb.ap[:, 1:2], 1e-6), reads=[], writes=[epsb])
    P.dma('sp', rcnt, rcnt.ap[:], rcnt_d[:, :])
    P.dma('sp', ecap_t, ecap_t.ap[:], ecap_d[:, :])
    P.op('pool', lambda e: e.memset(onesB.ap[:], 1.0), writes=[onesB])
    P.op('pool', lambda e: e.memset(upperB.ap[:], 1.0), writes=[upperB])
    P.op('pool', lambda e: e.affine_select(upperB.ap[:], upperB.ap[:], pattern=[[1, 128]], compare_op=ALU.is_gt,
                                           fill=0.0, base=0, channel_multiplier=-1), reads=[upperB], writes=[upperB])
    P.op('pool', lambda e: e.memset(Zs[0][0:1, :], 0.0), writes=[Zb[0]])
    P.dma('sp', ys_bufs[0], ys_d[NSLOT:NSLOT + 1, :], Zs[0][0:1, :], reads=[Zb[0]])
    P.dma('sp', rb_t, rb_t.ap[:], router_bias[0:1, :].broadcast_to([128, 16]))
    P.dma('pool', wr_t, wr_t.ap[:].rearrange("p (k e) -> p k e", k=8), w_router.rearrange("(k p) e -> p k e", p=128))
    pi_b, pi_ap = ns()
    P.dma('sp', pi_b, pi_ap[0:16, 0:128].bitcast(I32), pos_d[:, :])
    pf_b, pf_ap = ns()
    cp('dve', pf_ap[0:16, 0:128], pi_ap[0:16, 0:128].bitcast(I32), [pi_b], [pf_b])
    pp = nf()
    tr(pp, pp.ap[:, 0:16], pf_ap[0:16, 0:128], identF.ap[0:16, 0:16], [pf_b, identF])
    post_b, post_ap = ns()
    cp('dve', post_ap[:, 0:16], pp.ap[:, 0:16], [pp], [post_b])
    invf_b, invf_ap = ns()
    P.dma('sp', invf_b, invf_ap[:, 0:16], invf_d[:, :])
    ang_b, ang_ap = ns()
    ang3 = ang_ap[:, 0:256].rearrange("p (t i) -> p t i", t=16)
    tt('dve', ang3, post_ap[:, 0:16, None].broadcast_to([128, 16, 16]), invf_ap[:, None, 0:16].broadcast_to([128, 16, 16]),
       ALU.mult, [post_b, invf_b], [ang_b])

    def sin_of(dst, shift):
        kf_b, kf_ap = ns()
        ki_b, ki_ap = ns()
        a_b, a_ap = ns()
        kf, ki, a = kf_ap[:, 0:256], ki_ap[:, 0:256].bitcast(I32), a_ap[:, 0:256]
        ts('dve', a, ang_ap[:, 0:256], shift, None, ALU.add, None, [ang_b], [a_b])
        ts('dve', kf, a, 1.0 / (2 * np.pi), None, ALU.mult, None, [a_b], [kf_b])
        cp('dve', ki, kf, [kf_b], [ki_b])
        cp('dve', kf, ki, [ki_b], [kf_b])
        stt('dve', a, kf, -2 * np.pi, a, ALU.mult, ALU.add, [a_b, kf_b], [a_b])
        ts('dve', kf, a, np.pi, -2 * np.pi, ALU.is_gt, ALU.mult, [a_b], [kf_b])
        tt('dve', a, a, kf, ALU.add, [a_b, kf_b], [a_b])
        ts('dve', kf, a, -np.pi, 2 * np.pi, ALU.is_lt, ALU.mult, [a_b], [kf_b])
        tt('dve', a, a, kf, ALU.add, [a_b, kf_b], [a_b])
        act(dst.ap[:], a, AF.Sin, [a_b], [dst])
    sin_of(sinT, 0.0)
    sin_of(cosT, np.pi / 2)
    cos3 = cosT.ap[:].rearrange("p (t i) -> p t i", t=16)
    sin3 = sinT.ap[:].rearrange("p (t i) -> p t i", t=16)

    def ln_tile(t, zi, g_ap, b_ap, gb_bufs, dest, part='ab'):
        zb, z = ZL[zi]
        st = st_t.ap[:, zi * 12:(zi + 1) * 12]
        mv = mv_t.ap[:, zi * 2:(zi + 1) * 2]
        sd = sd_t.ap[:, zi * 2:(zi + 1) * 2]
        sb_ = stb[zi]
        if 'a' in part:
            P.op('dve', lambda e: e.bn_stats(st[:, 0:6], z[:, 0:512]), [zb], [sb_])
            P.op('dve', lambda e: e.bn_stats(st[:, 6:12], z[:, 512:1024]), [zb, sb_], [sb_])
            P.op('dve', lambda e: e.bn_aggr(mv, st), [sb_], [sb_])
            act(sd[:, 0:1], mv[:, 1:2], AF.Sqrt, [sb_, epsb], [sb_], bias=epsb.ap[:, 0:1])
        if 'b' not in part:
            return
        P.op('dve', lambda e: e.reciprocal(sd[:, 1:2], sd[:, 0:1]), [sb_], [sb_])
        ts('dve', z, z, mv[:, 0:1], sd[:, 1:2], ALU.subtract, ALU.mult, [zb, sb_], [zb])
        tt(LN_ENG, z, z, g_ap, ALU.mult, [zb] + gb_bufs, [zb])
        tt(LN_ENG, z, z, b_ap, ALU.add, [zb] + gb_bufs, [zb])
        hbb, hb = HBb[zi % 2], HB_t[:, (zi % 2) * 1024:((zi % 2) + 1) * 1024]
        cp('act', hb, z, [zb], [hbb])
        pb = nb()
        for j in range(8):
            tr(pb, pb.ap[:, j * 128:(j + 1) * 128], hb[:, j * 128:(j + 1) * 128], identB.ap[:], [hbb, identB])
        cp('dve', hT3[:, :, t * 128:(t + 1) * 128], pb.ap[:].rearrange("p (k n) -> p k n", k=8), [pb], [hTb[t]])
        if dest == 'h32':
            P.dma('sp', h32_buf, h32_d[t * 128:(t + 1) * 128, :], z, reads=[zb])
        elif dest == 'out':
            P.dma('sp', out_buf, out_d[t * 128:(t + 1) * 128, :], z, reads=[zb])
        else:
            P.op('act', lambda e: e.mul(R_t[:, t * 1024:(t + 1) * 1024], z, ALPHA), [zb], [Rb[t]])
            P.dma(LNQ, hbf_bufs[t], hbf_d[t * 128:(t + 1) * 128, :], hb, reads=[hbb])
            if dest == 'acc+out':
                P.dma('sp', out_buf, out_d[t * 128:(t + 1) * 128, :], z, reads=[zb])

    out_buf = Buf("outd", semkey=('b', 'outd'))
    h32_buf = Buf("h32d_all", semkey=('b', 'h32d'))

    def load_gb(g_src, b_src, slot):
        v = Wv(slot).bitcast(F32)
        P.dma('sp', Wb[slot], v[:, 0:1024], g_src.broadcast_to([128, 1024]))
        P.dma('sp', Wb[slot], v[:, 1024:2048], b_src.broadcast_to([128, 1024]))
        return v[:, 0:1024], v[:, 1024:2048], [Wb[slot]]

    g_ap, b_ap, gbb = load_gb(ln_in_g[0:1, :], ln_in_b[0:1, :], 3)
    d0 = 'h32' if n_layers > 0 else 'out'
    P.dma('sp', ZL[0][0], ZL[0][1], x_d[0:128, :])
    ln_tile(0, 0, g_ap, b_ap, gbb, d0, part='a')
    for t in range(NT):
        if t + 1 < NT:
            zi = (t + 1) % NZ
            P.dma('sp', ZL[zi][0], ZL[zi][1], x_d[(t + 1) * 128:(t + 2) * 128, :])
            ln_tile(t + 1, zi, g_ap, b_ap, gbb, d0, part='a')
        ln_tile(t, t % NZ, g_ap, b_ap, gbb, d0, part='b')


    wt_t = P.sb("wt", [128, 256], F32)
    psS = P.ps("psS", [128, 512], F32)
    psO = [psS, P.ps("psO1", [128, 512], F32)]
    ctr['wide'] = True
    kro_t = P.sb("kro", [128, 512], BF16)
    CB = [[Buf("CB%d_%d" % (c, r)) for r in range(4)] for c in range(4)]
    wsrc = lambda l: w_in[l].rearrange("(k p) n -> p k n", p=128)
    QS = 96.0 ** -0.5

    def merge_branch(l, b, srcT3, src_bufs, proj_d, first):
        Wg3 = Wv(0, 2).rearrange("p (k n) -> p k n", k=8)
        Wp3 = Wv(2).rearrange("p (c n) -> p c n", c=4)
        c0 = 2208 + b * 1024
        wload([Wb[0]], Wg3[:, 0:4, :], wsrc(l)[:, 0:4, c0:c0 + 1024])
        wload([Wb[1]], Wg3[:, 4:8, :], wsrc(l)[:, 4:8, c0:c0 + 1024])
        wload([Wb[2]], Wp3, proj_d.rearrange("(c p) n -> p c n", p=128))
        for T in range(4):
            tsl = slice(T * 512, (T + 1) * 512)
            for j in range(8):
                jsl = slice(j * 128, (j + 1) * 128)
                psy = nf()
                mm(psy, psy.ap[:, :], [(Wp3[:, c, jsl], srcT3[:, c, tsl]) for c in range(4)], [Wb[2]] + src_bufs)
                psg = nf()
                mm(psg, psg.ap[:, :], [(Wg3[:, k, jsl], hT3[:, k, tsl]) for k in range(8)], [Wb[0], Wb[1]] + hTb[4 * T:4 * T + 4])
                gb_, gs_ = ns()
                gs = gs_.bitcast(BF16)[:, 0:512]
                act(gs, psg.ap[:, :], AF.Sigmoid, [psg, colA], [gb_], bias=colA.ap[:, b * 8 + j:b * 8 + j + 1])
                if first:
                    tt('dve', M3[:, j, tsl], psy.ap[:, :], gs, ALU.mult, [psy, gb_], [Mb[T]])
                else:
                    tb_, tm_ = ns()
                    tmp = tm_.bitcast(BF16)[:, 0:512]
                    tt('dve', tmp, psy.ap[:, :], gs, ALU.mult, [psy, gb_], [tb_])
                    tt(MERGE_ENG, M3[:, j, tsl], M3[:, j, tsl], tmp, ALU.add, [Mb[T], tb_], [Mb[T]])

    def proj_T(l, T):
        W0 = Wv(0)[:, 0:8 * 384].rearrange("p (k n) -> p k n", k=8)
        W1 = Wv(1)[:, 0:8 * 288].rearrange("p (k n) -> p k n", k=8)
        cqT = Rv(0, 3, BF16).rearrange("p (k n) -> p k n", k=3)
        ckvT = Rv(3, 2, BF16).rearrange("p (k n) -> p k n", k=2)
        tsl = slice(T * 512, (T + 1) * 512)
        for (nj, Wx, dstT, rb0, col0) in ((3, W0, cqT, 0, 0), (2, W1, ckvT, 3, 16)):
            wbx = Wb[0] if nj == 3 else Wb[1]
            for j in range(nj):
                ps = nf()
                mm(ps, ps.ap[:, :], [(Wx[:, k, j * 128:(j + 1) * 128], hT3[:, k, tsl]) for k in range(8)], [wbx] + hTb[4 * T:4 * T + 4])
                cp('act', dstT[:, j, tsl], ps.ap[:, :], [ps], [Rb[rb0 + j]])
                sqb, sq = ns()
                act(sq, ps.ap[:, :], AF.Square, [ps], [sqb])

                def fn(e, sq=sq, T=T, j=j, nj=nj, col0=col0):
                    ins = None
                    for s_ in range(4):
                        c = col0 + 4 * T + s_
                        ins = e.matmul(psS.ap[:, c:c + 1], sq[:, s_ * 128:(s_ + 1) * 128], onesF.ap[:, 0:1],
                                       start=(c == 0 and j == 0), stop=(j == nj - 1), skip_group_check=True)
                    return ins
                P.op('pe', fn, [sqb, onesF], [psS])

    def layer_prologue(l):
        for (r0, n, src) in ((0, 24, b_gate[l]), (24, 4, pool_scale[l]), (28, 4, conv_b[l]), (32, 4, conv_ln_g[l]),
                             (36, 4, conv_ln_b[l]), (40, 3, q_norm_g[l]), (43, 2, kv_norm_g[l])):
            P.dma('sp', colsrcA, colsrcA.ap[r0:r0 + n, :], src)
        P.dma('sp', colsrcB, colsrcB.ap[:, :], conv_dw[l])
        pa = nf()
        tr(pa, pa.ap[:, 0:45], colsrcA.ap[0:45, :], identF.ap[0:45, 0:45], [colsrcA, identF])
        cp('dve', colA.ap[:, 0:45], pa.ap[:, 0:45], [pa], [colA])
        pa = nf()
        tr(pa, pa.ap[:, 0:124], colsrcB.ap[0:124, :], identF.ap[0:124, 0:124], [colsrcB, identF])
        cp('dve', colB.ap[:, 0:124], pa.ap[:, 0:124], [pa], [colB])

        W0 = Wv(0)[:, 0:8 * 384].rearrange("p (k n) -> p k n", k=8)
        W1 = Wv(1)[:, 0:8 * 288].rearrange("p (k n) -> p k n", k=8)
        Wq3 = Wv(2)[:, 0:3 * 768].rearrange("p (k n) -> p k n", k=3)
        wload([Wb[0]], W0, wsrc(l)[:, :, 1536:1920])
        wload([Wb[1]], W1, wsrc(l)[:, :, 1920:2208])
        wload([Wb[2]], Wq3, w_uq[l].rearrange("(k p) n -> p k n", p=128))
        for j in range(3):
            ts('dve', Wq3[:, j, :], Wq3[:, j, :], colA.ap[:, 40 + j:41 + j], None, ALU.mult, None, [Wb[2], colA], [Wb[2]])

    if n_layers > 0:
        layer_prologue(0)
    for l in range(n_layers):
        last = (l == n_layers - 1)
        W0 = Wv(0)[:, 0:8 * 384].rearrange("p (k n) -> p k n", k=8)
        W1 = Wv(1)[:, 0:8 * 288].rearrange("p (k n) -> p k n", k=8)
        Wq3 = Wv(2)[:, 0:3 * 768].rearrange("p (k n) -> p k n", k=3)
        Wkv3 = Wv(3)[:, 0:2 * 1024].rearrange("p (k n) -> p k n", k=2)
        wload([Wb[3]], Wkv3, w_ukv[l].rearrange("(k p) n -> p k n", p=128))
        for j in range(2):
            ts('dve', Wkv3[:, j, :], Wkv3[:, j, :], colA.ap[:, 43 + j:44 + j], None, ALU.mult, None, [Wb[3], colA], [Wb[3]])
        cqT = Rv(0, 3, BF16).rearrange("p (k n) -> p k n", k=3)
        ckvT = Rv(3, 2, BF16).rearrange("p (k n) -> p k n", k=2)
        if l == 0:
            for T in range(4):
                proj_T(l, T)
        rq = small.ap[:, 0:16]
        rkv = small.ap[:, 16:32]
        act(small.ap[:, 32:48], psS.ap[:, 0:16], AF.Sqrt, [psS, epsb], [small], bias=epsb.ap[:, 1:2], scale=1.0 / 384)
        act(small.ap[:, 48:64], psS.ap[:, 16:32], AF.Sqrt, [psS, epsb], [small], bias=epsb.ap[:, 1:2], scale=1.0 / 256)
        P.op('dve', lambda e: e.reciprocal(small.ap[:, 0:32], small.ap[:, 32:64]), [small], [small])
        ts('dve', rq, rq, QS, None, ALU.mult, None, [small], [small])
        psK = nf()
        for t in range(NT):
            mm(psK, psK.ap[:, t * 32:(t + 1) * 32], [(hT3[:, k, t * 128:(t + 1) * 128], W1[:, k, 256:288]) for k in range(8)], [Wb[1], hTb[t]])
        krb, kr_ = ns()
        cp('act', kr_, psK.ap[:, :], [psK], [krb])
        kr3 = kr_.rearrange("p (t d) -> p t d", t=16)
        kob = kro_t
        ko3 = kro_t.ap[:, :].rearrange("p (t d) -> p t d", t=16)
        tb1, t1_ = ns()
        tb2, t2_ = ns()
        t1 = t1_[:, 0:256].rearrange("p (t d) -> p t d", t=16)
        t2 = t2_[:, 0:256].rearrange("p (t d) -> p t d", t=16)
        tt('dve', t1, kr3[:, :, 0:16], cos3, ALU.mult, [krb, cosT], [tb1])
        tt('dve', t2, kr3[:, :, 16:32], sin3, ALU.mult, [krb, sinT], [tb2])
        tt('dve', ko3[:, :, 0:16], t1, t2, ALU.subtract, [tb1, tb2], [kob])
        tt('dve', t1, kr3[:, :, 16:32], cos3, ALU.mult, [krb, cosT, kob], [tb1])
        tt('dve', t2, kr3[:, :, 0:16], sin3, ALU.mult, [krb, sinT, kob], [tb2])
        tt('dve', ko3[:, :, 16:32], t1, t2, ALU.add, [tb1, tb2], [kob])

        qT3 = Rv(5, 4, BF16).rearrange("p (h n) -> p h n", h=4)
        kT3 = Rv(9, 4, BF16).rearrange("p (h n) -> p h n", h=4)
        v4 = Rv(13, 3, BF16)[:, 0:16 * 260].rearrange("p (t h d) -> p t h d", t=16, h=4)
        o_tm = M_t[:, 0:16 * 512].rearrange("p (t c) -> p t c", t=16)
        for g in range(2):
            P.op('pool', lambda e: e.memset(v4[:, :, :, 64:65], 1.0), [], Rb[13:16])
            def prep_front(t):
                tsl = slice(t * 128, (t + 1) * 128)
                psq = nf()
                mm(psq, psq.ap[:, 0:384], [(cqT[:, j, tsl], Wq3[:, j, g * 384:(g + 1) * 384]) for j in range(3)], [Wb[2]] + Rb[0:3])
                q32b, q32_ = ns()
                ts('dve', q32_[:, 0:384], psq.ap[:, 0:384], rq[:, t:t + 1], None, ALU.mult, None, [psq, small], [q32b])
                q3 = q32_[:, 0:384].rearrange("p (h d) -> p h d", h=4)
                qbb, qb_ = ns()
                qb3 = qb_.bitcast(BF16)[:, 0:384].rearrange("p (h d) -> p h d", h=4)
                cp(PREP_ENG, qb3[:, :, 0:64], q3[:, :, 0:64], [q32b], [qbb])
                cs = cos3[:, t, None, :].broadcast_to([128, 4, 16])
                sn = sin3[:, t, None, :].broadcast_to([128, 4, 16])
                ab, a_ = ns()
                a1 = a_[:, 0:64].rearrange("p (h d) -> p h d", h=4)
                a2 = a_[:, 64:128].rearrange("p (h d) -> p h d", h=4)
                a3 = a_[:, 128:192].rearrange("p (h d) -> p h d", h=4)
                a4 = a_[:, 192:256].rearrange("p (h d) -> p h d", h=4)
                tt('dve', a1, q3[:, :, 64:80], cs, ALU.mult, [q32b, cosT], [ab])
                tt(PREP_ENG, a2, q3[:, :, 80:96], sn, ALU.mult, [q32b, sinT], [ab])
                tt('dve', a3, q3[:, :, 80:96], cs, ALU.mult, [q32b, cosT], [ab])
                tt(PREP_ENG, a4, q3[:, :, 64:80], sn, ALU.mult, [q32b, sinT], [ab])
                tt('dve', qb3[:, :, 64:80], a1, a2, ALU.subtract, [ab], [qbb])
                tt('dve', qb3[:, :, 80:96], a3, a4, ALU.add, [ab], [qbb])
                pskv = nf()
                mm(pskv, pskv.ap[:, :], [(ckvT[:, j, tsl], Wkv3[:, j, g * 512:(g + 1) * 512]) for j in range(2)], [Wb[3]] + Rb[3:5])
                kv3 = pskv.ap[:, :].rearrange("p (h d) -> p h d", h=4)
                kbb, kb_ = ns()
                kb3 = kb_.bitcast(BF16)[:, 0:384].rearrange("p (h d) -> p h d", h=4)
                ts('dve', kb3[:, :, 0:64], kv3[:, :, 0:64], rkv[:, t:t + 1], None, ALU.mult, None, [pskv, small], [kbb])
                ts('dve', v4[:, t, :, 0:64], kv3[:, :, 64:128], rkv[:, t:t + 1], None, ALU.mult, None, [pskv, small], Rb[13:16])
                cp(PREP_ENG, kb3[:, :, 64:96], ko3[:, t, None, :].broadcast_to([128, 4, 32]), [kob], [kbb])
                return qbb, qb3, kbb, kb3

            def prep_back(t, st_):
                qbb, qb3, kbb, kb3 = st_
                tsl = slice(t * 128, (t + 1) * 128)
                pb = nb()
                for hh in range(4):
                    tr(pb, pb.ap[0:96, hh * 128:(hh + 1) * 128], qb3[:, hh, :], identB.ap[:], [qbb, identB])
                cp('act', qT3[0:96, :, tsl], pb.ap[0:96, 0:512].rearrange("p (h n) -> p h n", h=4), [pb], Rb[5:9])
                pb = nb()
                for hh in range(4):
                    tr(pb, pb.ap[0:96, hh * 128:(hh + 1) * 128], kb3[:, hh, :], identB.ap[:], [kbb, identB])
                cp('act', kT3[0:96, :, tsl], pb.ap[0:96, 0:512].rearrange("p (h n) -> p h n", h=4), [pb], Rb[9:13])

            st_ = prep_front(0)
            for t in range(NT):
                nx_ = prep_front(t + 1) if t + 1 < NT else None
                prep_back(t, st_)
                st_ = nx_
            ctr['wide'] = False
            steps = [(hh, Q, kt) for hh in range(4) for Q in range(4) for kt in range(4 * Q + 4)]

            def qk_exp(hh, Q, kt):
                qlo = max(512 * Q, 128 * kt)
                n = 512 * (Q + 1) - qlo
                pss = nf()
                mm(pss, pss.ap[:, 0:n], [(kT3[0:96, hh, kt * 128:(kt + 1) * 128], qT3[0:96, hh, qlo:qlo + n])], [Rb[5 + hh], Rb[9 + hh]])
                ptb, pt_ = ns()
                pt = pt_.bitcast(BF16)[:, 0:512]
                act(pt[:, 0:n], pss.ap[:, 0:n], AF.Exp, [pss], [ptb])
                if kt >= 4 * Q:
                    P.op('pool', lambda e, pt=pt: e.memset(pt[64:128, 0:64], 0.0), [ptb], [ptb])
                return ptb, pt, qlo

            cur = qk_exp(*steps[0])
            pso = None
            for i_, (hh, Q, kt) in enumerate(steps):
                nxt = qk_exp(*steps[i_ + 1]) if i_ + 1 < len(steps) else None
                ptb, pt, qlo = cur
                if kt == 0:
                    ctr['z'] += 1
                    pso = psO[ctr['z'] % 2]

                def fn(e, pt=pt, kt=kt, Q=Q, qlo=qlo, hh=hh, pso=pso):
                    ins = None
                    for s_ in range((qlo - 512 * Q) // 128, 4):
                        off = 512 * Q + 128 * s_ - qlo
                        ins = e.matmul(pso.ap[:, s_ * 65:(s_ + 1) * 65], pt[:, off:off + 128], v4[:, kt, hh, 0:65],
                                       start=(kt == 0 and s_ == 0), stop=(kt == 4 * Q + s_), skip_group_check=True)
                    return ins
                P.op('pe', fn, [ptb] + Rb[13:16], [pso])
                if kt == 4 * Q + 3:
                    pso3 = pso.ap[:, 0:260].rearrange("p (s d) -> p s d", s=4)
                    rdb, rd_ = ns()
                    rd3 = rd_[:, 0:4].rearrange("p (s o) -> p s o", o=1)
                    P.op('dve', lambda e, rd3=rd3, pso3=pso3: e.reciprocal(rd3, pso3[:, :, 64:65]), [pso], [rdb])
                    hc = (4 * g + hh) * 64
                    tt('dve', o_tm[:, 4 * Q:4 * Q + 4, hc:hc + 64], pso3[:, :, 0:64], rd3.broadcast_to([128, 4, 64]), ALU.mult,
                       [pso, rdb], Mb[0:4])
                cur = nxt
        if tap == 'qk':
            P.dma('sp', dbg_buf, dbg_d[0], R_t[:, :], reads=Rb[0:16])
            P.dma('sp', dbg_buf, dbg_d[1, :, 0:8192], M_t[:, :].bitcast(F32), reads=Mb[0:4])
            break
        ctr['wide'] = True
        oT3 = Rv(0, 4, BF16).rearrange("p (c n) -> p c n", c=4)
        for t in range(NT):
            pb = nb()
            for c in range(4):
                tr(pb, pb.ap[:, c * 128:(c + 1) * 128], o_tm[:, t, c * 128:(c + 1) * 128], identB.ap[:], Mb[0:4] + [identB])
            cp('act', oT3[:, :, t * 128:(t + 1) * 128], pb.ap[:, 0:512].rearrange("p (c n) -> p c n", c=4), [pb], Rb[0:4])
        merge_branch(l, 2, oT3, Rb[0:4], mla_proj[l], True)
        if tap:
            P.dma('pool', dbg_buf, dbg_d[0], M_t[:, :], reads=Mb[0:4])

        W3 = Wv(3).rearrange("p (k n) -> p k n", k=8)
        wload([Wb[3]], W3, wsrc(l)[:, :, 0:512])
        pw3 = poolw_t.ap[:].rearrange("p (g d) -> p g d", g=4)
        P.dma('pool', poolw_t, pw3, pool_w[l].rearrange("g c d -> c g d"))
        sU = Rv(0, 3)[:, 0:2064]
        sB_ = Rv(3, 3)[:, 0:2064]
        sC = Rv(11, 3)[:, 0:2064]
        mixT = Rv(6, 1, BF16)
        pmT3 = Rv(7, 4, BF16).rearrange("p (c n) -> p c n", c=4)
        P.op('pool', lambda e: e.memset(sU[:, 0:16], 0.0), [], Rb[0:3])
        P.op('pool', lambda e: e.memset(sB_[:, 0:16], 0.0), [], Rb[3:6])
        P.op('pool', lambda e: e.memset(sC[:, 0:16], 0.0), [], Rb[11:14])
        for g in range(4):
            for T in range(4):
                ps = nf()
                mm(ps, ps.ap[:, :], [(W3[:, k, g * 128:(g + 1) * 128], hT3[:, k, T * 512:(T + 1) * 512]) for k in range(8)], [Wb[3]] + hTb[4 * T:4 * T + 4])
                cp('act', sU[:, 16 + T * 512:16 + (T + 1) * 512], ps.ap[:, :], [ps], Rb[0:3])
            src, srcb = sU, Rb[0:3]
            pp_ = [(sB_, Rb[3:6]), (sC, Rb[11:14])]
            for jj in range(g + 1):
                sh = 1 << jj
                dst, dstb = pp_[jj % 2]
                tt('dve', dst[:, 16:2064], src[:, 16:2064], src[:, 16 - sh:2064 - sh], ALU.add, srcb, dstb)
                src, srcb = dst, dstb
            w = 2 << g
            stt('dve', mixT[:, 0:2048], src[:, 16:2064], 1.0 / w, sU[:, 16:2064], ALU.mult, ALU.subtract, srcb + Rb[0:3], [Rb[6]])
            fb, f_ = ns()
            tt('dve', f_[:, 0:w - 1], src[:, 16:16 + w - 1], rcnt.ap[:, 0:w - 1], ALU.mult, srcb + [rcnt], [fb])
            tt('dve', mixT[:, 0:w - 1], f_[:, 0:w - 1], sU[:, 16:16 + w - 1], ALU.subtract, [fb] + Rb[0:3], [Rb[6]])
            for T in range(4):
                ps = nf()
                mm(ps, ps.ap[:, :], [(pw3[:, g, :], mixT[:, T * 512:(T + 1) * 512])], [poolw_t, Rb[6]])
                ts('dve', pmT3[:, g, T * 512:(T + 1) * 512], ps.ap[:, :], colA.ap[:, 24 + g:25 + g], None, ALU.mult, None, [ps, colA], [Rb[7 + g]])
        merge_branch(l, 0, pmT3, Rb[7:11], pool_proj[l], False)
        if tap:
            P.dma('pool', dbg_buf, dbg_d[1], M_t[:, :], reads=Mb[0:4])

        wload([Wb[3]], W3, wsrc(l)[:, :, 512:1024])
        W0c = Wv(0).rearrange("p (k n) -> p k n", k=8)
        wload([Wb[0]], W0c, wsrc(l)[:, :, 1024:1536])
        zcb = Rv(0, 2, BF16)[:, 0:2080]
        conv4 = Rv(3, 8).rearrange("p (c n) -> p c n", c=4)
        cact3 = Rv(11, 4, BF16).rearrange("p (c n) -> p c n", c=4)
        P.op('pool', lambda e: e.memset(zcb[:, 0:32], 0.0), [], Rb[0:2])
        for c in range(4):
            dslot = 1 + (c % 2)
            D3 = Wv(dslot).rearrange("p (k n) -> p k n", k=32)
            for k in range(31):
                ts('dve', D3[:, k, :], identB.ap[:], colB.ap[:, k * 4 + c:k * 4 + c + 1], None, ALU.mult, None, [identB, colB], [Wb[dslot]])
            for T in range(4):
                tsl = slice(T * 512, (T + 1) * 512)
                psa = nf()
                mm(psa, psa.ap[:, :], [(W3[:, k, c * 128:(c + 1) * 128], hT3[:, k, tsl]) for k in range(8)], [Wb[3]] + hTb[4 * T:4 * T + 4])
                psg = nf()
                mm(psg, psg.ap[:, :], [(W0c[:, k, c * 128:(c + 1) * 128], hT3[:, k, tsl]) for k in range(8)], [Wb[0]] + hTb[4 * T:4 * T + 4])
                sgb, sg = ns()
                act(sg, psg.ap[:, :], AF.Sigmoid, [psg], [sgb])
                tt('dve', zcb[:, 32 + T * 512:32 + (T + 1) * 512], psa.ap[:, :], sg, ALU.mult, [psa, sgb], Rb[0:2])
            for T in range(4):
                psc = nf()
                mm(psc, psc.ap[:, :], [(D3[:, k, :], zcb[:, 2 + k + T * 512:2 + k + (T + 1) * 512]) for k in range(31)], [Wb[dslot]] + Rb[0:2])
                ts('dve', conv4[:, c, T * 512:(T + 1) * 512], psc.ap[:, :], colA.ap[:, 28 + c:29 + c], None, ALU.add, None,
                   [psc, colA], [Rb[3 + 2 * c + T // 2]])
        for T in range(4):
            tsl = slice(T * 512, (T + 1) * 512)
            cbs = [[Rb[3 + 2 * c + T // 2]] for c in range(4)]
            ps1 = nf()
            mm(ps1, ps1.ap[:, :], [(onesF.ap[:], conv4[:, c, tsl]) for c in range(4)], [onesF] + sum(cbs, []))
            sqs = []
            for c in range(4):
                sqb, sq = ns()
                act(sq, conv4[:, c, tsl], AF.Square, cbs[c], [sqb])
                sqs.append((sqb, sq))
            ps2 = nf()
            mm(ps2, ps2.ap[:, :], [(onesF.ap[:], sq) for (_, sq) in sqs], [onesF] + [b_ for (b_, _) in sqs])
            mb_, m_ = ns()
            P.op('act', lambda e, m_=m_, ps1=ps1: e.mul(m_, ps1.ap[:, :], 1.0 / 512), [ps1], [mb_])
            qb_, msq = ns()
            tt('dve', msq, m_, m_, ALU.mult, [mb_], [qb_])
            vb_, var = ns()
            stt('dve', var, ps2.ap[:, :], 1.0 / 512, msq, ALU.mult, ALU.subtract, [ps2, qb_], [vb_])
            act(var, var, AF.Sqrt, [vb_, epsb], [vb_], bias=epsb.ap[:, 0:1])
            P.op('dve', lambda e, var=var: e.reciprocal(var, var), [vb_], [vb_])
            for c in range(4):
                t1b, t1 = ns()
                tt('dve', t1, conv4[:, c, tsl], m_, ALU.subtract, cbs[c] + [mb_], [t1b])
                tt('dve', t1, t1, var, ALU.mult, [t1b, vb_], [t1b])
                ts('dve', t1, t1, colA.ap[:, 32 + c:33 + c], colA.ap[:, 36 + c:37 + c], ALU.mult, ALU.add, [t1b, colA], [t1b])
                act(cact3[:, c, tsl], t1, AF.Silu, [t1b], [Rb[11 + c]])
        merge_branch(l, 1, cact3, Rb[11:15], conv_proj[l], False)
        if tap:
            P.dma('pool', dbg_buf, dbg_d[2], M_t[:, :], reads=Mb[0:4])

        Wo3 = Wv(0, 2).rearrange("p (k n) -> p k n", k=8)
        wload([Wb[0]], Wo3[:, 0:4, :], w_out[l].rearrange("(k p) n -> p k n", p=128)[:, 0:4, :])
        wload([Wb[1]], Wo3[:, 4:8, :], w_out[l].rearrange("(k p) n -> p k n", p=128)[:, 4:8, :])
        g_ap, b_ap, gbb = load_gb(ln1_g[l:l + 1, :], ln1_b[l:l + 1, :], 3)
        if tap == 'h32' and last:
            P.dma('sp', dbg_buf, dbg_d[0].rearrange("p (t d) -> p t d", t=16), h32_d.rearrange("(t p) d -> p t d", p=128), reads=[h32_buf])
            P.dma('sp', dbg_buf, dbg_d[1, :, 0:8192], hT_t[:, :].bitcast(F32), reads=hTb[0:16])
        def ln1_front(t):
            zi = t % NZ
            zbuf, z = ZL[zi]
            P.dma('sp', zbuf, z, h32_d[t * 128:(t + 1) * 128, :], reads=[h32_buf])
            for h in range(2):
                psy = nf()
                mm(psy, psy.ap[:, :], [(M3[:, k, t * 128:(t + 1) * 128], Wo3[:, k, h * 512:(h + 1) * 512]) for k in range(8)], [Wb[0], Wb[1], Mb[t // 4]])
                stt('dve', z[:, h * 512:(h + 1) * 512], z[:, h * 512:(h + 1) * 512], ALPHA, psy.ap[:, :], ALU.mult, ALU.add, [zbuf, psy], [zbuf])

        d1 = 'out' if (tap == 'ln1' and last) else ('acc+out' if (tap == 'ffn2' and last) else 'acc')
        ln1_front(0)
        ln1_front(1)
        ln_tile(0, 0, g_ap, b_ap, gbb, d1, part='a')
        for t in range(NT):
            if t + 2 < NT:
                ln1_front(t + 2)
            if t + 1 < NT:
                ln_tile(t + 1, (t + 1) % NZ, g_ap, b_ap, gbb, d1, part='a')
            ln_tile(t, t % NZ, g_ap, b_ap, gbb, d1, part='b')
        if tap == 'ln1' and last:
            break
        if tap == 'h32' and last and os.environ.get("BRK") == "ln1":
            break

        Wpg3 = Wv(0, 2).rearrange("p (k n) -> p k n", k=8)
        wload([Wb[0]], Wpg3[:, 0:4, :], ple_gate[l].rearrange("(k p) n -> p k n", p=128)[:, 0:4, :])
        wload([Wb[1]], Wpg3[:, 4:8, :], ple_gate[l].rearrange("(k p) n -> p k n", p=128)[:, 4:8, :])
        Wpp3 = Wv(3)[:, 0:2048].rearrange("p (c n) -> p c n", c=2)
        wload([Wb[3]], Wpp3, ple_proj[l].rearrange("(c p) n -> p c n", p=128))
        pT3 = Wv(2).rearrange("p (c n) -> p c n", c=2)
        for t in range(NT):
            sbf, sap = ns()
            pin = sap.bitcast(BF16)[:, 0:256]
            P.dma('pool', sbf, pin, p_d[l, t * 128:(t + 1) * 128, :])
            pb = nb()
            for c in range(2):
                tr(pb, pb.ap[:, c * 128:(c + 1) * 128], pin[:, c * 128:(c + 1) * 128], identB.ap[:], [sbf, identB])
            cp('act', pT3[:, :, t * 128:(t + 1) * 128], pb.ap[:, 0:256].rearrange("p (c n) -> p c n", c=2), [pb], [Wb[2]])
        psr = nf()
        wr3 = wr_t.ap[:].rearrange("p (k e) -> p k e", k=8)
        for t in range(NT):
            mm(psr, psr.ap[:, t * 16:(t + 1) * 16], [(hT3[:, k, t * 128:(t + 1) * 128], wr3[:, k, :]) for k in range(8)], [wr_t, hTb[t]])
        ab_, aff = ns(); aff = aff[:, 0:256]
        act(aff, psr.ap[:, 0:256], AF.Sigmoid, [psr], [ab_])
        sb2, sel = ns(); sel = sel[:, 0:256]
        tt('dve', sel.rearrange("p (t e) -> p t e", t=16), aff.rearrange("p (t e) -> p t e", t=16),
           rb_t.ap[:, None, :].broadcast_to([128, 16, 16]), ALU.add, [ab_, rb_t], [sb2])
        sel4 = sel.rearrange("p (g e) -> p g e", e=4)
        xb1, x1 = ns(); xb2, x2 = ns(); xb3, x3 = ns(); xb4, x4 = ns()
        m1 = x1[:, 0:64]; m2 = x1[:, 64:128]; gs_ = x1[:, 128:192]; gmx = x1[:, 192:208]; pen = x1[:, 256:320]
        e1 = x1[:, 320:336]; e2 = x1[:, 336:352]; wsum = x1[:, 352:368]
        eq = x2[:, 0:256]; sel2 = x3[:, 0:256]; mk = x4[:, 0:256]
        red(m1, sel4, ALU.max, [sb2], [xb1])
        tt('dve', eq.rearrange("p (g e) -> p g e", e=4), sel4, m1[:, :, None].broadcast_to([128, 64, 4]), ALU.is_equal, [sb2, xb1], [xb2])
        stt('dve', sel2, eq, -BIG, sel, ALU.mult, ALU.add, [xb2, sb2], [xb3])
        red(m2, sel2.rearrange("p (g e) -> p g e", e=4), ALU.max, [xb3], [xb1])
        tt('dve', gs_, m1, m2, ALU.add, [xb1], [xb1])
        red(gmx, gs_.rearrange("p (t g) -> p t g", g=4), ALU.max, [xb1], [xb1])
        tt('dve', pen.rearrange("p (t g) -> p t g", g=4), gs_.rearrange("p (t g) -> p t g", g=4),
           gmx[:, :, None].broadcast_to([128, 16, 4]), ALU.is_equal, [xb1], [xb1])
        ts('dve', pen, pen, BIG, -BIG, ALU.mult, ALU.add, [xb1], [xb1])
        tt('dve', sel2.rearrange("p (g e) -> p g e", e=4), sel4, pen[:, :, None].broadcast_to([128, 64, 4]), ALU.add, [sb2, xb1], [xb3])
        sel2_3 = sel2.rearrange("p (t e) -> p t e", t=16)
        red(e1, sel2_3, ALU.max, [xb3], [xb1])
        tt('dve', mk.rearrange("p (t e) -> p t e", t=16), sel2_3, e1[:, :, None].broadcast_to([128, 16, 16]), ALU.is_equal, [xb3, xb1], [xb4])
        stt('dve', eq, mk, -2 * BIG, sel2, ALU.mult, ALU.add, [xb4, xb3], [xb2])
        eq3 = eq.rearrange("p (t e) -> p t e", t=16)
        red(e2, eq3, ALU.max, [xb2], [xb1])
        tt('dve', sel2_3, eq3, e2[:, :, None].broadcast_to([128, 16, 16]), ALU.is_equal, [xb2, xb1], [xb3])
        tt('dve', mk, mk, sel2, ALU.add, [xb4, xb3], [xb4])
        mkb = x3[:, 256:384].bitcast(BF16)
        cp('dve', mkb, mk, [xb4], [xb3])
        w_ = eq
        tt('dve', w_, mk, aff, ALU.mult, [xb4, ab_], [xb2])
        P.op('dve', lambda e, wsum=wsum, w_=w_: e.tensor_reduce(wsum, w_.rearrange("p (t e) -> p t e", t=16), AX.X, ALU.add), [xb2], [xb1])
        P.op('dve', lambda e, wsum=wsum: e.reciprocal(wsum, wsum), [xb1], [xb1])
        tt('dve', w_.rearrange("p (t e) -> p t e", t=16), w_.rearrange("p (t e) -> p t e", t=16),
           wsum[:, :, None].broadcast_to([128, 16, 16]), ALU.mult, [xb2, xb1], [xb2])
        psR = nf()

        def fn_rank(e, mkb=mkb, psR=psR):
            ins = None
            first = True
            for i in range(NT):
                for i2 in range(i + 1):
                    ins = e.matmul(psR.ap[:, i * 16:(i + 1) * 16], (upperB.ap[:] if i2 == i else onesB.ap[:]), mkb[:, i2 * 16:(i2 + 1) * 16],
                                   start=first, stop=(i2 == i), skip_group_check=True)
                    first = False
            return ins
        P.op('pe', fn_rank, [xb3, onesB, upperB], [psR])
        xb5, x5 = ns(); xb6, x6 = ns()
        dst = x5[:, 0:256]; lt = x5[:, 256:512]; dm = x6[:, 0:256]; eqa = x6[:, 256:512]
        v3 = lambda a: a.rearrange("p (t e) -> p t e", t=16)
        ts('dve', lt, psR.ap[:, 0:256], float(CAP), None, ALU.is_lt, None, [psR], [xb5])
        tt('dve', v3(dst), v3(psR.ap[:, 0:256]), ecap_t.ap[:, None, :].broadcast_to([128, 16, 16]), ALU.add, [psR, ecap_t], [xb5])
        ts('dve', dst, dst, -float(NSLOT), None, ALU.add, None, [xb5], [xb5])
        tt('dve', dst, dst, lt, ALU.mult, [xb5], [xb5])
        ts('dve', dst, dst, float(NSLOT), None, ALU.add, None, [xb5], [xb5])
        ts('dve', dm, dst, -BIGV, None, ALU.add, None, [xb5], [xb6])
        tt('dve', dm, dm, mk, ALU.mult, [xb6, xb4], [xb6])
        ts('dve', dm, dm, BIGV, None, ALU.add, None, [xb6], [xb6])
        destA = wt_t.ap[:, 32:48]; destB = wt_t.ap[:, 48:64]; wA = wt_t.ap[:, 0:16]; wB = wt_t.ap[:, 16:32]
        idxA = wt_t.ap[:, 64:80].bitcast(I32); idxB = wt_t.ap[:, 80:96].bitcast(I32)
        P.op('dve', lambda e, dm=dm: e.tensor_reduce(destA, v3(dm), AX.X, ALU.min), [xb6], [wt_t])
        tt('dve', v3(eqa), v3(dm), destA[:, :, None].broadcast_to([128, 16, 16]), ALU.is_equal, [xb6, wt_t], [xb6])
        tt('dve', eqa, eqa, w_, ALU.mult, [xb6, xb2], [xb6])
        P.op('dve', lambda e, eqa=eqa: e.tensor_reduce(wA, v3(eqa), AX.X, ALU.add), [xb6], [wt_t])
        ts('dve', wB, wA, -1.0, 1.0, ALU.mult, ALU.add, [wt_t], [wt_t])
        ts('dve', dm, dst, 1.0, None, ALU.add, None, [xb5], [xb6])
        tt('dve', dm, dm, mk, ALU.mult, [xb6, xb4], [xb6])
        ts('dve', dm, dm, -1.0, None, ALU.add, None, [xb6], [xb6])
        P.op('dve', lambda e, dm=dm: e.tensor_reduce(destB, v3(dm), AX.X, ALU.max), [xb6], [wt_t])
        cp('dve', idxA, destA, [wt_t], [wt_t])
        cp('dve', idxB, destB, [wt_t], [wt_t])
        stage4 = [(HBb[0], HB_t[:, 0:1024]), (HBb[1], HB_t[:, 1024:2048]),
                  (Zb[0], Zs[0][:, 0:512].bitcast(BF16)), (Zb[1], Zs[1][:, 0:512].bitcast(BF16))]
        for t in range(NT):
            sbuf_, hbt = stage4[t % 4]
            P.dma('sp', sbuf_, hbt, hbf_d[t * 128:(t + 1) * 128, :], reads=[hbf_bufs[t]])
            for ix in (idxA, idxB):
                P.dma_fn('pool', xs_bufs[t], lambda e, ix=ix, t=t, hbt=hbt: e.indirect_dma_start(
                    out=xs_d[:, :], out_offset=bass.IndirectOffsetOnAxis(ap=ix[:, t:t + 1], axis=0),
                    in_=hbt, in_offset=None), reads=[sbuf_, wt_t])

        for t in range(NT):
            tsl = slice(t * 128, (t + 1) * 128)
            for h in range(2):
                hsl = slice(h * 512, (h + 1) * 512)
                pse = nf()
                mm(pse, pse.ap[:, :], [(pT3[:, c, tsl], Wpp3[:, c, hsl]) for c in range(2)], [Wb[2], Wb[3]])
                psg = nf()
                mm(psg, psg.ap[:, :], [(hT3[:, k, tsl], Wpg3[:, k, hsl]) for k in range(8)], [Wb[0], Wb[1], hTb[t]])
                sgb, sg = ns()
                act(sg, psg.ap[:, :], AF.Sigmoid, [psg], [sgb])
                tb_, tmp = ns()
                tt('dve', tmp, pse.ap[:, :], sg, ALU.mult, [pse, sgb], [tb_])
                accv = R_t[:, t * 1024 + h * 512:t * 1024 + (h + 1) * 512]
                tt('dve', accv, accv, tmp, ALU.add, [Rb[t], tb_], [Rb[t]])

        def wset(e_):
            if e_ % 2 == 0:
                return (Wv(0, 2).rearrange("p (k n) -> p k n", k=8), Wv(2).rearrange("p (k n) -> p k n", k=4),
                        [Wb[0], Wb[1]], [Wb[2]])
            return (M_t[:, 0:8192].rearrange("p (k n) -> p k n", k=8), M_t[:, 8192:12288].rearrange("p (k n) -> p k n", k=4),
                    Mb[0:4], Mb[0:4])

        def wfetch(e_):
            Wu3, Wd3, ub, db = wset(e_)
            src = exp_w_up[l, e_].rearrange("(k p) n -> p k n", p=128)
            if e_ % 2 == 0:
                wload([Wb[0]], Wu3[:, 0:4, :], src[:, 0:4, :])
                wload([Wb[1]], Wu3[:, 4:8, :], src[:, 4:8, :])
                wload([Wb[2]], Wd3, exp_w_down[l, e_].rearrange("(k p) n -> p k n", p=128))
            else:
                wload(Mb[0:4], Wu3, src)
                wload(Mb[0:4], Wd3, exp_w_down[l, e_].rearrange("(k p) n -> p k n", p=128))
        ctr['smin'] = 4
        xsT_views = [S_t[:, 0:2048].bitcast(BF16).rearrange("p (k n) -> p k n", k=8),
                     M_t[:, 12288:16384].rearrange("p (k n) -> p k n", k=8)]
        XB = Buf("XB")
        ysl = Wv(3).bitcast(F32)
        land4 = [(HBb[0], HB_t[:, 0:1024]), (HBb[1], HB_t[:, 1024:2048]),
                 (Zb[0], Zs[0][:, 0:512].bitcast(BF16)), (Zb[1], Zs[1][:, 0:512].bitcast(BF16))]

        def prep_load(e_):
            for blk in range(4):
                lb, lap = land4[blk]
                r0 = e_ * CAP + blk * 128
                P.dma('sp', lb, lap, xs_d[r0:r0 + 128, :], reads=xs_bufs)

        def prep_T(e_):
            xsT3 = xsT_views[e_ % 2]
            xbufs = Sb[0:4] if e_ % 2 == 0 else [XB]
            for blk in range(4):
                lb, lap = land4[blk]
                pb = nb()
                for j in range(8):
                    tr(pb, pb.ap[:, j * 128:(j + 1) * 128], lap[:, j * 128:(j + 1) * 128], identB.ap[:], [lb, identB])
                cp('dve', xsT3[:, :, blk * 128:(blk + 1) * 128], pb.ap[:].rearrange("p (k n) -> p k n", k=8), [pb],
                   xbufs + (Mb[0:4] if (e_ == 1 and blk == 0) else []))

        def compute_up(e_):
            Wu3, Wd3, ub, db = wset(e_)
            xsT3 = xsT_views[e_ % 2]
            xbufs = Sb[0:4] if e_ % 2 == 0 else [XB]
            ai = e_ % 2
            aT3 = actT_t.ap[:, ai * 2048:(ai + 1) * 2048].rearrange("p (j n) -> p j n", j=4)
            for j in range(4):
                psg = nf()
                mm(psg, psg.ap[:, :], [(Wu3[:, k, j * 128:(j + 1) * 128], xsT3[:, k, :]) for k in range(8)], ub + xbufs)
                psu = nf()
                mm(psu, psu.ap[:, :], [(Wu3[:, k, 512 + j * 128:512 + (j + 1) * 128], xsT3[:, k, :]) for k in range(8)], ub + xbufs)
                sgb, sg_ = ns()
                sg = sg_.bitcast(BF16)[:, 0:512]
                act(sg, psg.ap[:, :], AF.Silu, [psg], [sgb])
                tt('dve', aT3[:, j, :], psu.ap[:, :], sg, ALU.mult, [psu, sgb], [actTb[ai]])

        def compute_down(e_):
            Wu3, Wd3, ub, db = wset(e_)
            ai = e_ % 2
            aT3 = actT_t.ap[:, ai * 2048:(ai + 1) * 2048].rearrange("p (j n) -> p j n", j=4)
            for s_ in range(4):
                yi = s_ % 2
                first_ = (e_ == 0 and s_ < 2)
                final_ = (e_ == 15 and s_ >= 2)
                for h in range(2):
                    psd = nf()
                    mm(psd, psd.ap[:, :], [(aT3[:, j, s_ * 128:(s_ + 1) * 128], Wd3[:, j, h * 512:(h + 1) * 512]) for j in range(4)], db + [actTb[ai]])
                    cp('act', ysl[:, yi * 1024 + h * 512:yi * 1024 + (h + 1) * 512], psd.ap[:, :], [psd], [YSb[yi]] + ([Wb[3]] if first_ else []))
                r0 = e_ * CAP + s_ * 128
                P.dma('act', ys_bufs[e_], ys_d[r0:r0 + 128, :], ysl[:, yi * 1024:(yi + 1) * 1024], reads=[YSb[yi]] + ([Wb[3]] if final_ else []))

        wfetch(0)
        wfetch(1)
        prep_load(0)
        prep_T(0)
        for e_ in range(16):
            if e_ + 1 < 16:
                prep_load(e_ + 1)
            compute_up(e_)
            if e_ + 1 < 16:
                prep_T(e_ + 1)
            compute_down(e_)
            if e_ + 2 < 16:
                wfetch(e_ + 2)
        if not last:
            layer_prologue(l + 1)
        g_ap, b_ap, gbb = load_gb(ln2_g[l:l + 1, :], ln2_b[l:l + 1, :], 3)
        land = [(Zb[0], Zs[0][:, :]), (Zb[1], Zs[1][:, :])]
        for t in range(NT):
            accv = R_t[:, t * 1024:(t + 1) * 1024]
            for q_, (ix, wv) in enumerate(((idxA, wA), (idxB, wB))):
                lb, lap = land[q_]
                P.dma_fn('pool', lb, lambda e, lap=lap, ix=ix, t=t: e.indirect_dma_start(
                    out=lap, out_offset=None, in_=ys_d[:, :],
                    in_offset=bass.IndirectOffsetOnAxis(ap=ix[:, t:t + 1], axis=0)),
                    reads=ys_bufs + [wt_t])
                stt('dve', accv, lap, wv[:, t:t + 1], accv, ALU.mult, ALU.add, [lb, wt_t, Rb[t]], [Rb[t]])
            zi = 2 + t % 2
            d2 = 'out' if (last and tap != 'ffn2') else 'h32'
            cp('act', ZL[zi][1], accv, [Rb[t]], [ZL[zi][0]])
            ln_tile(t, zi, g_ap, b_ap, gbb, d2, part='a')
            for tb in ([t - 1] if t >= 1 else []) + ([t] if t == NT - 1 else []):
                ln_tile(tb, 2 + tb % 2, g_ap, b_ap, gbb, d2, part='b')
                if (not last) and tb in (4, 7, 11, 15):
                    proj_T(l + 1, (4, 7, 11, 15).index(tb))
        ctr['smin'] = 0

    P.finish([out_buf] + ([dbg_buf] if tap else []))
    return nc


def make_in_maps(inp, cores=range(8)):
    f = lambda a: np.ascontiguousarray(np.asarray(a, dtype=np.float32))
    invf = (10000.0 ** (-np.arange(0, 32, 2, dtype=np.float32) / 32)).astype(np.float32)
    shared = {
        "invf": np.ascontiguousarray(np.broadcast_to(invf[None, :], (128, 16))),
        "ecap": np.ascontiguousarray(np.broadcast_to((np.arange(16, dtype=np.float32) * CAP)[None, :], (128, 16))),
        "rcnt": np.ascontiguousarray(np.broadcast_to((1.0 / np.arange(1, 17, dtype=np.float32))[None, :], (128, 16))),
        "ln_in_g": f(inp["ln_in_g"]).reshape(1, D), "ln_in_b": f(inp["ln_in_b"]).reshape(1, D),
        "w_in": f(inp["w_in"]), "b_gate": f(inp["b_gate"]).reshape(4, 24, 128),
        "pool_w": f(inp["pool_w"]), "pool_scale": f(inp["pool_scale"]).reshape(4, 4, 128),
        "pool_proj": f(inp["pool_proj"]), "conv_dw": f(inp["conv_dw"]).reshape(4, 124, 128),
        "conv_b": f(inp["conv_b"]).reshape(4, 4, 128), "conv_ln_g": f(inp["conv_ln_g"]).reshape(4, 4, 128),
        "conv_ln_b": f(inp["conv_ln_b"]).reshape(4, 4, 128), "conv_proj": f(inp["conv_proj"]),
        "q_norm_g": f(inp["q_norm_g"]).reshape(4, 3, 128), "w_uq": f(inp["w_uq"]),
        "kv_norm_g": f(inp["kv_norm_g"]).reshape(4, 2, 128), "w_ukv": f(inp["w_ukv"]),
        "mla_proj": f(inp["mla_proj"]), "w_out": f(inp["w_out"]), "ln1_g": f(inp["ln1_g"]), "ln1_b": f(inp["ln1_b"]),
        "w_router": f(inp["w_router"]), "router_bias": f(inp["router_bias"]).reshape(1, 16),
        "exp_w_up": f(inp["exp_w_up"]), "exp_w_down": f(inp["exp_w_down"]),
        "ple_proj": f(inp["ple_proj"]), "ple_gate": f(inp["ple_gate"]), "ln2_g": f(inp["ln2_g"]), "ln2_b": f(inp["ln2_b"]),
    }
    maps = []
    for c in cores:
        m = dict(shared)
        m["x"] = f(inp["x"][c])
        m["p"] = f(inp["p"][:, c])
        m["pos"] = np.ascontiguousarray(np.asarray(inp["positions"][c], dtype=np.int32).reshape(16, 128))
        maps.append(m)
    return maps


_NC_CACHE = {}


def kernel(**inputs):
    if 'nc' not in _NC_CACHE:
        _NC_CACHE['nc'] = build_nc()
    nc = _NC_CACHE['nc']
    maps = make_in_maps(inputs)
    res = run_bass_kernel_spmd(nc, maps, core_ids=list(range(8)))
    return np.stack([np.asarray(r["out"], dtype=np.float32) for r in res.results], axis=0)
```

```python
import os
import numpy as np
import concourse.bass as bass
import concourse.mybir as mybir
from concourse.bass_utils import run_bass_kernel_spmd
from contextlib import ExitStack

F32 = mybir.dt.float32
BF16 = mybir.dt.bfloat16
I32 = mybir.dt.int32
AF = mybir.ActivationFunctionType
ALU = mybir.AluOpType
AX = mybir.AxisListType

ENGS = ('pe', 'act', 'dve', 'pool', 'sp')


class Buf:
    def __init__(self, name, ap=None, semkey=None):
        self.name = name
        self.ap = ap
        self.last_write = None
        self.readers = []
        self.semkey = semkey if semkey is not None else ('b', name)


class Prog:
    def __init__(self, nc):
        self.nc = nc
        self.stack = ExitStack()
        self.ops = {e: [] for e in ENGS}
        self.cnt = {}
        self.seen = {e: {} for e in ENGS}
        self.nops = 0

    def sb(self, name, shape, dtype):
        t = self.stack.enter_context(self.nc.sbuf_tensor("sb_" + name, list(shape), dtype))
        return Buf(name, t)

    def ps(self, name, shape, dtype=F32):
        t = self.stack.enter_context(self.nc.psum_tensor("ps_" + name, list(shape), dtype))
        return Buf(name, t)

    def dram(self, name, ap=None, semkey=None):
        return Buf(name, ap, semkey)

    def view(self, name, ap, semkey=None):
        return Buf(name, ap, semkey)

    def _waits(self, eng, reads, writes):
        need = {}

        def add(tok, war):
            if tok is None:
                return
            k, v = tok
            if k[0] == 'b':
                v = self.cnt.get(k, 0)
            elif k == ('e', eng):
                if eng == 'pe' or war:
                    return
            if need.get(k, 0) < v:
                need[k] = v

        for b in reads:
            add(b.last_write, False)
        for b in writes:
            add(b.last_write, False)
            for t in b.readers:
                add(t, True)
        waits = []
        for k, v in need.items():
            if self.seen[eng].get(k, 0) < v:
                self.seen[eng][k] = v
                waits.append((k, v))
        return waits

    def _commit(self, tok, reads, writes):
        for b in writes:
            b.last_write = tok
            b.readers = []
        for b in reads:
            if b not in writes:
                b.readers.append(tok)

    def op(self, eng, fn, reads=(), writes=()):
        waits = self._waits(eng, reads, writes)
        k = ('e', eng)
        self.cnt[k] = self.cnt.get(k, 0) + 1
        tok = (k, self.cnt[k])
        self.ops[eng].append((waits, fn, k, 1))
        self._commit(tok, reads, writes)
        self.nops += 1

    def dma(self, q, dst, out_ap, in_ap, reads=(), extra_writes=()):
        writes = [dst] + list(extra_writes)
        waits = self._waits(q, reads, writes)
        k = dst.semkey
        self.cnt[k] = self.cnt.get(k, 0) + 16
        tok = (k, self.cnt[k])
        self.ops[q].append((waits, lambda e: e.dma_start(out=out_ap, in_=in_ap), k, 16))
        self._commit(tok, reads, writes)
        self.nops += 1

    def dma_fn(self, q, dst, fn, reads=(), extra_writes=()):
        writes = [dst] + list(extra_writes)
        waits = self._waits(q, reads, writes)
        k = dst.semkey
        self.cnt[k] = self.cnt.get(k, 0) + 16
        tok = (k, self.cnt[k])
        self.ops[q].append((waits, fn, k, 16))
        self._commit(tok, reads, writes)
        self.nops += 1

    def finish(self, outs):
        waits = self._waits('sp', outs, [])
        self.ops['sp'].append((waits, None, None, 0))
        nc = self.nc
        sems = {}
        for k in self.cnt:
            sems[k] = self.stack.enter_context(nc.semaphore("s_%s_%s" % k))
        ops = self.ops
        with nc.Block() as block:
            def mk(engname):
                def body(e):
                    for waits, fn, k, inc in ops[engname]:
                        for (wk, wv) in waits:
                            e.wait_ge(sems[wk], wv)
                        if fn is not None:
                            fn(e).then_inc(sems[k], inc)
                return body
            block.tensor(mk('pe'))
            block.scalar(mk('act'))
            block.vector(mk('dve'))
            block.gpsimd(mk('pool'))
            block.sync(mk('sp'))
        self.stack.close()


S = 2048
D = 1024
NT = 16
ALPHA = 8.0 ** 0.25
BIG = 100.0
CAP = 512
NSLOT = 16 * CAP
BIGV = 1.0e6
MERGE_ENG = os.environ.get('MERGE_ENG', 'dve')
LN_ENG = os.environ.get('LN_ENG', 'dve')
PREP_ENG = os.environ.get('PREP_ENG', 'dve')
LNQ = os.environ.get('LNQ', 'act')


def build_nc(n_layers=4, tap=None):
    nc = bass.Bass("TRN2", target_bir_lowering=False)

    def din(name, shape, dtype=F32):
        return nc.dram_tensor(name, list(shape), dtype, kind="ExternalInput").ap()

    x_d = din("x", [S, D]); p_d = din("p", [4, S, 256]); pos_d = din("pos", [16, 128], I32)
    invf_d = din("invf", [128, 16]); rcnt_d = din("rcnt", [128, 16]); ecap_d = din("ecap", [128, 16])
    ln_in_g = din("ln_in_g", [1, D]); ln_in_b = din("ln_in_b", [1, D])
    w_in = din("w_in", [4, D, 5280]); b_gate = din("b_gate", [4, 24, 128])
    pool_w = din("pool_w", [4, 4, 128, 128]); pool_scale = din("pool_scale", [4, 4, 128])
    pool_proj = din("pool_proj", [4, 512, D]); conv_dw = din("conv_dw", [4, 124, 128])
    conv_b = din("conv_b", [4, 4, 128]); conv_ln_g = din("conv_ln_g", [4, 4, 128]); conv_ln_b = din("conv_ln_b", [4, 4, 128])
    conv_proj = din("conv_proj", [4, 512, D]); q_norm_g = din("q_norm_g", [4, 3, 128]); w_uq = din("w_uq", [4, 384, 768])
    kv_norm_g = din("kv_norm_g", [4, 2, 128]); w_ukv = din("w_ukv", [4, 256, 1024]); mla_proj = din("mla_proj", [4, 512, D])
    w_out = din("w_out", [4, D, D]); ln1_g = din("ln1_g", [4, D]); ln1_b = din("ln1_b", [4, D])
    w_router = din("w_router", [D, 16]); router_bias = din("router_bias", [1, 16])
    exp_w_up = din("exp_w_up", [4, 16, D, 1024]); exp_w_down = din("exp_w_down", [4, 16, 512, D])
    ple_proj = din("ple_proj", [4, 256, D]); ple_gate = din("ple_gate", [4, D, D])
    ln2_g = din("ln2_g", [4, D]); ln2_b = din("ln2_b", [4, D])
    out_d = nc.dram_tensor("out", [S, D], F32, kind="ExternalOutput").ap()
    h32_d = nc.dram_tensor("h32_d", [S, D], F32, kind="Internal").ap()
    xs_d = nc.dram_tensor("xs_d", [NSLOT + 1, D], BF16, kind="Internal").ap()
    hbf_d = nc.dram_tensor("hbf_d", [S, D], BF16, kind="Internal").ap()
    ys_d = nc.dram_tensor("ys_d", [NSLOT + 1, D], F32, kind="Internal").ap()
    dbg_d = nc.dram_tensor("dbg", [3, 128, 8 * S], F32, kind="ExternalOutput").ap() if tap else None
    dbg_buf = Buf("dbg", semkey=('b', 'dbg'))

    P = Prog(nc)
    hT_t = P.sb("hT", [128, 8 * S], BF16).ap
    hT3 = hT_t[:].rearrange("p (k n) -> p k n", k=8)
    hTb = [Buf("hT%d" % t) for t in range(NT)]
    M_t = P.sb("M", [128, 8 * S], BF16).ap
    M3 = M_t[:].rearrange("p (k n) -> p k n", k=8)
    Mb = [Buf("M%d" % i, semkey=('b', 'wB')) for i in range(4)]
    R_t = P.sb("R", [128, 16 * 1024], F32).ap
    Rb = [Buf("R%d" % i) for i in range(16)]
    W_t = P.sb("W", [128, 4 * 4096], BF16).ap
    Wb = [Buf("W%d" % i, semkey=('b', 'w%d' % i)) for i in range(4)]
    S_t = P.sb("Sx", [128, 8 * 512], F32).ap
    Sb = [Buf("S%d" % i) for i in range(8)]
    Zs = [P.sb("Z0", [128, 1024], F32).ap, P.sb("Z1", [128, 1024], F32).ap]
    Zb = [Buf("Z%d" % i, semkey=('b', 'z%d' % i)) for i in range(2)]
    HB_t = P.sb("HB", [128, 2 * 1024], BF16).ap
    HBb = [Buf("HB%d" % i) for i in range(2)]
    identF = P.sb("identF", [128, 128], F32); identB = P.sb("identB", [128, 128], BF16)
    onesF = P.sb("onesF", [128, 128], F32)
    epsb = P.sb("epsb", [128, 2], F32)
    cosT = P.sb("cosT", [128, 256], F32); sinT = P.sb("sinT", [128, 256], F32)
    wr_t = P.sb("wr", [128, 8 * 16], BF16); rb_t = P.sb("rb", [128, 16], F32)
    rcnt = P.sb("rcnt", [128, 16], F32)
    colsrcA = P.sb("colsrcA", [48, 128], F32); colsrcB = P.sb("colsrcB", [124, 128], F32)
    colsrcA.semkey = colsrcB.semkey = ('b', 'cols')
    colA = P.sb("colA", [128, 48], F32); colB = P.sb("colB", [128, 124], F32)
    poolw_t = P.sb("poolw", [128, 4 * 128], BF16)
    st_t = P.sb("st", [128, 4 * 12], F32); mv_t = P.sb("mv", [128, 4 * 2], F32); sd_t = P.sb("sd", [128, 4 * 2], F32)
    stb = [Buf("st%d" % i) for i in range(4)]
    small = P.sb("small", [128, 64], F32)
    actT_t = P.sb("actT", [128, 2 * 2048], BF16)
    actTb = [Buf("actT%d" % i) for i in range(2)]
    ZL = [(Zb[0], Zs[0][:, :]), (Zb[1], Zs[1][:, :]),
          (actTb[0], actT_t.ap[:, 0:2048].bitcast(F32)), (actTb[1], actT_t.ap[:, 2048:4096].bitcast(F32))]
    NZ = 4
    onesB = P.sb("onesB", [128, 128], BF16)
    upperB = P.sb("upperB", [128, 128], BF16)
    ecap_t = P.sb("ecap", [128, 16], F32)
    xs_bufs = [Buf("xs%d" % i, semkey=('b', 'xsd')) for i in range(NT)]
    hbf_bufs = [Buf("hbf%d" % i, semkey=('b', 'hbfd')) for i in range(NT)]
    ys_bufs = [Buf("ys%d" % i, semkey=('b', 'ysd')) for i in range(16)]
    YSb = [Buf("YS%d" % i, semkey=('b', 'ysl%d' % i)) for i in range(2)]
    psF = [P.ps("psF%d" % i, [128, 512], F32) for i in range(4)]
    psB = [P.ps("psB%d" % i, [128, 1024], BF16) for i in range(2)]
    ctr = {'f': 0, 'b': 0, 's': 0, 'z': 0}

    def nf():
        ctr['f'] += 1
        banks = psF + ([psO[1]] if ctr.get('wide', False) else []) + ([psO[0]] if ctr.get('wide2', False) else [])
        return banks[ctr['f'] % len(banks)]

    def nb():
        ctr['b'] += 1
        return psB[ctr['b'] % 2]

    def ns():
        ctr['s'] += 1
        lo = ctr.get('smin', 0)
        i = lo + ctr['s'] % (8 - lo)
        return Sb[i], S_t[:, i * 512:(i + 1) * 512]

    def mm(ps, out_ap, terms, reads):
        def fn(e):
            n = len(terms)
            ins = None
            for i, (a, b) in enumerate(terms):
                ins = e.matmul(out_ap, a, b, start=(i == 0), stop=(i == n - 1))
            return ins
        P.op('pe', fn, reads=reads, writes=[ps])

    def tr(ps, out_ap, in_ap, ident_ap, reads, start=True):
        P.op('pe', lambda e: e.transpose(out_ap, in_ap, ident_ap), reads=reads, writes=[ps])

    def act(out, in_, func, reads, writes, bias=None, scale=1.0):
        if bias is None:
            P.op('act', lambda e: e.activation(out, in_, func, scale=scale), reads, writes)
        else:
            P.op('act', lambda e: e.activation(out, in_, func, bias=bias, scale=scale), reads, writes)

    def tt(eng, out, a, b, op, reads, writes):
        P.op(eng, lambda e: e.tensor_tensor(out, a, b, op), reads, writes)

    def ts(eng, out, a, s1, s2, op0, op1, reads, writes):
        if op1 is None:
            P.op(eng, lambda e: e.tensor_scalar(out, a, s1, None, op0), reads, writes)
        else:
            P.op(eng, lambda e: e.tensor_scalar(out, a, s1, s2, op0, op1), reads, writes)

    def stt(eng, out, a, s, b, op0, op1, reads, writes):
        P.op(eng, lambda e: e.scalar_tensor_tensor(out, a, s, b, op0, op1), reads, writes)

    def cp(eng, out, in_, reads, writes):
        if eng == 'act':
            P.op('act', lambda e: e.copy(out, in_), reads, writes)
        else:
            P.op(eng, lambda e: e.tensor_copy(out, in_), reads, writes)

    def red(out, in_, op, reads, writes):
        P.op('dve', lambda e: e.tensor_reduce(out, in_, AX.X, op), reads, writes)

    def wload(bufs, dst_ap, src_ap, q='pool'):
        P.dma(q, bufs[0], dst_ap, src_ap, extra_writes=bufs[1:])

    def Wv(slot, n=1):
        return W_t[:, slot * 4096:(slot + n) * 4096]

    def Rv(i0, n, dtype=F32):
        v = R_t[:, i0 * 1024:(i0 + n) * 1024]
        return v.bitcast(BF16) if dtype == BF16 else v

    taps = {}

    P.op('pool', lambda e: e.memset(identF.ap[:], 1.0), writes=[identF])
    P.op('pool', lambda e: e.affine_select(identF.ap[:], identF.ap[:], pattern=[[-1, 128]], compare_op=ALU.is_equal,
                                           fill=0.0, base=0, channel_multiplier=1), reads=[identF], writes=[identF])
    cp('dve', identB.ap[:], identF.ap[:], [identF], [identB])
    P.op('pool', lambda e: e.memset(onesF.ap[:], 1.0), writes=[onesF])
    P.op('pool', lambda e: e.memset(epsb.ap[:, 0:1], 1e-5), writes=[epsb])
    P.op('pool', lambda e: e.memset(epsb.ap[:, 1:2], 1e-6), reads=[], writes=[epsb])
    P.dma('sp', rcnt, rcnt.ap[:], rcnt_d[:, :])
    P.dma('sp', ecap_t, ecap_t.ap[:], ecap_d[:, :])
    P.op('pool', lambda e: e.memset(onesB.ap[:], 1.0), writes=[onesB])
    P.op('pool', lambda e: e.memset(upperB.ap[:], 1.0), writes=[upperB])
    P.op('pool', lambda e: e.affine_select(upperB.ap[:], upperB.ap[:], pattern=[[1, 128]], compare_op=ALU.is_gt,
                                           fill=0.0, base=0, channel_multiplier=-1), reads=[upperB], writes=[upperB])
    P.op('pool', lambda e: e.memset(Zs[0][0:1, :], 0.0), writes=[Zb[0]])
    P.dma('sp', ys_bufs[0], ys_d[NSLOT:NSLOT + 1, :], Zs[0][0:1, :], reads=[Zb[0]])
    P.dma('sp', rb_t, rb_t.ap[:], router_bias[0:1, :].broadcast_to([128, 16]))
    P.dma('pool', wr_t, wr_t.ap[:].rearrange("p (k e) -> p k e", k=8), w_router.rearrange("(k p) e -> p k e", p=128))
    pi_b, pi_ap = ns()
    P.dma('sp', pi_b, pi_ap[0:16, 0:128].bitcast(I32), pos_d[:, :])
    pf_b, pf_ap = ns()
    cp('dve', pf_ap[0:16, 0:128], pi_ap[0:16, 0:128].bitcast(I32), [pi_b], [pf_b])
    pp = nf()
    tr(pp, pp.ap[:, 0:16], pf_ap[0:16, 0:128], identF.ap[0:16, 0:16], [pf_b, identF])
    post_b, post_ap = ns()
    cp('dve', post_ap[:, 0:16], pp.ap[:, 0:16], [pp], [post_b])
    invf_b, invf_ap = ns()
    P.dma('sp', invf_b, invf_ap[:, 0:16], invf_d[:, :])
    ang_b, ang_ap = ns()
    ang3 = ang_ap[:, 0:256].rearrange("p (t i) -> p t i", t=16)
    tt('dve', ang3, post_ap[:, 0:16, None].broadcast_to([128, 16, 16]), invf_ap[:, None, 0:16].broadcast_to([128, 16, 16]),
       ALU.mult, [post_b, invf_b], [ang_b])

    def sin_of(dst, shift):
        kf_b, kf_ap = ns()
        ki_b, ki_ap = ns()
        a_b, a_ap = ns()
        kf, ki, a = kf_ap[:, 0:256], ki_ap[:, 0:256].bitcast(I32), a_ap[:, 0:256]
        ts('dve', a, ang_ap[:, 0:256], shift, None, ALU.add, None, [ang_b], [a_b])
        ts('dve', kf, a, 1.0 / (2 * np.pi), None, ALU.mult, None, [a_b], [kf_b])
        cp('dve', ki, kf, [kf_b], [ki_b])
        cp('dve', kf, ki, [ki_b], [kf_b])
        stt('dve', a, kf, -2 * np.pi, a, ALU.mult, ALU.add, [a_b, kf_b], [a_b])
        ts('dve', kf, a, np.pi, -2 * np.pi, ALU.is_gt, ALU.mult, [a_b], [kf_b])
        tt('dve', a, a, kf, ALU.add, [a_b, kf_b], [a_b])
        ts('dve', kf, a, -np.pi, 2 * np.pi, ALU.is_lt, ALU.mult, [a_b], [kf_b])
        tt('dve', a, a, kf, ALU.add, [a_b, kf_b], [a_b])
        act(dst.ap[:], a, AF.Sin, [a_b], [dst])
    sin_of(sinT, 0.0)
    sin_of(cosT, np.pi / 2)
    cos3 = cosT.ap[:].rearrange("p (t i) -> p t i", t=16)
    sin3 = sinT.ap[:].rearrange("p (t i) -> p t i", t=16)

    def ln_tile(t, zi, g_ap, b_ap, gb_bufs, dest, part='ab'):
        zb, z = ZL[zi]
        st = st_t.ap[:, zi * 12:(zi + 1) * 12]
        mv = mv_t.ap[:, zi * 2:(zi + 1) * 2]
        sd = sd_t.ap[:, zi * 2:(zi + 1) * 2]
        sb_ = stb[zi]
        if 'a' in part:
            P.op('dve', lambda e: e.bn_stats(st[:, 0:6], z[:, 0:512]), [zb], [sb_])
            P.op('dve', lambda e: e.bn_stats(st[:, 6:12], z[:, 512:1024]), [zb, sb_], [sb_])
            P.op('dve', lambda e: e.bn_aggr(mv, st), [sb_], [sb_])
            act(sd[:, 0:1], mv[:, 1:2], AF.Sqrt, [sb_, epsb], [sb_], bias=epsb.ap[:, 0:1])
        if 'b' not in part:
            return
        P.op('dve', lambda e: e.reciprocal(sd[:, 1:2], sd[:, 0:1]), [sb_], [sb_])
        ts('dve', z, z, mv[:, 0:1], sd[:, 1:2], ALU.subtract, ALU.mult, [zb, sb_], [zb])
        tt(LN_ENG, z, z, g_ap, ALU.mult, [zb] + gb_bufs, [zb])
        tt(LN_ENG, z, z, b_ap, ALU.add, [zb] + gb_bufs, [zb])
        hbb, hb = HBb[zi % 2], HB_t[:, (zi % 2) * 1024:((zi % 2) + 1) * 1024]
        cp('act', hb, z, [zb], [hbb])
        pb = nb()
        for j in range(8):
            tr(pb, pb.ap[:, j * 128:(j + 1) * 128], hb[:, j * 128:(j + 1) * 128], identB.ap[:], [hbb, identB])
        cp('dve', hT3[:, :, t * 128:(t + 1) * 128], pb.ap[:].rearrange("p (k n) -> p k n", k=8), [pb], [hTb[t]])
        if dest == 'h32':
            P.dma('sp', h32_buf, h32_d[t * 128:(t + 1) * 128, :], z, reads=[zb])
        elif dest == 'out':
            P.dma('sp', out_buf, out_d[t * 128:(t + 1) * 128, :], z, reads=[zb])
        else:
            P.op('act', lambda e: e.mul(R_t[:, t * 1024:(t + 1) * 1024], z, ALPHA), [zb], [Rb[t]])
            P.dma(LNQ, hbf_bufs[t], hbf_d[t * 128:(t + 1) * 128, :], hb, reads=[hbb])
            if dest == 'acc+out':
                P.dma('sp', out_buf, out_d[t * 128:(t + 1) * 128, :], z, reads=[zb])

    out_buf = Buf("outd", semkey=('b', 'outd'))
    h32_buf = Buf("h32d_all", semkey=('b', 'h32d'))

    def load_gb(g_src, b_src, slot):
        v = Wv(slot).bitcast(F32)
        P.dma('sp', Wb[slot], v[:, 0:1024], g_src.broadcast_to([128, 1024]))
        P.dma('sp', Wb[slot], v[:, 1024:2048], b_src.broadcast_to([128, 1024]))
        return v[:, 0:1024], v[:, 1024:2048], [Wb[slot]]

    g_ap, b_ap, gbb = load_gb(ln_in_g[0:1, :], ln_in_b[0:1, :], 3)
    d0 = 'h32' if n_layers > 0 else 'out'
    P.dma('sp', ZL[0][0], ZL[0][1], x_d[0:128, :])
    ln_tile(0, 0, g_ap, b_ap, gbb, d0, part='a')
    for t in range(NT):
        if t + 1 < NT:
            zi = (t + 1) % NZ
            P.dma('sp', ZL[zi][0], ZL[zi][1], x_d[(t + 1) * 128:(t + 2) * 128, :])
            ln_tile(t + 1, zi, g_ap, b_ap, gbb, d0, part='a')
        ln_tile(t, t % NZ, g_ap, b_ap, gbb, d0, part='b')


    wt_t = P.sb("wt", [128, 256], F32)
    psS = P.ps("psS", [128, 512], F32)
    psO = [psS, P.ps("psO1", [128, 512], F32)]
    ctr['wide'] = True
    kro_t = P.sb("kro", [128, 512], BF16)
    CB = [[Buf("CB%d_%d" % (c, r)) for r in range(4)] for c in range(4)]
    wsrc = lambda l: w_in[l].rearrange("(k p) n -> p k n", p=128)
    QS = 96.0 ** -0.5

    def merge_branch(l, b, srcT3, src_bufs, proj_d, first):
        Wg3 = Wv(0, 2).rearrange("p (k n) -> p k n", k=8)
        Wp3 = Wv(2).rearrange("p (c n) -> p c n", c=4)
        c0 = 2208 + b * 1024
        wload([Wb[0]], Wg3[:, 0:4, :], wsrc(l)[:, 0:4, c0:c0 + 1024])
        wload([Wb[1]], Wg3[:, 4:8, :], wsrc(l)[:, 4:8, c0:c0 + 1024])
        wload([Wb[2]], Wp3, proj_d.rearrange("(c p) n -> p c n", p=128))
        for T in range(4):
            tsl = slice(T * 512, (T + 1) * 512)
            for j in range(8):
                jsl = slice(j * 128, (j + 1) * 128)
                psy = nf()
                mm(psy, psy.ap[:, :], [(Wp3[:, c, jsl], srcT3[:, c, tsl]) for c in range(4)], [Wb[2]] + src_bufs)
                psg = nf()
                mm(psg, psg.ap[:, :], [(Wg3[:, k, jsl], hT3[:, k, tsl]) for k in range(8)], [Wb[0], Wb[1]] + hTb[4 * T:4 * T + 4])
                gb_, gs_ = ns()
                gs = gs_.bitcast(BF16)[:, 0:512]
                act(gs, psg.ap[:, :], AF.Sigmoid, [psg, colA], [gb_], bias=colA.ap[:, b * 8 + j:b * 8 + j + 1])
                if first:
                    tt('dve', M3[:, j, tsl], psy.ap[:, :], gs, ALU.mult, [psy, gb_], [Mb[T]])
                else:
                    tb_, tm_ = ns()
                    tmp = tm_.bitcast(BF16)[:, 0:512]
                    tt('dve', tmp, psy.ap[:, :], gs, ALU.mult, [psy, gb_], [tb_])
                    tt(MERGE_ENG, M3[:, j, tsl], M3[:, j, tsl], tmp, ALU.add, [Mb[T], tb_], [Mb[T]])

    def proj_T(l, T):
        W0 = Wv(0)[:, 0:8 * 384].rearrange("p (k n) -> p k n", k=8)
        W1 = Wv(1)[:, 0:8 * 288].rearrange("p (k n) -> p k n", k=8)
        cqT = Rv(0, 3, BF16).rearrange("p (k n) -> p k n", k=3)
        ckvT = Rv(3, 2, BF16).rearrange("p (k n) -> p k n", k=2)
        tsl = slice(T * 512, (T + 1) * 512)
        for (nj, Wx, dstT, rb0, col0) in ((3, W0, cqT, 0, 0), (2, W1, ckvT, 3, 16)):
            wbx = Wb[0] if nj == 3 else Wb[1]
            for j in range(nj):
                ps = nf()
                mm(ps, ps.ap[:, :], [(Wx[:, k, j * 128:(j + 1) * 128], hT3[:, k, tsl]) for k in range(8)], [wbx] + hTb[4 * T:4 * T + 4])
                cp('act', dstT[:, j, tsl], ps.ap[:, :], [ps], [Rb[rb0 + j]])
                sqb, sq = ns()
                act(sq, ps.ap[:, :], AF.Square, [ps], [sqb])

                def fn(e, sq=sq, T=T, j=j, nj=nj, col0=col0):
                    ins = None
                    for s_ in range(4):
                        c = col0 + 4 * T + s_
                        ins = e.matmul(psS.ap[:, c:c + 1], sq[:, s_ * 128:(s_ + 1) * 128], onesF.ap[:, 0:1],
                                       start=(c == 0 and j == 0), stop=(j == nj - 1), skip_group_check=True)
                    return ins
                P.op('pe', fn, [sqb, onesF], [psS])

    def layer_prologue(l):
        for (r0, n, src) in ((0, 24, b_gate[l]), (24, 4, pool_scale[l]), (28, 4, conv_b[l]), (32, 4, conv_ln_g[l]),
                             (36, 4, conv_ln_b[l]), (40, 3, q_norm_g[l]), (43, 2, kv_norm_g[l])):
            P.dma('sp', colsrcA, colsrcA.ap[r0:r0 + n, :], src)
        P.dma('sp', colsrcB, colsrcB.ap[:, :], conv_dw[l])
        pa = nf()
        tr(pa, pa.ap[:, 0:45], colsrcA.ap[0:45, :], identF.ap[0:45, 0:45], [colsrcA, identF])
        cp('dve', colA.ap[:, 0:45], pa.ap[:, 0:45], [pa], [colA])
        pa = nf()
        tr(pa, pa.ap[:, 0:124], colsrcB.ap[0:124, :], identF.ap[0:124, 0:124], [colsrcB, identF])
        cp('dve', colB.ap[:, 0:124], pa.ap[:, 0:124], [pa], [colB])

        W0 = Wv(0)[:, 0:8 * 384].rearrange("p (k n) -> p k n", k=8)
        W1 = Wv(1)[:, 0:8 * 288].rearrange("p (k n) -> p k n", k=8)
        Wq3 = Wv(2)[:, 0:3 * 768].rearrange("p (k n) -> p k n", k=3)
        wload([Wb[0]], W0, wsrc(l)[:, :, 1536:1920])
        wload([Wb[1]], W1, wsrc(l)[:, :, 1920:2208])
        wload([Wb[2]], Wq3, w_uq[l].rearrange("(k p) n -> p k n", p=128))
        for j in range(3):
            ts('dve', Wq3[:, j, :], Wq3[:, j, :], colA.ap[:, 40 + j:41 + j], None, ALU.mult, None, [Wb[2], colA], [Wb[2]])

    if n_layers > 0:
        layer_prologue(0)
    for l in range(n_layers):
        last = (l == n_layers - 1)
        W0 = Wv(0)[:, 0:8 * 384].rearrange("p (k n) -> p k n", k=8)
        W1 = Wv(1)[:, 0:8 * 288].rearrange("p (k n) -> p k n", k=8)
        Wq3 = Wv(2)[:, 0:3 * 768].rearrange("p (k n) -> p k n", k=3)
        Wkv3 = Wv(3)[:, 0:2 * 1024].rearrange("p (k n) -> p k n", k=2)
        wload([Wb[3]], Wkv3, w_ukv[l].rearrange("(k p) n -> p k n", p=128))
        for j in range(2):
            ts('dve', Wkv3[:, j, :], Wkv3[:, j, :], colA.ap[:, 43 + j:44 + j], None, ALU.mult, None, [Wb[3], colA], [Wb[3]])
        cqT = Rv(0, 3, BF16).rearrange("p (k n) -> p k n", k=3)
        ckvT = Rv(3, 2, BF16).rearrange("p (k n) -> p k n", k=2)
        if l == 0:
            for T in range(4):
                proj_T(l, T)
        rq = small.ap[:, 0:16]
        rkv = small.ap[:, 16:32]
        act(small.ap[:, 32:48], psS.ap[:, 0:16], AF.Sqrt, [psS, epsb], [small], bias=epsb.ap[:, 1:2], scale=1.0 / 384)
        act(small.ap[:, 48:64], psS.ap[:, 16:32], AF.Sqrt, [psS, epsb], [small], bias=epsb.ap[:, 1:2], scale=1.0 / 256)
        P.op('dve', lambda e: e.reciprocal(small.ap[:, 0:32], small.ap[:, 32:64]), [small], [small])
        ts('dve', rq, rq, QS, None, ALU.mult, None, [small], [small])
        psK = nf()
        for t in range(NT):
            mm(psK, psK.ap[:, t * 32:(t + 1) * 32], [(hT3[:, k, t * 128:(t + 1) * 128], W1[:, k, 256:288]) for k in range(8)], [Wb[1], hTb[t]])
        krb, kr_ = ns()
        cp('act', kr_, psK.ap[:, :], [psK], [krb])
        kr3 = kr_.rearrange("p (t d) -> p t d", t=16)
        kob = kro_t
        ko3 = kro_t.ap[:, :].rearrange("p (t d) -> p t d", t=16)
        tb1, t1_ = ns()
        tb2, t2_ = ns()
        t1 = t1_[:, 0:256].rearrange("p (t d) -> p t d", t=16)
        t2 = t2_[:, 0:256].rearrange("p (t d) -> p t d", t=16)
        tt('dve', t1, kr3[:, :, 0:16], cos3, ALU.mult, [krb, cosT], [tb1])
        tt('dve', t2, kr3[:, :, 16:32], sin3, ALU.mult, [krb, sinT], [tb2])
        tt('dve', ko3[:, :, 0:16], t1, t2, ALU.subtract, [tb1, tb2], [kob])
        tt('dve', t1, kr3[:, :, 16:32], cos3, ALU.mult, [krb, cosT, kob], [tb1])
        tt('dve', t2, kr3[:, :, 0:16], sin3, ALU.mult, [krb, sinT, kob], [tb2])
        tt('dve', ko3[:, :, 16:32], t1, t2, ALU.add, [tb1, tb2], [kob])

        qT3 = Rv(5, 4, BF16).rearrange("p (h n) -> p h n", h=4)
        kT3 = Rv(9, 4, BF16).rearrange("p (h n) -> p h n", h=4)
        v4 = Rv(13, 3, BF16)[:, 0:16 * 260].rearrange("p (t h d) -> p t h d", t=16, h=4)
        o_tm = M_t[:, 0:16 * 512].rearrange("p (t c) -> p t c", t=16)
        for g in range(2):
            P.op('pool', lambda e: e.memset(v4[:, :, :, 64:65], 1.0), [], Rb[13:16])
            def prep_front(t):
                tsl = slice(t * 128, (t + 1) * 128)
                psq = nf()
                mm(psq, psq.ap[:, 0:384], [(cqT[:, j, tsl], Wq3[:, j, g * 384:(g + 1) * 384]) for j in range(3)], [Wb[2]] + Rb[0:3])
                q32b, q32_ = ns()
                ts('dve', q32_[:, 0:384], psq.ap[:, 0:384], rq[:, t:t + 1], None, ALU.mult, None, [psq, small], [q32b])
                q3 = q32_[:, 0:384].rearrange("p (h d) -> p h d", h=4)
                qbb, qb_ = ns()
                qb3 = qb_.bitcast(BF16)[:, 0:384].rearrange("p (h d) -> p h d", h=4)
                cp(PREP_ENG, qb3[:, :, 0:64], q3[:, :, 0:64], [q32b], [qbb])
                cs = cos3[:, t, None, :].broadcast_to([128, 4, 16])
                sn = sin3[:, t, None, :].broadcast_to([128, 4, 16])
                ab, a_ = ns()
                a1 = a_[:, 0:64].rearrange("p (h d) -> p h d", h=4)
                a2 = a_[:, 64:128].rearrange("p (h d) -> p h d", h=4)
                a3 = a_[:, 128:192].rearrange("p (h d) -> p h d", h=4)
                a4 = a_[:, 192:256].rearrange("p (h d) -> p h d", h=4)
                tt('dve', a1, q3[:, :, 64:80], cs, ALU.mult, [q32b, cosT], [ab])
                tt(PREP_ENG, a2, q3[:, :, 80:96], sn, ALU.mult, [q32b, sinT], [ab])
                tt('dve', a3, q3[:, :, 80:96], cs, ALU.mult, [q32b, cosT], [ab])
                tt(PREP_ENG, a4, q3[:, :, 64:80], sn, ALU.mult, [q32b, sinT], [ab])
                tt('dve', qb3[:, :, 64:80], a1, a2, ALU.subtract, [ab], [qbb])
                tt('dve', qb3[:, :, 80:96], a3, a4, ALU.add, [ab], [qbb])
                pskv = nf()
                mm(pskv, pskv.ap[:, :], [(ckvT[:, j, tsl], Wkv3[:, j, g * 512:(g + 1) * 512]) for j in range(2)], [Wb[3]] + Rb[3:5])
                kv3 = pskv.ap[:, :].rearrange("p (h d) -> p h d", h=4)
                kbb, kb_ = ns()
                kb3 = kb_.bitcast(BF16)[:, 0:384].rearrange("p (h d) -> p h d", h=4)
                ts('dve', kb3[:, :, 0:64], kv3[:, :, 0:64], rkv[:, t:t + 1], None, ALU.mult, None, [pskv, small], [kbb])
                ts('dve', v4[:, t, :, 0:64], kv3[:, :, 64:128], rkv[:, t:t + 1], None, ALU.mult, None, [pskv, small], Rb[13:16])
                cp(PREP_ENG, kb3[:, :, 64:96], ko3[:, t, None, :].broadcast_to([128, 4, 32]), [kob], [kbb])
                return qbb, qb3, kbb, kb3

            def prep_back(t, st_):
                qbb, qb3, kbb, kb3 = st_
                tsl = slice(t * 128, (t + 1) * 128)
                pb = nb()
                for hh in range(4):
                    tr(pb, pb.ap[0:96, hh * 128:(hh + 1) * 128], qb3[:, hh, :], identB.ap[:], [qbb, identB])
                cp('act', qT3[0:96, :, tsl], pb.ap[0:96, 0:512].rearrange("p (h n) -> p h n", h=4), [pb], Rb[5:9])
                pb = nb()
                for hh in range(4):
                    tr(pb, pb.ap[0:96, hh * 128:(hh + 1) * 128], kb3[:, hh, :], identB.ap[:], [kbb, identB])
                cp('act', kT3[0:96, :, tsl], pb.ap[0:96, 0:512].rearrange("p (h n) -> p h n", h=4), [pb], Rb[9:13])

            st_ = prep_front(0)
            for t in range(NT):
                nx_ = prep_front(t + 1) if t + 1 < NT else None
                prep_back(t, st_)
                st_ = nx_
            ctr['wide'] = False
            steps = [(hh, Q, kt) for hh in range(4) for Q in range(4) for kt in range(4 * Q + 4)]

            def qk_exp(hh, Q, kt):
                qlo = max(512 * Q, 128 * kt)
                n = 512 * (Q + 1) - qlo
                pss = nf()
                mm(pss, pss.ap[:, 0:n], [(kT3[0:96, hh, kt * 128:(kt + 1) * 128], qT3[0:96, hh, qlo:qlo + n])], [Rb[5 + hh], Rb[9 + hh]])
                ptb, pt_ = ns()
                pt = pt_.bitcast(BF16)[:, 0:512]
                act(pt[:, 0:n], pss.ap[:, 0:n], AF.Exp, [pss], [ptb])
                if kt >= 4 * Q:
                    P.op('pool', lambda e, pt=pt: e.memset(pt[64:128, 0:64], 0.0), [ptb], [ptb])
                return ptb, pt, qlo

            cur = qk_exp(*steps[0])
            pso = None
            for i_, (hh, Q, kt) in enumerate(steps):
                nxt = qk_exp(*steps[i_ + 1]) if i_ + 1 < len(steps) else None
                ptb, pt, qlo = cur
                if kt == 0:
                    ctr['z'] += 1
                    pso = psO[ctr['z'] % 2]

                def fn(e, pt=pt, kt=kt, Q=Q, qlo=qlo, hh=hh, pso=pso):
                    ins = None
                    for s_ in range((qlo - 512 * Q) // 128, 4):
                        off = 512 * Q + 128 * s_ - qlo
                        ins = e.matmul(pso.ap[:, s_ * 65:(s_ + 1) * 65], pt[:, off:off + 128], v4[:, kt, hh, 0:65],
                                       start=(kt == 0 and s_ == 0), stop=(kt == 4 * Q + s_), skip_group_check=True)
                    return ins
                P.op('pe', fn, [ptb] + Rb[13:16], [pso])
                if kt == 4 * Q + 3:
                    pso3 = pso.ap[:, 0:260].rearrange("p (s d) -> p s d", s=4)
                    rdb, rd_ = ns()
                    rd3 = rd_[:, 0:4].rearrange("p (s o) -> p s o", o=1)
                    P.op('dve', lambda e, rd3=rd3, pso3=pso3: e.reciprocal(rd3, pso3[:, :, 64:65]), [pso], [rdb])
                    hc = (4 * g + hh) * 64
                    tt('dve', o_tm[:, 4 * Q:4 * Q + 4, hc:hc + 64], pso3[:, :, 0:64], rd3.broadcast_to([128, 4, 64]), ALU.mult,
                       [pso, rdb], Mb[0:4])
                cur = nxt
        if tap == 'qk':
            P.dma('sp', dbg_buf, dbg_d[0], R_t[:, :], reads=Rb[0:16])
            P.dma('sp', dbg_buf, dbg_d[1, :, 0:8192], M_t[:, :].bitcast(F32), reads=Mb[0:4])
            break
        ctr['wide'] = True
        ctr['wide2'] = True
        oT3 = Rv(0, 4, BF16).rearrange("p (c n) -> p c n", c=4)
        for t in range(NT):
            pb = nb()
            for c in range(4):
                tr(pb, pb.ap[:, c * 128:(c + 1) * 128], o_tm[:, t, c * 128:(c + 1) * 128], identB.ap[:], Mb[0:4] + [identB])
            cp('act', oT3[:, :, t * 128:(t + 1) * 128], pb.ap[:, 0:512].rearrange("p (c n) -> p c n", c=4), [pb], Rb[0:4])
        merge_branch(l, 2, oT3, Rb[0:4], mla_proj[l], True)
        if tap:
            P.dma('pool', dbg_buf, dbg_d[0], M_t[:, :], reads=Mb[0:4])

        W3 = Wv(3).rearrange("p (k n) -> p k n", k=8)
        wload([Wb[3]], W3, wsrc(l)[:, :, 0:512])
        pw3 = poolw_t.ap[:].rearrange("p (g d) -> p g d", g=4)
        P.dma('pool', poolw_t, pw3, pool_w[l].rearrange("g c d -> c g d"))
        sU = Rv(0, 3)[:, 0:2064]
        sB_ = Rv(3, 3)[:, 0:2064]
        sC = Rv(11, 3)[:, 0:2064]
        mixT = Rv(6, 1, BF16)
        pmT3 = Rv(7, 4, BF16).rearrange("p (c n) -> p c n", c=4)
        P.op('pool', lambda e: e.memset(sU[:, 0:16], 0.0), [], Rb[0:3])
        P.op('pool', lambda e: e.memset(sB_[:, 0:16], 0.0), [], Rb[3:6])
        P.op('pool', lambda e: e.memset(sC[:, 0:16], 0.0), [], Rb[11:14])
        for g in range(4):
            for T in range(4):
                ps = nf()
                mm(ps, ps.ap[:, :], [(W3[:, k, g * 128:(g + 1) * 128], hT3[:, k, T * 512:(T + 1) * 512]) for k in range(8)], [Wb[3]] + hTb[4 * T:4 * T + 4])
                cp('act', sU[:, 16 + T * 512:16 + (T + 1) * 512], ps.ap[:, :], [ps], Rb[0:3])
            src, srcb = sU, Rb[0:3]
            pp_ = [(sB_, Rb[3:6]), (sC, Rb[11:14])]
            for jj in range(g + 1):
                sh = 1 << jj
                dst, dstb = pp_[jj % 2]
                tt('dve', dst[:, 16:2064], src[:, 16:2064], src[:, 16 - sh:2064 - sh], ALU.add, srcb, dstb)
                src, srcb = dst, dstb
            w = 2 << g
            stt('dve', mixT[:, 0:2048], src[:, 16:2064], 1.0 / w, sU[:, 16:2064], ALU.mult, ALU.subtract, srcb + Rb[0:3], [Rb[6]])
            fb, f_ = ns()
            tt('dve', f_[:, 0:w - 1], src[:, 16:16 + w - 1], rcnt.ap[:, 0:w - 1], ALU.mult, srcb + [rcnt], [fb])
            tt('dve', mixT[:, 0:w - 1], f_[:, 0:w - 1], sU[:, 16:16 + w - 1], ALU.subtract, [fb] + Rb[0:3], [Rb[6]])
            for T in range(4):
                ps = nf()
                mm(ps, ps.ap[:, :], [(pw3[:, g, :], mixT[:, T * 512:(T + 1) * 512])], [poolw_t, Rb[6]])
                ts('dve', pmT3[:, g, T * 512:(T + 1) * 512], ps.ap[:, :], colA.ap[:, 24 + g:25 + g], None, ALU.mult, None, [ps, colA], [Rb[7 + g]])
        merge_branch(l, 0, pmT3, Rb[7:11], pool_proj[l], False)
        if tap:
            P.dma('pool', dbg_buf, dbg_d[1], M_t[:, :], reads=Mb[0:4])

        wload([Wb[3]], W3, wsrc(l)[:, :, 512:1024])
        W0c = Wv(0).rearrange("p (k n) -> p k n", k=8)
        wload([Wb[0]], W0c, wsrc(l)[:, :, 1024:1536])
        zcb = Rv(0, 2, BF16)[:, 0:2080]
        conv4 = Rv(3, 8).rearrange("p (c n) -> p c n", c=4)
        cact3 = Rv(11, 4, BF16).rearrange("p (c n) -> p c n", c=4)
        P.op('pool', lambda e: e.memset(zcb[:, 0:32], 0.0), [], Rb[0:2])
        for c in range(4):
            dslot = 1 + (c % 2)
            D3 = Wv(dslot).rearrange("p (k n) -> p k n", k=32)
            for k in range(31):
                ts('dve', D3[:, k, :], identB.ap[:], colB.ap[:, k * 4 + c:k * 4 + c + 1], None, ALU.mult, None, [identB, colB], [Wb[dslot]])
            for T in range(4):
                tsl = slice(T * 512, (T + 1) * 512)
                psa = nf()
                mm(psa, psa.ap[:, :], [(W3[:, k, c * 128:(c + 1) * 128], hT3[:, k, tsl]) for k in range(8)], [Wb[3]] + hTb[4 * T:4 * T + 4])
                psg = nf()
                mm(psg, psg.ap[:, :], [(W0c[:, k, c * 128:(c + 1) * 128], hT3[:, k, tsl]) for k in range(8)], [Wb[0]] + hTb[4 * T:4 * T + 4])
                sgb, sg = ns()
                act(sg, psg.ap[:, :], AF.Sigmoid, [psg], [sgb])
                tt('dve', zcb[:, 32 + T * 512:32 + (T + 1) * 512], psa.ap[:, :], sg, ALU.mult, [psa, sgb], Rb[0:2])
            for T in range(4):
                psc = nf()
                mm(psc, psc.ap[:, :], [(D3[:, k, :], zcb[:, 2 + k + T * 512:2 + k + (T + 1) * 512]) for k in range(31)], [Wb[dslot]] + Rb[0:2])
                ts('dve', conv4[:, c, T * 512:(T + 1) * 512], psc.ap[:, :], colA.ap[:, 28 + c:29 + c], None, ALU.add, None,
                   [psc, colA], [Rb[3 + 2 * c + T // 2]])
        for T in range(4):
            tsl = slice(T * 512, (T + 1) * 512)
            cbs = [[Rb[3 + 2 * c + T // 2]] for c in range(4)]
            ps1 = nf()
            mm(ps1, ps1.ap[:, :], [(onesF.ap[:], conv4[:, c, tsl]) for c in range(4)], [onesF] + sum(cbs, []))
            sqs = []
            for c in range(4):
                sqb, sq = ns()
                act(sq, conv4[:, c, tsl], AF.Square, cbs[c], [sqb])
                sqs.append((sqb, sq))
            ps2 = nf()
            mm(ps2, ps2.ap[:, :], [(onesF.ap[:], sq) for (_, sq) in sqs], [onesF] + [b_ for (b_, _) in sqs])
            mb_, m_ = ns()
            P.op('act', lambda e, m_=m_, ps1=ps1: e.mul(m_, ps1.ap[:, :], 1.0 / 512), [ps1], [mb_])
            qb_, msq = ns()
            tt('dve', msq, m_, m_, ALU.mult, [mb_], [qb_])
            vb_, var = ns()
            stt('dve', var, ps2.ap[:, :], 1.0 / 512, msq, ALU.mult, ALU.subtract, [ps2, qb_], [vb_])
            act(var, var, AF.Sqrt, [vb_, epsb], [vb_], bias=epsb.ap[:, 0:1])
            P.op('dve', lambda e, var=var: e.reciprocal(var, var), [vb_], [vb_])
            for c in range(4):
                t1b, t1 = ns()
                tt('dve', t1, conv4[:, c, tsl], m_, ALU.subtract, cbs[c] + [mb_], [t1b])
                tt('dve', t1, t1, var, ALU.mult, [t1b, vb_], [t1b])
                ts('dve', t1, t1, colA.ap[:, 32 + c:33 + c], colA.ap[:, 36 + c:37 + c], ALU.mult, ALU.add, [t1b, colA], [t1b])
                act(cact3[:, c, tsl], t1, AF.Silu, [t1b], [Rb[11 + c]])
        merge_branch(l, 1, cact3, Rb[11:15], conv_proj[l], False)
        if tap:
            P.dma('pool', dbg_buf, dbg_d[2], M_t[:, :], reads=Mb[0:4])

        Wo3 = Wv(0, 2).rearrange("p (k n) -> p k n", k=8)
        wload([Wb[0]], Wo3[:, 0:4, :], w_out[l].rearrange("(k p) n -> p k n", p=128)[:, 0:4, :])
        wload([Wb[1]], Wo3[:, 4:8, :], w_out[l].rearrange("(k p) n -> p k n", p=128)[:, 4:8, :])
        g_ap, b_ap, gbb = load_gb(ln1_g[l:l + 1, :], ln1_b[l:l + 1, :], 3)
        if tap == 'h32' and last:
            P.dma('sp', dbg_buf, dbg_d[0].rearrange("p (t d) -> p t d", t=16), h32_d.rearrange("(t p) d -> p t d", p=128), reads=[h32_buf])
            P.dma('sp', dbg_buf, dbg_d[1, :, 0:8192], hT_t[:, :].bitcast(F32), reads=hTb[0:16])
        def ln1_front(t):
            zi = t % NZ
            zbuf, z = ZL[zi]
            P.dma('sp', zbuf, z, h32_d[t * 128:(t + 1) * 128, :], reads=[h32_buf])
            for h in range(2):
                psy = nf()
                mm(psy, psy.ap[:, :], [(M3[:, k, t * 128:(t + 1) * 128], Wo3[:, k, h * 512:(h + 1) * 512]) for k in range(8)], [Wb[0], Wb[1], Mb[t // 4]])
                stt('dve', z[:, h * 512:(h + 1) * 512], z[:, h * 512:(h + 1) * 512], ALPHA, psy.ap[:, :], ALU.mult, ALU.add, [zbuf, psy], [zbuf])

        d1 = 'out' if (tap == 'ln1' and last) else ('acc+out' if (tap == 'ffn2' and last) else 'acc')
        ln1_front(0)
        ln1_front(1)
        ln_tile(0, 0, g_ap, b_ap, gbb, d1, part='a')
        for t in range(NT):
            if t + 2 < NT:
                ln1_front(t + 2)
            if t + 1 < NT:
                ln_tile(t + 1, (t + 1) % NZ, g_ap, b_ap, gbb, d1, part='a')
            ln_tile(t, t % NZ, g_ap, b_ap, gbb, d1, part='b')
        if tap == 'ln1' and last:
            break
        if tap == 'h32' and last and os.environ.get("BRK") == "ln1":
            break

        Wpg3 = Wv(0, 2).rearrange("p (k n) -> p k n", k=8)
        wload([Wb[0]], Wpg3[:, 0:4, :], ple_gate[l].rearrange("(k p) n -> p k n", p=128)[:, 0:4, :])
        wload([Wb[1]], Wpg3[:, 4:8, :], ple_gate[l].rearrange("(k p) n -> p k n", p=128)[:, 4:8, :])
        Wpp3 = Wv(3)[:, 0:2048].rearrange("p (c n) -> p c n", c=2)
        wload([Wb[3]], Wpp3, ple_proj[l].rearrange("(c p) n -> p c n", p=128))
        pT3 = Wv(2).rearrange("p (c n) -> p c n", c=2)
        for t in range(NT):
            sbf, sap = ns()
            pin = sap.bitcast(BF16)[:, 0:256]
            P.dma('pool', sbf, pin, p_d[l, t * 128:(t + 1) * 128, :])
            pb = nb()
            for c in range(2):
                tr(pb, pb.ap[:, c * 128:(c + 1) * 128], pin[:, c * 128:(c + 1) * 128], identB.ap[:], [sbf, identB])
            cp('act', pT3[:, :, t * 128:(t + 1) * 128], pb.ap[:, 0:256].rearrange("p (c n) -> p c n", c=2), [pb], [Wb[2]])
        psr = nf()
        wr3 = wr_t.ap[:].rearrange("p (k e) -> p k e", k=8)
        for t in range(NT):
            mm(psr, psr.ap[:, t * 16:(t + 1) * 16], [(hT3[:, k, t * 128:(t + 1) * 128], wr3[:, k, :]) for k in range(8)], [wr_t, hTb[t]])
        ab_, aff = ns(); aff = aff[:, 0:256]
        act(aff, psr.ap[:, 0:256], AF.Sigmoid, [psr], [ab_])
        sb2, sel = ns(); sel = sel[:, 0:256]
        tt('dve', sel.rearrange("p (t e) -> p t e", t=16), aff.rearrange("p (t e) -> p t e", t=16),
           rb_t.ap[:, None, :].broadcast_to([128, 16, 16]), ALU.add, [ab_, rb_t], [sb2])
        sel4 = sel.rearrange("p (g e) -> p g e", e=4)
        xb1, x1 = ns(); xb2, x2 = ns(); xb3, x3 = ns(); xb4, x4 = ns()
        m1 = x1[:, 0:64]; m2 = x1[:, 64:128]; gs_ = x1[:, 128:192]; gmx = x1[:, 192:208]; pen = x1[:, 256:320]
        e1 = x1[:, 320:336]; e2 = x1[:, 336:352]; wsum = x1[:, 352:368]
        eq = x2[:, 0:256]; sel2 = x3[:, 0:256]; mk = x4[:, 0:256]
        red(m1, sel4, ALU.max, [sb2], [xb1])
        tt('dve', eq.rearrange("p (g e) -> p g e", e=4), sel4, m1[:, :, None].broadcast_to([128, 64, 4]), ALU.is_equal, [sb2, xb1], [xb2])
        stt('dve', sel2, eq, -BIG, sel, ALU.mult, ALU.add, [xb2, sb2], [xb3])
        red(m2, sel2.rearrange("p (g e) -> p g e", e=4), ALU.max, [xb3], [xb1])
        tt('dve', gs_, m1, m2, ALU.add, [xb1], [xb1])
        red(gmx, gs_.rearrange("p (t g) -> p t g", g=4), ALU.max, [xb1], [xb1])
        tt('dve', pen.rearrange("p (t g) -> p t g", g=4), gs_.rearrange("p (t g) -> p t g", g=4),
           gmx[:, :, None].broadcast_to([128, 16, 4]), ALU.is_equal, [xb1], [xb1])
        ts('dve', pen, pen, BIG, -BIG, ALU.mult, ALU.add, [xb1], [xb1])
        tt('dve', sel2.rearrange("p (g e) -> p g e", e=4), sel4, pen[:, :, None].broadcast_to([128, 64, 4]), ALU.add, [sb2, xb1], [xb3])
        sel2_3 = sel2.rearrange("p (t e) -> p t e", t=16)
        red(e1, sel2_3, ALU.max, [xb3], [xb1])
        tt('dve', mk.rearrange("p (t e) -> p t e", t=16), sel2_3, e1[:, :, None].broadcast_to([128, 16, 16]), ALU.is_equal, [xb3, xb1], [xb4])
        stt('dve', eq, mk, -2 * BIG, sel2, ALU.mult, ALU.add, [xb4, xb3], [xb2])
        eq3 = eq.rearrange("p (t e) -> p t e", t=16)
        red(e2, eq3, ALU.max, [xb2], [xb1])
        tt('dve', sel2_3, eq3, e2[:, :, None].broadcast_to([128, 16, 16]), ALU.is_equal, [xb2, xb1], [xb3])
        tt('dve', mk, mk, sel2, ALU.add, [xb4, xb3], [xb4])
        mkb = x3[:, 256:384].bitcast(BF16)
        cp('dve', mkb, mk, [xb4], [xb3])
        w_ = eq
        tt('dve', w_, mk, aff, ALU.mult, [xb4, ab_], [xb2])
        P.op('dve', lambda e, wsum=wsum, w_=w_: e.tensor_reduce(wsum, w_.rearrange("p (t e) -> p t e", t=16), AX.X, ALU.add), [xb2], [xb1])
        P.op('dve', lambda e, wsum=wsum: e.reciprocal(wsum, wsum), [xb1], [xb1])
        tt('dve', w_.rearrange("p (t e) -> p t e", t=16), w_.rearrange("p (t e) -> p t e", t=16),
           wsum[:, :, None].broadcast_to([128, 16, 16]), ALU.mult, [xb2, xb1], [xb2])
        psR = nf()

        def fn_rank(e, mkb=mkb, psR=psR):
            ins = None
            first = True
            for i in range(NT):
                for i2 in range(i + 1):
                    ins = e.matmul(psR.ap[:, i * 16:(i + 1) * 16], (upperB.ap[:] if i2 == i else onesB.ap[:]), mkb[:, i2 * 16:(i2 + 1) * 16],
                                   start=first, stop=(i2 == i), skip_group_check=True)
                    first = False
            return ins
        P.op('pe', fn_rank, [xb3, onesB, upperB], [psR])
        xb5, x5 = ns(); xb6, x6 = ns()
        dst = x5[:, 0:256]; lt = x5[:, 256:512]; dm = x6[:, 0:256]; eqa = x6[:, 256:512]
        v3 = lambda a: a.rearrange("p (t e) -> p t e", t=16)
        ts('dve', lt, psR.ap[:, 0:256], float(CAP), None, ALU.is_lt, None, [psR], [xb5])
        tt('dve', v3(dst), v3(psR.ap[:, 0:256]), ecap_t.ap[:, None, :].broadcast_to([128, 16, 16]), ALU.add, [psR, ecap_t], [xb5])
        ts('dve', dst, dst, -float(NSLOT), None, ALU.add, None, [xb5], [xb5])
        tt('dve', dst, dst, lt, ALU.mult, [xb5], [xb5])
        ts('dve', dst, dst, float(NSLOT), None, ALU.add, None, [xb5], [xb5])
        ts('dve', dm, dst, -BIGV, None, ALU.add, None, [xb5], [xb6])
        tt('dve', dm, dm, mk, ALU.mult, [xb6, xb4], [xb6])
        ts('dve', dm, dm, BIGV, None, ALU.add, None, [xb6], [xb6])
        destA = wt_t.ap[:, 32:48]; destB = wt_t.ap[:, 48:64]; wA = wt_t.ap[:, 0:16]; wB = wt_t.ap[:, 16:32]
        idxA = wt_t.ap[:, 64:80].bitcast(I32); idxB = wt_t.ap[:, 80:96].bitcast(I32)
        P.op('dve', lambda e, dm=dm: e.tensor_reduce(destA, v3(dm), AX.X, ALU.min), [xb6], [wt_t])
        tt('dve', v3(eqa), v3(dm), destA[:, :, None].broadcast_to([128, 16, 16]), ALU.is_equal, [xb6, wt_t], [xb6])
        tt('dve', eqa, eqa, w_, ALU.mult, [xb6, xb2], [xb6])
        P.op('dve', lambda e, eqa=eqa: e.tensor_reduce(wA, v3(eqa), AX.X, ALU.add), [xb6], [wt_t])
        ts('dve', wB, wA, -1.0, 1.0, ALU.mult, ALU.add, [wt_t], [wt_t])
        ts('dve', dm, dst, 1.0, None, ALU.add, None, [xb5], [xb6])
        tt('dve', dm, dm, mk, ALU.mult, [xb6, xb4], [xb6])
        ts('dve', dm, dm, -1.0, None, ALU.add, None, [xb6], [xb6])
        P.op('dve', lambda e, dm=dm: e.tensor_reduce(destB, v3(dm), AX.X, ALU.max), [xb6], [wt_t])
        cp('dve', idxA, destA, [wt_t], [wt_t])
        cp('dve', idxB, destB, [wt_t], [wt_t])
        stage4 = [(HBb[0], HB_t[:, 0:1024]), (HBb[1], HB_t[:, 1024:2048]),
                  (Zb[0], Zs[0][:, 0:512].bitcast(BF16)), (Zb[1], Zs[1][:, 0:512].bitcast(BF16))]
        for t in range(NT):
            sbuf_, hbt = stage4[t % 4]
            P.dma('sp', sbuf_, hbt, hbf_d[t * 128:(t + 1) * 128, :], reads=[hbf_bufs[t]])
            for ix in (idxA, idxB):
                P.dma_fn('pool', xs_bufs[t], lambda e, ix=ix, t=t, hbt=hbt: e.indirect_dma_start(
                    out=xs_d[:, :], out_offset=bass.IndirectOffsetOnAxis(ap=ix[:, t:t + 1], axis=0),
                    in_=hbt, in_offset=None), reads=[sbuf_, wt_t])

        for t in range(NT):
            tsl = slice(t * 128, (t + 1) * 128)
            for h in range(2):
                hsl = slice(h * 512, (h + 1) * 512)
                pse = nf()
                mm(pse, pse.ap[:, :], [(pT3[:, c, tsl], Wpp3[:, c, hsl]) for c in range(2)], [Wb[2], Wb[3]])
                psg = nf()
                mm(psg, psg.ap[:, :], [(hT3[:, k, tsl], Wpg3[:, k, hsl]) for k in range(8)], [Wb[0], Wb[1], hTb[t]])
                sgb, sg = ns()
                act(sg, psg.ap[:, :], AF.Sigmoid, [psg], [sgb])
                tb_, tmp = ns()
                tt('dve', tmp, pse.ap[:, :], sg, ALU.mult, [pse, sgb], [tb_])
                accv = R_t[:, t * 1024 + h * 512:t * 1024 + (h + 1) * 512]
                tt('dve', accv, accv, tmp, ALU.add, [Rb[t], tb_], [Rb[t]])

        def wset(e_):
            if e_ % 2 == 0:
                return (Wv(0, 2).rearrange("p (k n) -> p k n", k=8), Wv(2).rearrange("p (k n) -> p k n", k=4),
                        [Wb[0], Wb[1]], [Wb[2]])
            return (M_t[:, 0:8192].rearrange("p (k n) -> p k n", k=8), M_t[:, 8192:12288].rearrange("p (k n) -> p k n", k=4),
                    Mb[0:4], Mb[0:4])

        def wfetch(e_):
            Wu3, Wd3, ub, db = wset(e_)
            src = exp_w_up[l, e_].rearrange("(k p) n -> p k n", p=128)
            if e_ % 2 == 0:
                wload([Wb[0]], Wu3[:, 0:4, :], src[:, 0:4, :])
                wload([Wb[1]], Wu3[:, 4:8, :], src[:, 4:8, :])
                wload([Wb[2]], Wd3, exp_w_down[l, e_].rearrange("(k p) n -> p k n", p=128))
            else:
                wload(Mb[0:4], Wu3, src)
                wload(Mb[0:4], Wd3, exp_w_down[l, e_].rearrange("(k p) n -> p k n", p=128))
        ctr['smin'] = 4
        xsT_views = [S_t[:, 0:2048].bitcast(BF16).rearrange("p (k n) -> p k n", k=8),
                     M_t[:, 12288:16384].rearrange("p (k n) -> p k n", k=8)]
        XB = Buf("XB")
        ysl = Wv(3).bitcast(F32)
        land4 = [(HBb[0], HB_t[:, 0:1024]), (HBb[1], HB_t[:, 1024:2048]),
                 (Zb[0], Zs[0][:, 0:512].bitcast(BF16)), (Zb[1], Zs[1][:, 0:512].bitcast(BF16))]

        def prep_load(e_):
            for blk in range(4):
                lb, lap = land4[blk]
                r0 = e_ * CAP + blk * 128
                P.dma('sp', lb, lap, xs_d[r0:r0 + 128, :], reads=xs_bufs)

        def prep_T(e_):
            xsT3 = xsT_views[e_ % 2]
            xbufs = Sb[0:4] if e_ % 2 == 0 else [XB]
            for blk in range(4):
                lb, lap = land4[blk]
                pb = nb()
                for j in range(8):
                    tr(pb, pb.ap[:, j * 128:(j + 1) * 128], lap[:, j * 128:(j + 1) * 128], identB.ap[:], [lb, identB])
                cp('dve', xsT3[:, :, blk * 128:(blk + 1) * 128], pb.ap[:].rearrange("p (k n) -> p k n", k=8), [pb],
                   xbufs + (Mb[0:4] if (e_ == 1 and blk == 0) else []))

        def compute_up(e_):
            Wu3, Wd3, ub, db = wset(e_)
            xsT3 = xsT_views[e_ % 2]
            xbufs = Sb[0:4] if e_ % 2 == 0 else [XB]
            ai = e_ % 2
            aT3 = actT_t.ap[:, ai * 2048:(ai + 1) * 2048].rearrange("p (j n) -> p j n", j=4)
            for j in range(4):
                psg = nf()
                mm(psg, psg.ap[:, :], [(Wu3[:, k, j * 128:(j + 1) * 128], xsT3[:, k, :]) for k in range(8)], ub + xbufs)
                psu = nf()
                mm(psu, psu.ap[:, :], [(Wu3[:, k, 512 + j * 128:512 + (j + 1) * 128], xsT3[:, k, :]) for k in range(8)], ub + xbufs)
                sgb, sg_ = ns()
                sg = sg_.bitcast(BF16)[:, 0:512]
                act(sg, psg.ap[:, :], AF.Silu, [psg], [sgb])
                tt('dve', aT3[:, j, :], psu.ap[:, :], sg, ALU.mult, [psu, sgb], [actTb[ai]])

        def compute_down(e_):
            Wu3, Wd3, ub, db = wset(e_)
            ai = e_ % 2
            aT3 = actT_t.ap[:, ai * 2048:(ai + 1) * 2048].rearrange("p (j n) -> p j n", j=4)
            for s_ in range(4):
                yi = s_ % 2
                first_ = (e_ == 0 and s_ < 2)
                final_ = (e_ == 15 and s_ >= 2)
                for h in range(2):
                    psd = nf()
                    mm(psd, psd.ap[:, :], [(aT3[:, j, s_ * 128:(s_ + 1) * 128], Wd3[:, j, h * 512:(h + 1) * 512]) for j in range(4)], db + [actTb[ai]])
                    cp('act', ysl[:, yi * 1024 + h * 512:yi * 1024 + (h + 1) * 512], psd.ap[:, :], [psd], [YSb[yi]] + ([Wb[3]] if first_ else []))
                r0 = e_ * CAP + s_ * 128
                P.dma('act', ys_bufs[e_], ys_d[r0:r0 + 128, :], ysl[:, yi * 1024:(yi + 1) * 1024], reads=[YSb[yi]] + ([Wb[3]] if final_ else []))

        wfetch(0)
        wfetch(1)
        prep_load(0)
        prep_T(0)
        for e_ in range(16):
            if e_ + 1 < 16:
                prep_load(e_ + 1)
            compute_up(e_)
            if e_ + 1 < 16:
                prep_T(e_ + 1)
            compute_down(e_)
            if e_ + 2 < 16:
                wfetch(e_ + 2)
        if not last:
            layer_prologue(l + 1)
        ctr['wide2'] = False
        g_ap, b_ap, gbb = load_gb(ln2_g[l:l + 1, :], ln2_b[l:l + 1, :], 3)
        land = [(Zb[0], Zs[0][:, :]), (Zb[1], Zs[1][:, :])]
        for t in range(NT):
            accv = R_t[:, t * 1024:(t + 1) * 1024]
            for q_, (ix, wv) in enumerate(((idxA, wA), (idxB, wB))):
                lb, lap = land[q_]
                P.dma_fn('pool', lb, lambda e, lap=lap, ix=ix, t=t: e.indirect_dma_start(
                    out=lap, out_offset=None, in_=ys_d[:, :],
                    in_offset=bass.IndirectOffsetOnAxis(ap=ix[:, t:t + 1], axis=0)),
                    reads=ys_bufs + [wt_t])
                stt('dve', accv, lap, wv[:, t:t + 1], accv, ALU.mult, ALU.add, [lb, wt_t, Rb[t]], [Rb[t]])
            zi = 2 + t % 2
            d2 = 'out' if (last and tap != 'ffn2') else 'h32'
            cp('act', ZL[zi][1], accv, [Rb[t]], [ZL[zi][0]])
            ln_tile(t, zi, g_ap, b_ap, gbb, d2, part='a')
            for tb in ([t - 1] if t >= 1 else []) + ([t] if t == NT - 1 else []):
                ln_tile(tb, 2 + tb % 2, g_ap, b_ap, gbb, d2, part='b')
                if (not last) and tb in (4, 7, 11, 15):
                    proj_T(l + 1, (4, 7, 11, 15).index(tb))
        ctr['smin'] = 0

    P.finish([out_buf] + ([dbg_buf] if tap else []))
    return nc


def make_in_maps(inp, cores=range(8)):
    f = lambda a: np.ascontiguousarray(np.asarray(a, dtype=np.float32))
    invf = (10000.0 ** (-np.arange(0, 32, 2, dtype=np.float32) / 32)).astype(np.float32)
    shared = {
        "invf": np.ascontiguousarray(np.broadcast_to(invf[None, :], (128, 16))),
        "ecap": np.ascontiguousarray(np.broadcast_to((np.arange(16, dtype=np.float32) * CAP)[None, :], (128, 16))),
        "rcnt": np.ascontiguousarray(np.broadcast_to((1.0 / np.arange(1, 17, dtype=np.float32))[None, :], (128, 16))),
        "ln_in_g": f(inp["ln_in_g"]).reshape(1, D), "ln_in_b": f(inp["ln_in_b"]).reshape(1, D),
        "w_in": f(inp["w_in"]), "b_gate": f(inp["b_gate"]).reshape(4, 24, 128),
        "pool_w": f(inp["pool_w"]), "pool_scale": f(inp["pool_scale"]).reshape(4, 4, 128),
        "pool_proj": f(inp["pool_proj"]), "conv_dw": f(inp["conv_dw"]).reshape(4, 124, 128),
        "conv_b": f(inp["conv_b"]).reshape(4, 4, 128), "conv_ln_g": f(inp["conv_ln_g"]).reshape(4, 4, 128),
        "conv_ln_b": f(inp["conv_ln_b"]).reshape(4, 4, 128), "conv_proj": f(inp["conv_proj"]),
        "q_norm_g": f(inp["q_norm_g"]).reshape(4, 3, 128), "w_uq": f(inp["w_uq"]),
        "kv_norm_g": f(inp["kv_norm_g"]).reshape(4, 2, 128), "w_ukv": f(inp["w_ukv"]),
        "mla_proj": f(inp["mla_proj"]), "w_out": f(inp["w_out"]), "ln1_g": f(inp["ln1_g"]), "ln1_b": f(inp["ln1_b"]),
        "w_router": f(inp["w_router"]), "router_bias": f(inp["router_bias"]).reshape(1, 16),
        "exp_w_up": f(inp["exp_w_up"]), "exp_w_down": f(inp["exp_w_down"]),
        "ple_proj": f(inp["ple_proj"]), "ple_gate": f(inp["ple_gate"]), "ln2_g": f(inp["ln2_g"]), "ln2_b": f(inp["ln2_b"]),
    }
    maps = []
    for c in cores:
        m = dict(shared)
        m["x"] = f(inp["x"][c])
        m["p"] = f(inp["p"][:, c])
        m["pos"] = np.ascontiguousarray(np.asarray(inp["positions"][c], dtype=np.int32).reshape(16, 128))
        maps.append(m)
    return maps


_NC_CACHE = {}


def kernel(**inputs):
    if 'nc' not in _NC_CACHE:
        _NC_CACHE['nc'] = build_nc()
    nc = _NC_CACHE['nc']
    maps = make_in_maps(inputs)
    res = run_bass_kernel_spmd(nc, maps, core_ids=list(range(8)))
    return np.stack([np.asarray(r["out"], dtype=np.float32) for r in res.results], axis=0)
```
